# Optimizing a Trainium2 kernel written in Bass

```python
import math
import jax, jax.numpy as jnp
from jax import lax
import numpy as np

D_MODEL = 1024
BATCH = 16
SEQ = 2048
DEPTH = 4

CTX_LEN = 256
GRID_W = 64
N_MOD = 6
EPS = 1e-6
ATTN_HEADS = 4
HEAD_DIM = 64
ATTN_VDIM = 2 * HEAD_DIM
Q_WIDTH = ATTN_HEADS * 2 * HEAD_DIM
ATTN_WIDTH = ATTN_HEADS * ATTN_VDIM
ROPE_BASE = 10000.0
Q_BLOCK = 128
SSD_HEADDIM = 64
SSD_GROUPS = 2
SSD_HPG = 4
SSD_HEADS = SSD_GROUPS * SSD_HPG
SSD_INNER = SSD_HEADS * SSD_HEADDIM
D_STATE = 128
D_CONV = 3
CHUNK = 128
CONV_DIM = SSD_INNER + 2 * SSD_GROUPS * D_STATE
MIX_WIDTH = ATTN_WIDTH + SSD_INNER
P_IN = 2 * Q_WIDTH + ATTN_WIDTH + SSD_INNER + CONV_DIM + 2 * SSD_HEADS
N_EXPERTS = 16
EC_CAPACITY = 2
D_EXPERT = 1408

kernel_name = "hybrid_diffattn_ssd_ecmoe_dit"


def rmsnorm(x, w):
    xf = x.astype(jnp.float32)
    y = xf * lax.rsqrt(jnp.mean(xf * xf, axis=-1, keepdims=True) + EPS)
    return (y * w.astype(jnp.float32)).astype(x.dtype)


def axial_rope_tables(n):
    rows = n // GRID_W
    row = jnp.repeat(jnp.arange(rows), GRID_W)
    col = jnp.tile(jnp.arange(GRID_W), rows)
    n_freq = HEAD_DIM // 4
    inv = ROPE_BASE ** (-jnp.arange(n_freq, dtype=jnp.float32) / n_freq)
    ang = jnp.stack([row, col], axis=-1).astype(jnp.float32)[..., None] * inv
    return jnp.cos(ang), jnp.sin(ang)


def apply_rope(x, cos, sin):
    xr = x.reshape(x.shape[:-1] + (2, 2, HEAD_DIM // 4))
    x1, x2 = xr[..., 0, :], xr[..., 1, :]
    c = cos[:, None, None].astype(x.dtype)
    s = sin[:, None, None].astype(x.dtype)
    out = jnp.stack([x1 * c - x2 * s, x1 * s + x2 * c], axis=-2)
    return out.reshape(x.shape)


def split_proj(p):
    idx = [int(v) for v in np.cumsum([Q_WIDTH, Q_WIDTH, ATTN_WIDTH, SSD_INNER, CONV_DIM, SSD_HEADS])]
    return jnp.split(p, idx, axis=-1)


def diff_attend(q, k_all, v_all, lam):
    s = jnp.einsum("bqhid,bkhid->bhiqk", q, k_all).astype(jnp.float32) * (HEAD_DIM ** -0.5)
    p = jax.nn.softmax(s, axis=-1)
    w = p[:, :, 0] - lam * p[:, :, 1]
    return jnp.einsum("bhqk,bkhe->bqhe", w.astype(v_all.dtype), v_all)


def centred_dwconv(x, w, bias):
    pad = D_CONV // 2
    l = x.shape[1]
    xp = jnp.pad(x, ((0, 0), (pad, pad), (0, 0)))
    out = bias
    for tap in range(D_CONV):
        out = out + xp[:, tap:tap + l] * w[tap]
    return out


def ssd_chunked(xs, dt, a, bm, cm, h0):
    f32 = jnp.float32
    b, l, g, r, p = xs.shape
    nst = bm.shape[-1]
    nc = l // CHUNK
    dtf = dt.astype(f32)
    xdt = (xs.astype(f32) * dtf[..., None]).reshape(b, nc, CHUNK, g, r, p)
    a_cs = jnp.cumsum((dtf * a).reshape(b, nc, CHUNK, g, r), axis=2)
    bc = bm.astype(f32).reshape(b, nc, CHUNK, g, nst)
    cc = cm.astype(f32).reshape(b, nc, CHUNK, g, nst)
    tri = jnp.tril(jnp.ones((CHUNK, CHUNK), bool))[:, :, None, None]
    seg = a_cs[:, :, :, None] - a_cs[:, :, None, :]
    decay = jnp.exp(jnp.where(tri, seg, -jnp.inf))
    cb = jnp.einsum("bclgn,bcsgn->bclsg", cc, bc)
    y_diag = jnp.einsum("bclsgr,bcsgrp->bclgrp", cb[..., None] * decay, xdt)
    to_end = jnp.exp(a_cs[:, :, -1:] - a_cs)
    states = jnp.einsum("bclgn,bclgrp->bcgrpn", bc, xdt * to_end[..., None])
    chunk_decay = jnp.exp(a_cs[:, :, -1])

    def step(h, inp):
        s, d = inp
        return h * d[..., None, None] + s, h

    h_final, h_in = lax.scan(step, h0.astype(f32),
                             (jnp.moveaxis(states, 1, 0), jnp.moveaxis(chunk_decay, 1, 0)))
    h_in = jnp.moveaxis(h_in, 0, 1)
    y_off = jnp.einsum("bclgn,bcgrpn->bclgrp", cc, h_in) * jnp.exp(a_cs)[..., None]
    y = (y_diag + y_off).reshape(b, l, g, r, p)
    return y.astype(xs.dtype), h_final


def ssd_bidirectional(xs, bm, cm, dt_f, dt_b, a_f, a_b, h0_f, h0_b):
    flip = lambda t: jnp.flip(t, axis=1)
    y_f, h_f = ssd_chunked(xs, dt_f, a_f, bm, cm, h0_f)
    y_b, h_b = ssd_chunked(flip(xs), flip(dt_b), a_b, flip(bm), flip(cm), h0_b)
    return y_f + flip(y_b), h_f, h_b


def ssd_inputs(xbc, dtf, dtb, conv_w, conv_b, dt_bias_f, dt_bias_b):
    b, l, _ = xbc.shape
    xbc = jax.nn.silu(centred_dwconv(xbc, conv_w, conv_b))
    xs, bm, cm = jnp.split(xbc, [SSD_INNER, SSD_INNER + SSD_GROUPS * D_STATE], axis=-1)
    xs = xs.reshape(b, l, SSD_GROUPS, SSD_HPG, SSD_HEADDIM)
    bm = bm.reshape(b, l, SSD_GROUPS, D_STATE)
    cm = cm.reshape(b, l, SSD_GROUPS, D_STATE)
    dt_f = jax.nn.softplus(dtf + dt_bias_f).reshape(b, l, SSD_GROUPS, SSD_HPG)
    dt_b = jax.nn.softplus(dtb + dt_bias_b).reshape(b, l, SSD_GROUPS, SSD_HPG)
    return xs, bm, cm, dt_f, dt_b


def gated_group_rmsnorm(y, z, w):
    b, l = z.shape[:2]
    g = (y.reshape(b, l, SSD_INNER) * jax.nn.silu(z)).reshape(b, l, SSD_GROUPS, SSD_INNER // SSD_GROUPS)
    gf = g.astype(jnp.float32)
    gn = gf * lax.rsqrt(jnp.mean(gf * gf, axis=-1, keepdims=True) + EPS)
    return (gn.reshape(b, l, SSD_INNER) * w.astype(jnp.float32)).astype(z.dtype)


def hybrid_mixer(h, hc, cos, sin, lam_init, with_ctx_out, w_in, q_norm_w, k_norm_w,
                 lq1, lk1, lq2, lk2, subln_w, conv_w, conv_b, dt_bias_f, dt_bias_b,
                 a_log_f, a_log_b, d_skip, ssd_norm_w, w_out):
    b, n, _ = h.shape
    m = hc.shape[1]
    q, k, v, z, xbc, dtf, dtb = split_proj(h @ w_in)
    qc, kc, vc, zc, xbcc, dtfc, dtbc = split_proj(hc @ w_in)

    heads = lambda t, w: rmsnorm(t.reshape(t.shape[0], t.shape[1], ATTN_HEADS, 2, HEAD_DIM), w)
    q = apply_rope(heads(q, q_norm_w), cos, sin)
    k = apply_rope(heads(k, k_norm_w), cos, sin)
    qc = heads(qc, q_norm_w)
    kc = heads(kc, k_norm_w)
    v = v.reshape(b, n, ATTN_HEADS, ATTN_VDIM)
    vc = vc.reshape(b, m, ATTN_HEADS, ATTN_VDIM)
    f32 = jnp.float32
    lam = (jnp.exp(jnp.sum(lq1.astype(f32) * lk1.astype(f32)))
           - jnp.exp(jnp.sum(lq2.astype(f32) * lk2.astype(f32))) + lam_init)
    k_all = jnp.concatenate([kc, k], axis=1)
    v_all = jnp.concatenate([vc, v], axis=1)
    nblk = n // Q_BLOCK
    qb = jnp.moveaxis(q.reshape(b, nblk, Q_BLOCK, ATTN_HEADS, 2, HEAD_DIM), 1, 0)
    ob = lax.map(lambda qq: diff_attend(qq, k_all, v_all, lam), qb)
    o = jnp.moveaxis(ob, 0, 1).reshape(b, n, ATTN_HEADS, ATTN_VDIM)
    attn = (rmsnorm(o, subln_w) * (1.0 - lam_init)).reshape(b, n, ATTN_WIDTH)

    a_f = -jnp.exp(a_log_f.astype(f32)).reshape(SSD_GROUPS, SSD_HPG)
    a_b = -jnp.exp(a_log_b.astype(f32)).reshape(SSD_GROUPS, SSD_HPG)
    d_res = d_skip.reshape(SSD_GROUPS, SSD_HPG, 1)
    xs_c, bm_c, cm_c, dtf_c, dtb_c = ssd_inputs(xbcc, dtfc, dtbc, conv_w, conv_b, dt_bias_f, dt_bias_b)
    h0 = jnp.zeros((b, SSD_GROUPS, SSD_HPG, SSD_HEADDIM, D_STATE), f32)
    y_c, hf_c, hb_c = ssd_bidirectional(xs_c, bm_c, cm_c, dtf_c, dtb_c, a_f, a_b, h0, h0)
    xs, bm, cm, dt_f, dt_b = ssd_inputs(xbc, dtf, dtb, conv_w, conv_b, dt_bias_f, dt_bias_b)
    y, _, _ = ssd_bidirectional(xs, bm, cm, dt_f, dt_b, a_f, a_b, hf_c, hb_c)
    ssd = gated_group_rmsnorm(y + d_res * xs, z, ssd_norm_w)

    mix_lat = jnp.concatenate([attn, ssd], axis=-1) @ w_out
    if not with_ctx_out:
        return mix_lat, None
    oc = diff_attend(qc, kc, vc, lam)
    attn_c = (rmsnorm(oc, subln_w) * (1.0 - lam_init)).reshape(b, m, ATTN_WIDTH)
    ssd_c = gated_group_rmsnorm(y_c + d_res * xs_c, zc, ssd_norm_w)
    mix_ctx = jnp.concatenate([attn_c, ssd_c], axis=-1) @ w_out
    return mix_lat, mix_ctx


def expert_choice_ffn(h, w_router, w_gate, w_up, w_down):
    b, n, d = h.shape
    cap = EC_CAPACITY * n // N_EXPERTS
    aff = jax.nn.softmax(jnp.einsum("bnd,de->ben", h, w_router).astype(jnp.float32), axis=1)
    gate, idx = lax.top_k(aff, cap)
    xe = jax.vmap(lambda hb, ib: hb[ib])(h, idx)
    hid = jax.nn.silu(jnp.einsum("becd,edf->becf", xe, w_gate)) * jnp.einsum("becd,edf->becf", xe, w_up)
    ye = jnp.einsum("becf,efd->becd", hid, w_down) * gate[..., None].astype(h.dtype)
    return jax.vmap(lambda ib, yb: jnp.zeros((n, d), yb.dtype).at[ib.reshape(-1)].add(yb.reshape(-1, d)))(idx, ye)


def setup_inputs(seed: int = 0) -> dict:
    key = jax.random.key(seed)
    ks = jax.random.split(key, 32)
    f32 = jnp.float32
    L = DEPTH
    nrm = lambda k, shape, scale: jax.random.normal(k, shape, f32) * scale
    gain = lambda k, shape: 1.0 + 0.02 * jax.random.normal(k, shape, f32)

    def dt_bias(k):
        dt0 = jnp.exp(jax.random.uniform(k, (L, SSD_HEADS), f32, math.log(1e-3), math.log(1e-1)))
        return dt0 + jnp.log(-jnp.expm1(-dt0))

    return {
        "x": nrm(ks[0], (BATCH, SEQ, D_MODEL), 1.0),
        "c": nrm(ks[1], (BATCH, D_MODEL), 1.0),
        "ctx": nrm(ks[2], (BATCH, CTX_LEN, D_MODEL), 1.0),
        "c_ctx": nrm(ks[3], (D_MODEL,), 1.0),
        "w_mod": nrm(ks[4], (L, D_MODEL, N_MOD * D_MODEL), 0.5 * D_MODEL ** -0.5),
        "b_mod": nrm(ks[5], (L, N_MOD * D_MODEL), 0.02),
        "norm1_w": gain(ks[6], (L, D_MODEL)),
        "norm2_w": gain(ks[7], (L, D_MODEL)),
        "w_in": nrm(ks[8], (L, D_MODEL, P_IN), D_MODEL ** -0.5),
        "q_norm_w": gain(ks[9], (L, HEAD_DIM)),
        "k_norm_w": gain(ks[10], (L, HEAD_DIM)),
        "lambda_q1": nrm(ks[11], (L, HEAD_DIM), 0.1),
        "lambda_k1": nrm(ks[12], (L, HEAD_DIM), 0.1),
        "lambda_q2": nrm(ks[13], (L, HEAD_DIM), 0.1),
        "lambda_k2": nrm(ks[14], (L, HEAD_DIM), 0.1),
        "subln_w": gain(ks[15], (L, ATTN_VDIM)),
        "conv_w": nrm(ks[16], (L, D_CONV, CONV_DIM), D_CONV ** -0.5),
        "conv_b": nrm(ks[17], (L, CONV_DIM), 0.02),
        "dt_bias_f": dt_bias(ks[18]),
        "dt_bias_b": dt_bias(ks[19]),
        "a_log_f": jnp.log(jax.random.uniform(ks[20], (L, SSD_HEADS), f32, 1.0, 16.0)),
        "a_log_b": jnp.log(jax.random.uniform(ks[21], (L, SSD_HEADS), f32, 1.0, 16.0)),
        "d_skip": gain(ks[22], (L, SSD_HEADS)),
        "ssd_norm_w": gain(ks[23], (L, SSD_INNER)),
        "w_out": nrm(ks[24], (L, MIX_WIDTH, D_MODEL), MIX_WIDTH ** -0.5),
        "w_router": nrm(ks[25], (L, D_MODEL, N_EXPERTS), D_MODEL ** -0.5),
        "w_gate": nrm(ks[26], (L, N_EXPERTS, D_MODEL, D_EXPERT), D_MODEL ** -0.5),
        "w_up": nrm(ks[27], (L, N_EXPERTS, D_MODEL, D_EXPERT), D_MODEL ** -0.5),
        "w_down": nrm(ks[28], (L, N_EXPERTS, D_EXPERT, D_MODEL), D_EXPERT ** -0.5),
    }


def reference(x, c, ctx, c_ctx, w_mod, b_mod, norm1_w, norm2_w, w_in, q_norm_w, k_norm_w,
              lambda_q1, lambda_k1, lambda_q2, lambda_k2, subln_w, conv_w, conv_b,
              dt_bias_f, dt_bias_b, a_log_f, a_log_b, d_skip, ssd_norm_w, w_out,
              w_router, w_gate, w_up, w_down):
    n = x.shape[1]
    cos, sin = axial_rope_tables(n)
    silu_c = jax.nn.silu(c)
    silu_cc = jax.nn.silu(c_ctx)
    x_lat, x_ctx = x, ctx
    for l in range(DEPTH):
        update_ctx = l < DEPTH - 1
        lam_init = 0.8 - 0.6 * math.exp(-0.3 * l)
        mod = silu_c @ w_mod[l] + b_mod[l]
        sh1, sc1, g1, sh2, sc2, g2 = jnp.split(mod[:, None, :], N_MOD, axis=-1)
        modc = silu_cc @ w_mod[l] + b_mod[l]
        sh1c, sc1c, g1c, sh2c, sc2c, g2c = jnp.split(modc, N_MOD, axis=-1)

        h = rmsnorm(x_lat, norm1_w[l]) * (1.0 + sc1) + sh1
        hc = rmsnorm(x_ctx, norm1_w[l]) * (1.0 + sc1c) + sh1c
        mix_lat, mix_ctx = hybrid_mixer(
            h, hc, cos, sin, lam_init, update_ctx, w_in[l], q_norm_w[l], k_norm_w[l],
            lambda_q1[l], lambda_k1[l], lambda_q2[l], lambda_k2[l], subln_w[l], conv_w[l], conv_b[l],
            dt_bias_f[l], dt_bias_b[l], a_log_f[l], a_log_b[l], d_skip[l], ssd_norm_w[l], w_out[l])
        x_lat = x_lat + g1 * mix_lat

        h2 = rmsnorm(x_lat, norm2_w[l]) * (1.0 + sc2) + sh2
        x_lat = x_lat + g2 * expert_choice_ffn(h2, w_router[l], w_gate[l], w_up[l], w_down[l])
        if update_ctx:
            x_ctx = x_ctx + g1c * mix_ctx
            hc2 = rmsnorm(x_ctx, norm2_w[l]) * (1.0 + sc2c) + sh2c
            x_ctx = x_ctx + g2c * expert_choice_ffn(hc2, w_router[l], w_gate[l], w_up[l], w_down[l])
    return x_lat
```

```python
import math
from contextlib import ExitStack, contextmanager
import numpy as np
import concourse.bass as bass
import concourse.mybir as mybir
from concourse.bass_utils import run_bass_kernel_spmd

F32 = mybir.dt.float32
BF16 = mybir.dt.bfloat16
U32 = mybir.dt.uint32
AF = mybir.ActivationFunctionType
ALU = mybir.AluOpType
AX = mybir.AxisListType

NRING = 8
D = 1024
SEQ = 2048
CTX = 256
T = SEQ + CTX
NT = T // 128
DEPTH = 4
P_IN = 3088
NE = 16
DE = 1408
NF = 11
CAP = 256
CAPC = 32
NSLOT = CAP + CAPC
EPS = 1e-6


class Buf:
    __slots__ = ("name", "w", "r")

    def __init__(self, name):
        self.name = name
        self.w = None
        self.r = []


class Tn:
    __slots__ = ("t", "_buf", "_subs", "name")

    def __init__(self, t, name):
        self.t = t
        self.name = name
        self._buf = Buf(name)
        self._subs = {}

    def __getitem__(self, k):
        return self.t[k]

    def s(self, i):
        b = self._subs.get(i)
        if b is None:
            b = self._subs[i] = Buf("%s.%s" % (self.name, i))
        return b


class Sched:
    ENGS = ("pe", "act", "dve", "pool", "sp")

    def __init__(self, nc, stack):
        self.nc = nc
        self.stacks = [stack]
        self.ops = {e: [] for e in self.ENGS}
        self.cnt = {e: 0 for e in self.ENGS}
        self.sem = {e: stack.enter_context(nc.semaphore("s_" + e)) for e in self.ENGS}
        self.dring = {e: [stack.enter_context(nc.semaphore("d_%s%d" % (e, i))) for i in range(NRING)]
                      for e in ("sp", "pool", "act")}
        self.dcnt = {e: 0 for e in ("sp", "pool", "act")}
        self.waited = {e: {} for e in self.ENGS}
        self.semobj = {}
        self.uid = 0
        self.retired = set()
        self.nep = 0

    def new_epoch(self):
        self.nep += 1
        for e in self.ENGS:
            self.retired.add(id(self.sem[e]))
            self.sem[e] = self.stacks[0].enter_context(self.nc.semaphore("s_%s_%d" % (e, self.nep)))
            self.cnt[e] = 0

    def sb(self, name, shape, dt):
        self.uid += 1
        nm = "%s_%d" % (name, self.uid)
        t = self.stacks[-1].enter_context(self.nc.sbuf_tensor(nm, list(shape), dt))
        return Tn(t, nm)

    def ps(self, name, shape, dt=F32):
        self.uid += 1
        nm = "%s_%d" % (name, self.uid)
        t = self.stacks[-1].enter_context(self.nc.psum_tensor(nm, list(shape), dt))
        return Tn(t, nm)

    def push(self):
        self.stacks.append(ExitStack())

    def pop(self):
        self.barrier()
        self.stacks.pop().close()

    @contextmanager
    def scope(self):
        st = ExitStack()
        self.stacks.append(st)
        try:
            yield
            self.barrier()
        finally:
            self.stacks.pop()
            st.close()

    def _need(self, eng, tok, waits):
        sid, val, teng, is_dma = tok
        if sid in self.retired:
            return
        w = self.waited[eng]
        if w.get(sid, 0) >= val:
            return
        w[sid] = val
        waits.append((sid, val))

    def _deps(self, eng, reads, writes, waits):
        for b in reads:
            if b.w is not None:
                self._need(eng, b.w, waits)
        for b in writes:
            tok = b.w
            if tok is not None and not (tok[2] == eng and not tok[3]):
                self._need(eng, tok, waits)
            for tok in b.r:
                if not (tok[2] == eng and not tok[3]):
                    self._need(eng, tok, waits)

    @staticmethod
    def _bufs(xs):
        out = []
        for x in xs:
            if x is None:
                continue
            out.append(x if isinstance(x, Buf) else x._buf)
        return out

    def _commit(self, tok, reads, writes):
        for b in reads:
            b.r.append(tok)
        for b in writes:
            b.w = tok
            b.r = []

    def op(self, eng, fn, reads=(), writes=()):
        reads = self._bufs(reads)
        writes = self._bufs(writes)
        waits = []
        self._deps(eng, reads, writes, waits)
        self.cnt[eng] += 1
        s = self.sem[eng]
        self.semobj[id(s)] = s
        tok = (id(s), self.cnt[eng], eng, False)
        self._commit(tok, reads, writes)
        self.ops[eng].append((waits, fn, s, 1))
        return tok

    def dma(self, q, fn, reads=(), writes=()):
        reads = self._bufs(reads)
        writes = self._bufs(writes)
        waits = []
        self._deps(q, reads, writes, waits)
        k = self.dcnt[q]
        self.dcnt[q] += 1
        s = self.dring[q][k % NRING]
        self.semobj[id(s)] = s
        target = 16 * (k // NRING + 1)
        if k >= NRING:
            self._need(q, (id(s), target - 16, q, True), waits)
        tok = (id(s), target, q, True)
        self._commit(tok, reads, writes)
        self.ops[q].append((waits, fn, s, 16))
        return tok

    def all_tokens(self):
        toks = []
        for e in self.ENGS:
            if self.cnt[e] > 0:
                s = self.sem[e]
                self.semobj[id(s)] = s
                toks.append((id(s), self.cnt[e], e, False))
        for q in self.dcnt:
            k = self.dcnt[q]
            for i in range(min(k, NRING)):
                uses = (k - i + NRING - 1) // NRING
                toks.append((id(self.dring[q][i]), 16 * uses, q, True))
        return toks

    def barrier(self):
        toks = self.all_tokens()
        for e in self.ENGS:
            waits = []
            for t in toks:
                if t[2] == e and not t[3]:
                    continue
                self._need(e, t, waits)
            if waits:
                self.ops[e].append((waits, None, None, 0))

    def emit(self):
        nc = self.nc
        with nc.Block() as block:
            def run(e):
                def body(engobj):
                    for waits, fn, s, inc in self.ops[e]:
                        for sid, val in waits:
                            engobj.wait_ge(self.semobj[sid], val)
                        if fn is not None:
                            fn(engobj).then_inc(s, inc)
                return body
            block.tensor(run("pe"))
            block.scalar(run("act"))
            block.vector(run("dve"))
            block.gpsimd(run("pool"))
            block.sync(run("sp"))


def host_consts():
    k = np.arange(128)
    c = {}
    c["ident"] = np.eye(128, dtype=np.float32)
    c["ones"] = np.ones((128, 128), np.float32)
    tri_f = (k[:, None] <= k[None, :]).astype(np.float32)
    tri_b = (k[:, None] >= k[None, :]).astype(np.float32)
    c["tri"] = np.stack([tri_f, tri_b])
    mf = np.where(k[None, :] >= k[:, None], 0.0, -1e5).astype(np.float32)
    mb = np.where(k[None, :] <= k[:, None], 0.0, -1e5).astype(np.float32)
    c["mneg"] = np.stack([np.tile(mf, (1, 4)), np.tile(mb, (1, 4))])
    n = SEQ
    row = np.repeat(np.arange(n // 64), 64)
    col = np.tile(np.arange(64), n // 64)
    inv = (10000.0 ** (-np.arange(16, dtype=np.float32) / 16)).astype(np.float32)
    ang = np.stack([row, col], -1).astype(np.float32)[..., None] * inv
    cs, sn = np.cos(ang).astype(np.float32), np.sin(ang).astype(np.float32)
    C = np.stack([cs, cs], 2)
    Sg = np.stack([-sn, sn], 2)
    c["ropeC"] = C.reshape(n, 64).astype(np.float32)
    c["ropeS"] = Sg.reshape(n, 64).astype(np.float32)
    c["iota"] = np.tile(np.arange(SEQ, dtype=np.float32)[None, :], (128, 1))
    c["tpos"] = (k[:, None] + 128 * np.arange(16)[None, :]).astype(np.float32)
    return c


CONST_SHAPES = {"ident": [128, 128], "ones": [128, 128], "tri": [2, 128, 128], "mneg": [2, 128, 512],
                "ropeC": [SEQ, 64], "ropeS": [SEQ, 64], "iota": [128, SEQ], "tpos": [128, 16]}

def param_shapes(depth=DEPTH, moe=True):
    ps = {
        "c_ctx": [D], "w_mod": [depth, D, 6 * D], "b_mod": [depth, 6 * D], "norm1_w": [depth, D], "norm2_w": [depth, D],
        "w_in": [depth, D, P_IN], "q_norm_w": [depth, 64], "k_norm_w": [depth, 64],
        "lambda_q1": [depth, 64], "lambda_k1": [depth, 64], "lambda_q2": [depth, 64], "lambda_k2": [depth, 64],
        "subln_w": [depth, 128], "conv_w": [depth, 3, D], "conv_b": [depth, D],
        "dt_bias_f": [depth, 8], "dt_bias_b": [depth, 8], "a_log_f": [depth, 8], "a_log_b": [depth, 8],
        "d_skip": [depth, 8], "ssd_norm_w": [depth, 512], "w_out": [depth, D, D], "w_router": [depth, D, NE],
    }
    if moe:
        ps.update({"w_gate": [depth, NE, D, DE], "w_up": [depth, NE, D, DE], "w_down": [depth, NE, DE, D]})
    return ps


PARAM_SHAPES = param_shapes()


def build(nb=2, depth=DEPTH, dbg=None, moe=True, force_last=False):
    nc = bass.Bass("TRN2", target_bir_lowering=False)
    din = {}
    din["x"] = nc.dram_tensor("x", [nb, SEQ, D], F32, kind="ExternalInput").ap()
    din["ctx"] = nc.dram_tensor("ctx", [nb, CTX, D], F32, kind="ExternalInput").ap()
    din["c"] = nc.dram_tensor("c", [nb, D], F32, kind="ExternalInput").ap()
    for k_, shp in param_shapes(depth, moe).items():
        din[k_] = nc.dram_tensor(k_, shp, F32, kind="ExternalInput").ap()
    for k_, shp in CONST_SHAPES.items():
        din[k_] = nc.dram_tensor("k_" + k_, shp, F32, kind="ExternalInput").ap()
    out = nc.dram_tensor("out", [nb, SEQ, D], F32, kind="ExternalOutput").ap()
    xres = nc.dram_tensor("xres", [T, D], F32).ap()
    modrow = nc.dram_tensor("modrow", [depth, nb + 1, 6 * D], F32).ap()
    affd = nc.dram_tensor("affd", [NE, T], F32).ap()
    dumps = {}

    with ExitStack() as st0:
        S = Sched(nc, st0)
        st0.enter_context(nc.allow_non_contiguous_dma(reason="small param layouts"))
        st0.enter_context(nc.allow_low_precision(reason="bf16 matmul operands by design"))

        def MM(oap, lap, rap, R, W, start=True, stop=True):
            S.op("pe", lambda e: e.matmul(oap, lhsT=lap, rhs=rap, start=start, stop=stop), R, W)

        def TR(oap, iap, idap, R, W):
            S.op("pe", lambda e: e.transpose(out=oap, in_=iap, identity=idap), R, W)

        def ACT(oap, iap, func, R, W, **kw):
            S.op("act", lambda e: e.activation(out=oap, in_=iap, func=func, **kw), R, W)

        def TT(eng, oap, a, b, op, R, W):
            S.op(eng, lambda e: e.tensor_tensor(out=oap, in0=a, in1=b, op=op), R, W)

        def TS(eng, oap, a, s1, s2, op0, op1, R, W):
            if op1 is None:
                S.op(eng, lambda e: e.tensor_scalar(out=oap, in0=a, scalar1=s1, scalar2=None, op0=op0), R, W)
            else:
                S.op(eng, lambda e: e.tensor_scalar(out=oap, in0=a, scalar1=s1, scalar2=s2, op0=op0, op1=op1), R, W)

        def STT(oap, a, s, b, op0, op1, R, W):
            S.op("dve", lambda e: e.scalar_tensor_tensor(out=oap, in0=a, scalar=s, in1=b, op0=op0, op1=op1), R, W)

        def CP(eng, oap, iap, R, W):
            if eng == "act":
                S.op("act", lambda e: e.copy(out=oap, in_=iap), R, W)
            else:
                S.op(eng, lambda e: e.tensor_copy(out=oap, in_=iap), R, W)

        def RED(oap, iap, R, W, op=ALU.add):
            S.op("dve", lambda e: e.tensor_reduce(out=oap, in_=iap, axis=AX.X, op=op), R, W)

        def RCP(oap, iap, R, W):
            S.op("dve", lambda e: e.reciprocal(out=oap, in_=iap), R, W)

        def MS(eng, ap, val, W):
            S.op(eng, lambda e: e.memset(ap, val), (), W)

        def DMA(q, oap, iap, R, W):
            return S.dma(q, lambda e: e.dma_start(out=oap, in_=iap), R, W)

        xres_b = [Buf("xres%d" % i) for i in range(NT)]
        modrow_b = Buf("modrow")
        affd_b = Buf("affd")
        out_toks = []

        def dump(name, tn_or_bufs, ap, shape, dt=F32):
            if dbg is None or name not in dbg or name in dumps:
                return
            d = nc.dram_tensor("dbg_" + name, list(shape), dt, kind="ExternalOutput").ap()
            dumps[name] = (list(shape), dt)
            R = tn_or_bufs if isinstance(tn_or_bufs, (list, tuple)) else [tn_or_bufs]
            out_toks.append(DMA("sp", d, ap, R, []))

        ident_f = S.sb("ident_f", [128, 128], F32)
        ident_b = S.sb("ident_b", [128, 128], BF16)
        ones_f = S.sb("ones_f", [128, 128], F32)
        tri = S.sb("tri", [128, 2, 128], F32)
        tpos = S.sb("tpos", [128, 16], F32)
        DMA("sp", ident_f[:], din["ident"], [], [ident_f])
        DMA("sp", ones_f[:], din["ones"], [], [ones_f])
        DMA("sp", tri[:], din["tri"].rearrange("a p l -> p a l"), [], [tri])
        DMA("sp", tpos[:], din["tpos"], [], [tpos])
        CP("dve", ident_b[:], ident_f[:], [ident_f], [ident_b])

        with S.scope():
            cT = S.sb("cT", [128, 8, nb + 1], F32)
            sT = S.sb("sT", [128, 8, nb + 1], BF16)
            sg = S.sb("sg", [128, 8, nb + 1], F32)
            for j in range(nb):
                DMA("sp", cT[:, :, j], din["c"][j].rearrange("(c p) -> p c", p=128), [], [cT])
            DMA("sp", cT[:, :, nb], din["c_ctx"].rearrange("(c p) -> p c", p=128), [], [cT])
            ACT(sg[:], cT[:], AF.Sigmoid, [cT], [sg])
            TT("dve", sT[:], cT[:], sg[:], ALU.mult, [cT, sg], [sT])
            wm = [S.sb("wm%d" % i, [128, 8, 1536], BF16) for i in range(2)]
            bm = S.sb("bm", [nb + 1, 6 * D], F32)
            mr = S.sb("mr", [nb + 1, 6 * D], F32)
            pm = [S.ps("pm%d" % i, [nb + 1, 512]) for i in range(2)]
            it = 0
            for l in range(depth):
                DMA("sp", bm[:], din["b_mod"][l:l + 1, :].to_broadcast([nb + 1, 6 * D]), [], [bm])
                for blk in range(4):
                    w_ = wm[it % 2]
                    it += 1
                    DMA("pool", w_[:], din["w_mod"][l, :, blk * 1536:(blk + 1) * 1536].rearrange("(c p) n -> p c n", p=128),
                        [], [w_])
                    for sub in range(3):
                        p_ = pm[sub % 2]
                        for c_ in range(8):
                            MM(p_[:], sT[:, c_, :], w_[:, c_, sub * 512:(sub + 1) * 512], [sT, w_], [p_],
                               start=(c_ == 0), stop=(c_ == 7))
                        col = blk * 1536 + sub * 512
                        TT("dve", mr[:, col:col + 512], p_[:], bm[:, col:col + 512], ALU.add, [p_, bm], [mr])
                DMA("sp", modrow[l], mr[:], [mr], [modrow_b])

        for b in range(nb):
            for l in range(depth):
                last = (l == DEPTH - 1) or force_last
                lam_init = 0.8 - 0.6 * math.exp(-0.3 * l)
                tiles = list(range(NT))

                def xsrc(tt):
                    if l == 0:
                        if tt < 2:
                            return din["ctx"][b, tt * 128:(tt + 1) * 128, :], []
                        return din["x"][b, (tt - 2) * 128:(tt - 1) * 128, :], []
                    return xres[tt * 128:(tt + 1) * 128, :], [xres_b[tt]]

                def load_mod(dst, which, eng="sp"):
                    DMA(eng, dst[:, 0, :], modrow[l, b:b + 1, which * D:(which + 1) * D].to_broadcast([128, D]), [modrow_b], [dst])
                    DMA(eng, dst[:, 1, :], modrow[l, nb:nb + 1, which * D:(which + 1) * D].to_broadcast([128, D]), [modrow_b], [dst])

                def do_norm1(hT):
                    with S.scope():
                        modA = S.sb("modA", [128, 2, D], F32)
                        modS = S.sb("modS", [128, 2, D], F32)
                        nw = S.sb("nw", [128, D], F32)
                        load_mod(modS, 0)
                        load_mod(modA, 1)
                        DMA("sp", nw[:], din["norm1_w"][l:l + 1, :].to_broadcast([128, D]), [], [nw])
                        for j in range(2):
                            STT(modA[:, j, :], modA[:, j, :], 1.0, nw[:], ALU.add, ALU.mult, [modA, nw], [modA])
                        xin = [S.sb("xin%d" % i, [128, D], F32) for i in range(2)]
                        junk = S.sb("junk", [128, D], BF16)
                        t1 = [S.sb("t1_%d" % i, [128, D], F32) for i in range(2)]
                        hb = [S.sb("hb%d" % i, [128, D], BF16) for i in range(2)]
                        st = [S.sb("st%d" % i, [128, 4], F32) for i in range(2)]
                        pT8 = [S.ps("pT8_%d" % i, [128, 8, 128], BF16) for i in range(2)]
                        for tt in tiles:
                            k2 = tt % 2
                            j = 1 if tt < 2 else 0
                            src, sb_ = xsrc(tt)
                            DMA("sp", xin[k2][:], src, sb_, [xin[k2]])
                            ACT(junk[:], xin[k2][:], AF.Square, [xin[k2]], [junk, st[k2]], accum_out=st[k2][:, 0:1])
                            ACT(st[k2][:, 1:2], st[k2][:, 0:1], AF.Sqrt, [st[k2]], [st[k2]], scale=1.0 / D, bias=EPS)
                            RCP(st[k2][:, 2:3], st[k2][:, 1:2], [st[k2]], [st[k2]])
                            STT(t1[k2][:], xin[k2][:], st[k2][:, 2:3], modA[:, j, :], ALU.mult, ALU.mult,
                                [xin[k2], st[k2], modA], [t1[k2]])
                            TT("pool", hb[k2][:], t1[k2][:], modS[:, j, :], ALU.add, [t1[k2], modS], [hb[k2]])
                            for c_ in range(8):
                                TR(pT8[k2][:, c_, :], hb[k2][:, c_ * 128:(c_ + 1) * 128], ident_b[:], [hb[k2], ident_b], [pT8[k2]])
                            CP("act", hT[:, :, tt * 128:(tt + 1) * 128], pT8[k2][:], [pT8[k2]], [hT.s(tt)])
                        if l == 0 and b == 0:
                            dump("hT", [hT.s(t_) for t_ in tiles], hT[:], [128, 8, T], BF16)


                S.push()
                tokbuf = S.sb("tokbuf", [128, NT, 1024], BF16)
                mixtok = tokbuf
                with S.scope():
                    hT = S.sb("hT", [128, 8, T], BF16)
                    do_norm1(hT)
                    with S.scope():
                        qkT = S.sb("qkT", [128, 8, T], BF16)
                        v_aug = S.sb("v_aug", [128, NT, 4, 132], BF16)
                        MS("pool", v_aug[:], 1.0, [v_aug])
                        nlam = S.sb("nlam", [128, 4], F32)
                        subw = S.sb("subw", [128, 128], F32)
                        with S.scope():
                            wqkv = S.sb("wqkv", [128, 8, 1536], BF16)
                            ropeC = S.sb("ropeC", [128, 16, 64], F32)
                            ropeS = S.sb("ropeS", [128, 16, 64], F32)
                            DMA("sp", ropeC[:], din["ropeC"].rearrange("(j p) d -> p j d", p=128), [], [ropeC])
                            DMA("sp", ropeS[:], din["ropeS"].rearrange("(j p) d -> p j d", p=128), [], [ropeS])
                            DMA("pool", wqkv[:], din["w_in"][l, :, 0:1536].rearrange("(c p) n -> p c n", p=128), [], [wqkv])
                            qkW = S.sb("qkW", [128, 16, 64], F32)
                            for g in range(8):
                                DMA("sp", qkW[:, g, :], din["q_norm_w"][l:l + 1, :].to_broadcast([128, 64]), [], [qkW])
                                DMA("sp", qkW[:, 8 + g, :], din["k_norm_w"][l:l + 1, :].to_broadcast([128, 64]), [], [qkW])
                            lp = S.sb("lp", [128, 4, 64], F32)
                            for i_, nm in enumerate(["lambda_q1", "lambda_k1", "lambda_q2", "lambda_k2"]):
                                DMA("sp", lp[:, i_, :], din[nm][l:l + 1, :].to_broadcast([128, 64]), [], [lp])
                            lpr = S.sb("lpr", [128, 2, 64], F32)
                            TT("dve", lpr[:, 0, :], lp[:, 0, :], lp[:, 1, :], ALU.mult, [lp], [lpr])
                            TT("dve", lpr[:, 1, :], lp[:, 2, :], lp[:, 3, :], ALU.mult, [lp], [lpr])
                            RED(nlam[:, 0:2], lpr[:], [lpr], [nlam])
                            ACT(nlam[:, 0:2], nlam[:, 0:2], AF.Exp, [nlam], [nlam])
                            TT("dve", nlam[:, 2:3], nlam[:, 1:2], nlam[:, 0:1], ALU.subtract, [nlam], [nlam])
                            TS("dve", nlam[:, 3:4], nlam[:, 2:3], -lam_init, None, ALU.add, None, [nlam], [nlam])
                            DMA("sp", subw[:], din["subln_w"][l:l + 1, :].to_broadcast([128, 128]), [], [subw])
                            TS("dve", subw[:], subw[:], 1.0 - lam_init, None, ALU.mult, None, [subw], [subw])

                            ps_qkv = S.ps("ps_qkv", [128, 1536])
                            pT8 = [S.ps("pT8q_%d" % i, [128, 8, 128], BF16) for i in range(2)]
                            sqb = S.sb("sqb", [128, 16, 64], F32)
                            s16 = S.sb("s16", [128, 3, 16], F32)
                            qn = S.sb("qn", [128, 16, 64], F32)
                            qn2 = S.sb("qn2", [128, 16, 64], F32)
                            ta = S.sb("ta", [128, 16, 64], F32)
                            tb = S.sb("tb", [128, 16, 64], F32)
                            qr = [S.sb("qr%d" % i, [128, 16 * 64], BF16) for i in range(2)]
                            for tt in tiles:
                                k2 = tt % 2
                                for j in range(3):
                                    for c_ in range(8):
                                        MM(ps_qkv[:, j * 512:(j + 1) * 512], hT[:, c_, tt * 128:(tt + 1) * 128],
                                           wqkv[:, c_, j * 512:(j + 1) * 512], [hT.s(tt), wqkv], [ps_qkv],
                                           start=(c_ == 0), stop=(c_ == 7))
                                CP("act", v_aug[:, tt, :, 0:128], ps_qkv[:, 1024:1536].rearrange("p (h e) -> p h e", h=4),
                                   [ps_qkv], [v_aug.s(tt)])
                                qk3 = ps_qkv[:, 0:1024].rearrange("p (g e) -> p g e", g=16)
                                ACT(sqb[:], qk3, AF.Square, [ps_qkv], [sqb])
                                RED(s16[:, 0, :], sqb[:], [sqb], [s16])
                                ACT(s16[:, 1, :], s16[:, 0, :], AF.Sqrt, [s16], [s16], scale=1.0 / 64, bias=EPS)
                                RCP(s16[:, 2, :], s16[:, 1, :], [s16], [s16])
                                TT("dve", qn[:], qk3, s16[:, 2, :].unsqueeze(2).to_broadcast([128, 16, 64]), ALU.mult,
                                   [ps_qkv, s16], [qn])
                                if tt < 2:
                                    TT("pool", qr[k2][:].rearrange("p (g e) -> p g e", g=16), qn[:], qkW[:], ALU.mult,
                                       [qn, qkW], [qr[k2]])
                                else:
                                    jj = tt - 2
                                    TT("pool", qn2[:], qn[:], qkW[:], ALU.mult, [qn, qkW], [qn2])
                                    TT("pool", ta[:], qn2[:], ropeC[:, jj, :].unsqueeze(1).to_broadcast([128, 16, 64]), ALU.mult,
                                       [qn2, ropeC], [ta])
                                    q5 = qn2[:].rearrange("p g (a h f) -> p g a h f", a=2, h=2)
                                    t5 = tb[:].rearrange("p g (a h f) -> p g a h f", a=2, h=2)
                                    s5 = ropeS[:, jj, :].rearrange("p (a h f) -> p a h f", a=2, h=2)
                                    for hh in range(2):
                                        TT("dve", t5[:, :, :, hh, :], q5[:, :, :, 1 - hh, :],
                                           s5[:, :, hh, :].unsqueeze(1).to_broadcast([128, 16, 2, 16]), ALU.mult,
                                           [qn2, ropeS], [tb])
                                    TT("dve", qr[k2][:].rearrange("p (g e) -> p g e", g=16), ta[:], tb[:], ALU.add,
                                       [ta, tb], [qr[k2]])
                                for m in range(8):
                                    TR(pT8[k2][:, m, :], qr[k2][:, m * 128:(m + 1) * 128], ident_b[:], [qr[k2], ident_b], [pT8[k2]])
                                CP("act", qkT[:, :, tt * 128:(tt + 1) * 128], pT8[k2][:], [pT8[k2]], [qkT.s(tt)])
                            if l == 0 and b == 0:
                                dump("qkT", [qkT.s(t_) for t_ in tiles], qkT[:], [128, 8, T], BF16)
                                dump("v_aug", [v_aug.s(t_) for t_ in tiles] + [v_aug], v_aug[:], [128, NT, 4, 132], BF16)

                        with S.scope():
                            pTb = [S.sb("pTb%d" % i, [128, NT, 512], BF16) for i in range(2)]
                            ps_s = [S.ps("ps_s%d" % i, [128, 512]) for i in range(3)]
                            ps_o = [S.ps("ps_o%d" % i, [128, 512]) for i in range(2)]
                            ob = [S.sb("ob%d" % i, [128, 2, 128], F32) for i in range(2)]
                            sc_ = [S.sb("sc%d" % i, [128, 8], F32) for i in range(2)]
                            junk2 = S.sb("junk2", [128, 128], BF16)
                            vall = [v_aug] + [v_aug.s(t_) for t_ in tiles]
                            blocks = []
                            if not last:
                                blocks.append((0, 256, [0, 1]))
                            for bq in range(4):
                                blocks.append((256 + bq * 512, 512, list(range(NT))))
                            nsc = 0
                            ncomb = 0
                            for hd in range(4):
                                for (q0, qn_, kcs) in blocks:
                                    for i in range(2):
                                        for kc in kcs:
                                            p_ = ps_s[nsc % 3]
                                            nsc += 1
                                            MM(p_[:, 0:qn_], qkT[i * 64:(i + 1) * 64, 4 + hd, kc * 128:(kc + 1) * 128],
                                               qkT[i * 64:(i + 1) * 64, hd, q0:q0 + qn_],
                                               [qkT.s(kc)] + [qkT.s(q0 // 128 + u) for u in range(qn_ // 128)], [p_])
                                            ACT(pTb[i][:, kc, 0:qn_], p_[:, 0:qn_], AF.Exp, [p_], [pTb[i].s(kc)], scale=0.125)
                                    for tq in range(qn_ // 128):
                                        tt = q0 // 128 + tq
                                        k2 = ncomb % 2
                                        ncomb += 1
                                        for i in range(2):
                                            for n_, kc in enumerate(kcs):
                                                MM(ps_o[i][:, 0:129], pTb[i][:, kc, tq * 128:(tq + 1) * 128], v_aug[:, kc, hd, 0:129],
                                                   [pTb[i].s(kc)] + vall, [ps_o[i]], start=(n_ == 0), stop=(n_ == len(kcs) - 1))
                                        s_ = sc_[k2]
                                        o_ = ob[k2]
                                        RCP(s_[:, 0:1], ps_o[0][:, 128:129], [ps_o[0]], [s_])
                                        RCP(s_[:, 1:2], ps_o[1][:, 128:129], [ps_o[1]], [s_])
                                        TT("dve", s_[:, 2:3], s_[:, 1:2], nlam[:, 3:4], ALU.mult, [s_, nlam], [s_])
                                        TS("dve", o_[:, 0, :], ps_o[0][:, 0:128], s_[:, 0:1], None, ALU.mult, None, [ps_o[0], s_], [o_])
                                        STT(o_[:, 1, :], ps_o[1][:, 0:128], s_[:, 2:3], o_[:, 0, :], ALU.mult, ALU.add,
                                            [ps_o[1], s_, o_], [o_])
                                        ACT(junk2[:], o_[:, 1, :], AF.Square, [o_], [junk2, s_], accum_out=s_[:, 3:4])
                                        ACT(s_[:, 4:5], s_[:, 3:4], AF.Sqrt, [s_], [s_], scale=1.0 / 128, bias=EPS)
                                        RCP(s_[:, 5:6], s_[:, 4:5], [s_], [s_])
                                        STT(mixtok[:, tt, hd * 128:(hd + 1) * 128], o_[:, 1, :], s_[:, 5:6], subw[:], ALU.mult, ALU.mult,
                                            [o_, s_, subw], [mixtok.s(tt)])
                            if l == 0 and b == 0:
                                dump("mixattn", [mixtok.s(t_) for t_ in tiles], mixtok[:, :, 0:512], [128, NT, 512], BF16)

                with S.scope():
                    BCT = S.sb("BCT", [128, 4, T], BF16)
                    xsB = S.sb("xsB", [128, NT, 768], BF16)
                    sz = S.sb("sz", [128, NT, 512], BF16)
                    dtt = S.sb("dtt", [128, NT, 16], F32)
                    dA = S.sb("dA", [128, NT, 16], F32)
                    dsk = S.sb("dsk", [128, 8], F32)
                    ssdw = S.sb("ssdw", [128, 512], F32)
                    DMA("sp", dsk[:], din["d_skip"][l:l + 1, :].to_broadcast([128, 8]), [], [dsk])
                    DMA("sp", ssdw[:], din["ssd_norm_w"][l:l + 1, :].to_broadcast([128, 512]), [], [ssdw])
                    with S.scope():
                        hT = S.sb("hT2", [128, 8, T], BF16)
                        do_norm1(hT)
                        hall = [hT.s(t_) for t_ in tiles]
                        with S.scope():
                            wz = S.sb("wz", [128, 8, 528], BF16)
                            DMA("pool", wz[:, :, 0:512], din["w_in"][l, :, 1536:2048].rearrange("(c p) n -> p c n", p=128), [], [wz])
                            DMA("pool", wz[:, :, 512:528], din["w_in"][l, :, 3072:3088].rearrange("(c p) n -> p c n", p=128), [], [wz])
                            dtb = S.sb("dtb", [128, 16], F32)
                            abc = S.sb("abc", [128, 16], F32)
                            DMA("sp", dtb[:, 0:8], din["dt_bias_f"][l:l + 1, :].to_broadcast([128, 8]), [], [dtb])
                            DMA("sp", dtb[:, 8:16], din["dt_bias_b"][l:l + 1, :].to_broadcast([128, 8]), [], [dtb])
                            DMA("sp", abc[:, 0:8], din["a_log_f"][l:l + 1, :].to_broadcast([128, 8]), [], [abc])
                            DMA("sp", abc[:, 8:16], din["a_log_b"][l:l + 1, :].to_broadcast([128, 8]), [], [abc])
                            ACT(abc[:], abc[:], AF.Exp, [abc], [abc])
                            TS("dve", abc[:], abc[:], -1.0, None, ALU.mult, None, [abc], [abc])
                            ps_z = [S.ps("ps_z%d" % i, [128, 512]) for i in range(2)]
                            ps_dt = [S.ps("ps_dt%d" % i, [128, 16]) for i in range(2)]
                            for tt in tiles:
                                k2 = tt % 2
                                for c_ in range(8):
                                    MM(ps_z[k2][:], hT[:, c_, tt * 128:(tt + 1) * 128], wz[:, c_, 0:512], [hT.s(tt), wz], [ps_z[k2]],
                                       start=(c_ == 0), stop=(c_ == 7))
                                for c_ in range(8):
                                    MM(ps_dt[k2][:], hT[:, c_, tt * 128:(tt + 1) * 128], wz[:, c_, 512:528], [hT.s(tt), wz], [ps_dt[k2]],
                                       start=(c_ == 0), stop=(c_ == 7))
                                ACT(sz[:, tt, :], ps_z[k2][:], AF.Silu, [ps_z[k2]], [sz.s(tt)])
                                TT("dve", dtt[:, tt, :], ps_dt[k2][:], dtb[:], ALU.add, [ps_dt[k2], dtb], [dtt])
                            ACT(dtt[:], dtt[:], AF.Exp, [dtt], [dtt])
                            ACT(dtt[:], dtt[:], AF.Ln, [dtt], [dtt], bias=1.0)
                            TT("dve", dA[:], dtt[:], abc[:].unsqueeze(1).to_broadcast([128, NT, 16]), ALU.mult, [dtt, abc], [dA])
                        with S.scope():
                            wx = S.sb("wx", [128, 8, 1024], BF16)
                            DMA("pool", wx[:], din["w_in"][l, :, 2048:3072].rearrange("(c p) n -> p c n", p=128), [], [wx])
                            cw = S.sb("cw", [128, 8, 3], F32)
                            cbv = S.sb("cbv", [128, 8], F32)
                            for k_ in range(3):
                                DMA("sp", cw[:, :, k_], din["conv_w"][l, k_].rearrange("(c p) -> p c", p=128), [], [cw])
                            DMA("sp", cbv[:], din["conv_b"][l].rearrange("(c p) -> p c", p=128), [], [cbv])
                            raw = [S.sb("raw%d" % i, [128, T + 4], F32) for i in range(1)]
                            acc = S.sb("acc", [128, T], F32)
                            fmb = S.sb("fmb", [128, T], BF16)
                            ps_x = [S.ps("ps_x%d" % i, [128, 512]) for i in range(2)]
                            pT4 = [S.ps("pT4_%d" % i, [128, 4, 128], BF16) for i in range(2)]
                            MS("pool", raw[0][:], 0.0, [raw[0]])
                            npx = 0
                            ntr = 0
                            segs = [(0, 256, 1), (256, 512, 259), (768, 512, 259 + 512), (1280, 512, 259 + 1024), (1792, 512, 259 + 1536)]
                            for ch in range(8):
                                r_ = raw[0]
                                for (c0, cn, ro) in segs:
                                    p_ = ps_x[npx % 2]
                                    npx += 1
                                    for c_ in range(8):
                                        MM(p_[:, 0:cn], wx[:, c_, ch * 128:(ch + 1) * 128], hT[:, c_, c0:c0 + cn],
                                           [wx] + [hT.s(c0 // 128 + u) for u in range(cn // 128)], [p_], start=(c_ == 0), stop=(c_ == 7))
                                    CP("act", r_[:, ro:ro + cn], p_[:, 0:cn], [p_], [r_])
                                for (a0, an, ro) in [(0, 256, 1), (256, 2048, 259)]:
                                    TS("pool", acc[:, a0:a0 + an], r_[:, ro:ro + an], cw[:, ch, 1:2], cbv[:, ch:ch + 1], ALU.mult, ALU.add,
                                       [r_, cw, cbv], [acc])
                                    STT(acc[:, a0:a0 + an], r_[:, ro - 1:ro - 1 + an], cw[:, ch, 0:1], acc[:, a0:a0 + an], ALU.mult, ALU.add,
                                        [r_, cw, acc], [acc])
                                    STT(acc[:, a0:a0 + an], r_[:, ro + 1:ro + 1 + an], cw[:, ch, 2:3], acc[:, a0:a0 + an], ALU.mult, ALU.add,
                                        [r_, cw, acc], [acc])
                                if ch >= 4:
                                    ACT(BCT[:, ch - 4, :], acc[:], AF.Silu, [acc], [BCT.s(ch - 4)])
                                    src_t, src_b = BCT, BCT.s(ch - 4)
                                    src_ap = lambda c0_, cn_: BCT[:, ch - 4, c0_:c0_ + cn_]
                                else:
                                    ACT(fmb[:], acc[:], AF.Silu, [acc], [fmb])
                                    src_b = fmb._buf
                                    src_ap = lambda c0_, cn_: fmb[:, c0_:c0_ + cn_]
                                if ch < 6:
                                    for t4 in range(0, NT, 4):
                                        n4 = min(4, NT - t4)
                                        p4 = pT4[ntr % 2]
                                        ntr += 1
                                        for u in range(n4):
                                            TR(p4[:, u, :], src_ap((t4 + u) * 128, 128), ident_b[:], [src_b, ident_b], [p4])
                                        CP("dve", xsB[:, t4:t4 + n4, ch * 128:(ch + 1) * 128], p4[:, 0:n4, :], [p4], [xsB])
                            if l == 0 and b == 0:
                                dump("BCT", [BCT.s(i) for i in range(4)], BCT[:], [128, 4, T], BF16)
                                dump("xsB", [xsB], xsB[:], [128, NT, 768], BF16)
                                dump("dtt", [dtt], dtt[:], [128, NT, 16], F32)

                    with S.scope():
                        yss = S.sb("yss", [128, NT, 512], F32)
                        mneg = S.sb("mneg", [128, 2, 512], F32)
                        DMA("sp", mneg[:], din["mneg"].rearrange("a p l -> p a l"), [], [mneg])
                        H = S.sb("H", [128, 8, 64], F32)
                        Hb = S.sb("Hb", [128, 8, 64], BF16)
                        dAtri = S.sb("dAtri", [128, 8, 128], F32)
                        negdA = S.sb("negdA", [128, 8, 128], F32)
                        dec = S.sb("dec", [128, 8, 128], F32)
                        Mt = S.sb("Mt", [128, 8, 128], BF16)
                        sm = S.sb("sm", [128, 48], F32)
                        xdt = S.sb("xdt", [128, 8, 64], BF16)
                        xdte = S.sb("xdte", [128, 8, 64], BF16)
                        tmpy = S.sb("tmpy", [128, 8, 64], F32)
                        tmpy2 = S.sb("tmpy2", [128, 8, 64], F32)
                        tmpH = S.sb("tmpH", [128, 8, 64], F32)
                        ps_cb = S.ps("ps_cb", [128, 512])
                        ps_seg = S.ps("ps_seg", [128, 1024])
                        ps_a = S.ps("ps_a", [128, 16])
                        ps_y = S.ps("ps_y", [128, 512])
                        ps_yo = S.ps("ps_yo", [128, 512])
                        ps_st = S.ps("ps_st", [128, 512])
                        for d in range(2):
                            order = list(range(NT)) if d == 0 else [1, 0] + list(range(NT - 1, 1, -1))
                            MS("dve", H[:], 0.0, [H])
                            MS("pool", Hb[:], 0.0, [Hb])
                            for tt in order:
                                cs = slice(tt * 128, (tt + 1) * 128)
                                dAd = dA[:, tt, d * 8:(d + 1) * 8]
                                dtd = dtt[:, tt, d * 8:(d + 1) * 8]
                                for g in range(2):
                                    MM(ps_cb[:, g * 128:(g + 1) * 128], BCT[:, g, cs], BCT[:, 2 + g, cs], [BCT.s(g), BCT.s(2 + g)], [ps_cb])
                                TT("pool", dAtri[:], tri[:, d, :].unsqueeze(1).to_broadcast([128, 8, 128]),
                                   dAd.unsqueeze(2).to_broadcast([128, 8, 128]), ALU.mult, [tri, dA], [dAtri])
                                TS("pool", negdA[:], dAd.unsqueeze(2).to_broadcast([128, 8, 128]), -1.0, None, ALU.mult, None, [dA], [negdA])
                                for hh in range(2):
                                    po = ps_seg[:, hh * 512:(hh + 1) * 512]
                                    MM(po, ones_f[:], dAtri[:, hh * 4:(hh + 1) * 4, :].rearrange("p a b -> p (a b)"), [ones_f, dAtri], [ps_seg],
                                       start=True, stop=False)
                                    MM(po, tri[:, d, :], negdA[:, hh * 4:(hh + 1) * 4, :].rearrange("p a b -> p (a b)"), [tri, negdA], [ps_seg],
                                       start=False, stop=False)
                                    MM(po, ident_f[:], mneg[:, d, :], [ident_f, mneg], [ps_seg], start=False, stop=True)
                                ACT(dec[:].rearrange("p a b -> p (a b)"), ps_seg[:], AF.Exp, [ps_seg], [dec])
                                TT("dve", Mt[:].rearrange("p (g r) l -> p g r l", g=2), dec[:].rearrange("p (g r) l -> p g r l", g=2),
                                   ps_cb[:, 0:256].rearrange("p (g l) -> p g l", g=2).unsqueeze(2).to_broadcast([128, 2, 4, 128]), ALU.mult,
                                   [dec, ps_cb], [Mt])
                                MM(ps_a[:, 0:8], tri[:, d, :], dAd, [tri, dA], [ps_a])
                                MM(ps_a[:, 8:16], ones_f[:], dAd, [ones_f, dA], [ps_a])
                                ACT(sm[:, 0:16], ps_a[:, 0:16], AF.Exp, [ps_a], [sm])
                                CP("act", sm[:, 32:48], ps_a[:, 0:16], [ps_a], [sm])
                                TT("dve", sm[:, 16:24], sm[:, 40:48], sm[:, 32:40], ALU.subtract, [sm], [sm])
                                ACT(sm[:, 16:24], sm[:, 16:24], AF.Exp, [sm], [sm])
                                TT("dve", sm[:, 24:32], sm[:, 16:24], dtd, ALU.mult, [sm, dtt], [sm])
                                xs3 = xsB[:, tt, 0:512].rearrange("p (h e) -> p h e", h=8)
                                TT("pool", xdt[:], xs3, dtd.unsqueeze(2).to_broadcast([128, 8, 64]), ALU.mult, [xsB, dtt], [xdt])
                                TT("pool", xdte[:], xs3, sm[:, 24:32].unsqueeze(2).to_broadcast([128, 8, 64]), ALU.mult, [xsB, sm], [xdte])
                                for h_ in range(8):
                                    MM(ps_y[:, h_ * 64:(h_ + 1) * 64], Mt[:, h_, :], xdt[:, h_, :], [Mt, xdt], [ps_y])
                                for g in range(2):
                                    MM(ps_yo[:, g * 256:(g + 1) * 256], BCT[:, 2 + g, cs], Hb[:, g * 4:(g + 1) * 4, :].rearrange("p a b -> p (a b)"),
                                       [BCT.s(2 + g), Hb], [ps_yo])
                                for g in range(2):
                                    MM(ps_st[:, g * 256:(g + 1) * 256], xsB[:, tt, 512 + g * 128:512 + (g + 1) * 128],
                                       xdte[:, g * 4:(g + 1) * 4, :].rearrange("p a b -> p (a b)"), [xsB, xdte], [ps_st])
                                TT("dve", tmpy[:], ps_yo[:].rearrange("p (h e) -> p h e", h=8), sm[:, 0:8].unsqueeze(2).to_broadcast([128, 8, 64]),
                                   ALU.mult, [ps_yo, sm], [tmpy])
                                if d == 0:
                                    TT("dve", yss[:, tt, :], ps_y[:], tmpy[:].rearrange("p a b -> p (a b)"), ALU.add, [ps_y, tmpy], [yss.s(tt)])
                                else:
                                    TT("dve", tmpy2[:].rearrange("p a b -> p (a b)"), ps_y[:], tmpy[:].rearrange("p a b -> p (a b)"), ALU.add,
                                       [ps_y, tmpy], [tmpy2])
                                    TT("pool", yss[:, tt, :], yss[:, tt, :], tmpy2[:].rearrange("p a b -> p (a b)"), ALU.add,
                                       [yss.s(tt), tmpy2], [yss.s(tt)])
                                TT("dve", tmpH[:], H[:], sm[:, 8:16].unsqueeze(2).to_broadcast([128, 8, 64]), ALU.mult, [H, sm], [tmpH])
                                TT("dve", H[:].rearrange("p a b -> p (a b)"), tmpH[:].rearrange("p a b -> p (a b)"), ps_st[:], ALU.add,
                                   [tmpH, ps_st], [H])
                                CP("act", Hb[:], H[:], [H], [Hb])
                        gg = S.sb("gg", [128, 512], F32)
                        junk3 = S.sb("junk3", [128, 256], BF16)
                        gs = S.sb("gs", [128, 8], F32)
                        for tt in tiles:
                            xs3 = xsB[:, tt, 0:512].rearrange("p (h e) -> p h e", h=8)
                            TT("pool", tmpy[:], xs3, dsk[:].unsqueeze(2).to_broadcast([128, 8, 64]), ALU.mult, [xsB, dsk], [tmpy])
                            TT("dve", tmpy2[:].rearrange("p a b -> p (a b)"), yss[:, tt, :], tmpy[:].rearrange("p a b -> p (a b)"), ALU.add,
                               [yss.s(tt), tmpy], [tmpy2])
                            TT("dve", gg[:], tmpy2[:].rearrange("p a b -> p (a b)"), sz[:, tt, :], ALU.mult, [tmpy2, sz.s(tt)], [gg])
                            for g in range(2):
                                ACT(junk3[:], gg[:, g * 256:(g + 1) * 256], AF.Square, [gg], [junk3, gs], accum_out=gs[:, g:g + 1])
                            ACT(gs[:, 2:4], gs[:, 0:2], AF.Sqrt, [gs], [gs], scale=1.0 / 256, bias=EPS)
                            RCP(gs[:, 4:6], gs[:, 2:4], [gs], [gs])
                            for g in range(2):
                                STT(mixtok[:, tt, 512 + g * 256:512 + (g + 1) * 256], gg[:, g * 256:(g + 1) * 256], gs[:, 4 + g:5 + g],
                                    ssdw[:, g * 256:(g + 1) * 256], ALU.mult, ALU.mult, [gg, gs, ssdw], [mixtok.s(tt)])
                        if l == 0 and b == 0:
                            dump("mixssd", [mixtok.s(t_) for t_ in tiles], mixtok[:, :, 512:1024], [128, NT, 512], BF16)
                            dump("yss", [yss.s(t_) for t_ in tiles], yss[:], [128, NT, 512], F32)

                with S.scope():
                    wo = S.sb("wo", [128, 8, D], BF16)
                    DMA("pool", wo[:], din["w_out"][l].rearrange("(c p) n -> p c n", p=128), [], [wo])
                    wr = S.sb("wr", [128, 8, NE], F32)
                    DMA("sp", wr[:], din["w_router"][l].rearrange("(c p) n -> p c n", p=128), [], [wr])
                    modG = S.sb("modG", [128, 2, D], F32)
                    modA = S.sb("modA2", [128, 2, D], F32)
                    modS = S.sb("modS2", [128, 2, D], F32)
                    nw = S.sb("nw2", [128, D], F32)
                    load_mod(modG, 2)
                    load_mod(modS, 3)
                    load_mod(modA, 4)
                    DMA("sp", nw[:], din["norm2_w"][l:l + 1, :].to_broadcast([128, D]), [], [nw])
                    for j in range(2):
                        STT(modA[:, j, :], modA[:, j, :], 1.0, nw[:], ALU.add, ALU.mult, [modA, nw], [modA])
                    mixT = [S.sb("mixT%d" % i, [128, 8, 128], BF16) for i in range(2)]
                    xin = [S.sb("xin2_%d" % i, [128, D], F32) for i in range(2)]
                    xnew = [S.sb("xnew%d" % i, [128, D], F32) for i in range(2)]
                    tg = S.sb("tg", [128, D], F32)
                    t1 = S.sb("t1b", [128, D], F32)
                    h2f = S.sb("h2f", [128, D], F32)
                    h2fT = S.sb("h2fT", [128, 8, 128], F32)
                    junk = S.sb("junk4", [128, D], BF16)
                    st = [S.sb("st2_%d" % i, [128, 4], F32) for i in range(2)]
                    rt = S.sb("rt", [128, 40], F32)
                    att = [S.sb("att%d" % i, [NE, 128], F32) for i in range(2)]
                    pT8 = S.ps("pT8o", [128, 8, 128], BF16)
                    ps_out = S.ps("ps_out", [128, 1024])
                    ps_hT = S.ps("ps_hT", [128, 8, 128], F32)
                    ps_lg = S.ps("ps_lg", [128, 16])
                    ps_at = S.ps("ps_at", [NE, 128])
                    ptiles = [t_ for t_ in tiles if not (last and t_ < 2)]
                    for tt in ptiles:
                        k2 = tt % 2
                        j = 1 if tt < 2 else 0
                        for c_ in range(8):
                            TR(pT8[:, c_, :], mixtok[:, tt, c_ * 128:(c_ + 1) * 128], ident_b[:], [mixtok.s(tt), ident_b], [pT8])
                        CP("act", mixT[k2][:], pT8[:], [pT8], [mixT[k2]])
                        for half in range(2):
                            for c_ in range(8):
                                MM(ps_out[:, half * 512:(half + 1) * 512], mixT[k2][:, c_, :], wo[:, c_, half * 512:(half + 1) * 512],
                                   [mixT[k2], wo], [ps_out], start=(c_ == 0), stop=(c_ == 7))
                        src, sb_ = xsrc(tt)
                        DMA("sp", xin[k2][:], src, sb_, [xin[k2]])
                        TT("dve", tg[:], ps_out[:], modG[:, j, :], ALU.mult, [ps_out, modG], [tg])
                        TT("pool", xnew[k2][:], tg[:], xin[k2][:], ALU.add, [tg, xin[k2]], [xnew[k2]])
                        DMA("sp", xres[tt * 128:(tt + 1) * 128, :], xnew[k2][:], [xnew[k2]], [xres_b[tt]])
                        ACT(junk[:], xnew[k2][:], AF.Square, [xnew[k2]], [junk, st[k2]], accum_out=st[k2][:, 0:1])
                        ACT(st[k2][:, 1:2], st[k2][:, 0:1], AF.Sqrt, [st[k2]], [st[k2]], scale=1.0 / D, bias=EPS)
                        RCP(st[k2][:, 2:3], st[k2][:, 1:2], [st[k2]], [st[k2]])
                        STT(t1[:], xnew[k2][:], st[k2][:, 2:3], modA[:, j, :], ALU.mult, ALU.mult, [xnew[k2], st[k2], modA], [t1])
                        TT("pool", h2f[:], t1[:], modS[:, j, :], ALU.add, [t1, modS], [h2f])
                        CP("act", tokbuf[:, tt, :], h2f[:], [h2f], [tokbuf.s(tt)])
                        for c_ in range(8):
                            TR(ps_hT[:, c_, :], h2f[:, c_ * 128:(c_ + 1) * 128], ident_f[:], [h2f, ident_f], [ps_hT])
                        CP("dve", h2fT[:], ps_hT[:], [ps_hT], [h2fT])
                        for c_ in range(8):
                            MM(ps_lg[:], h2fT[:, c_, :], wr[:, c_, :], [h2fT, wr], [ps_lg], start=(c_ == 0), stop=(c_ == 7))
                        RED(rt[:, 0:1], ps_lg[:], [ps_lg], [rt], op=ALU.max)
                        TS("dve", rt[:, 1:2], rt[:, 0:1], -1.0, None, ALU.mult, None, [rt], [rt])
                        ACT(rt[:, 8:24], ps_lg[:], AF.Exp, [ps_lg, rt], [rt], bias=rt[:, 1:2], accum_out=rt[:, 2:3])
                        RCP(rt[:, 3:4], rt[:, 2:3], [rt], [rt])
                        TS("dve", rt[:, 24:40], rt[:, 8:24], rt[:, 3:4], None, ALU.mult, None, [rt], [rt])
                        TR(ps_at[:], rt[:, 24:40], ident_f[:], [rt, ident_f], [ps_at])
                        CP("act", att[k2][:], ps_at[:], [ps_at], [att[k2]])
                        DMA("sp", affd[:, tt * 128:(tt + 1) * 128], att[k2][:], [att[k2]], [affd_b])
                    if l == 0 and b == 0:
                        dump("h2tok", [tokbuf.s(t_) for t_ in ptiles], tokbuf[:], [128, NT, 1024], BF16)
                        dump("affT", [affd_b], affd, [NE, T], F32)
                        dump("xres", [xres_b[t_] for t_ in ptiles], xres, [T, D], F32)
                if moe:
                    nslot = CAP if last else NSLOT
                    cchunks = [(0, 128), (128, 128)] + ([] if last else [(256, CAPC)])
                    ltiles = list(range(2, NT))
                    with S.scope():
                        vals = S.sb("vals", [NE, NSLOT], F32)
                        idxu = S.sb("idxu", [NE, NSLOT], U32)
                        idxf = S.sb("idxf", [NE, NSLOT], F32)
                        valsT = S.sb("valsT", [128, 3, NE], F32)
                        idxT = S.sb("idxT", [128, 3, NE], F32)
                        with S.scope():
                            affw = S.sb("affw", [NE, T], F32)
                            DMA("sp", affw[:], affd, [affd_b], [affw])
                            segs_k = [(256, SEQ, 0, CAP // 8)] + ([] if last else [(0, CTX, CAP, CAPC // 8)])
                            for (a0, an, s0, nr) in segs_k:
                                src = affw[:, a0:a0 + an]
                                for r in range(nr):
                                    vs = vals[:, s0 + r * 8:s0 + (r + 1) * 8]
                                    S.op("dve", lambda e, vs=vs, src=src: e.max(out=vs, in_=src), [affw], [vals])
                                    ix = idxu[:, s0 + r * 8:s0 + (r + 1) * 8]
                                    S.op("dve", lambda e, ix=ix, vs=vs, src=src: e.max_index(out=ix, in_max=vs, in_values=src),
                                         [affw, vals], [idxu])
                                    if r < nr - 1:
                                        S.op("dve", lambda e, vs=vs, src=src: e.match_replace(out=src, in_to_replace=vs, in_values=src,
                                                                                             imm_value=-1.0), [affw, vals], [affw])
                            CP("dve", idxf[:, 0:nslot], idxu[:, 0:nslot], [idxu], [idxf])
                            ps_t = S.ps("ps_t", [128, 2, 3, NE])
                            for ck, (c0, cn) in enumerate(cchunks):
                                TR(ps_t[0:cn, 0, ck, :], vals[:, c0:c0 + cn], ident_f[0:NE, 0:NE], [vals, ident_f], [ps_t])
                                TR(ps_t[0:cn, 1, ck, :], idxf[:, c0:c0 + cn], ident_f[0:NE, 0:NE], [idxf, ident_f], [ps_t])
                                CP("dve", valsT[0:cn, ck, :], ps_t[0:cn, 0, ck, :], [ps_t], [valsT])
                                CP("dve", idxT[0:cn, ck, :], ps_t[0:cn, 1, ck, :], [ps_t], [idxT])
                            if l == 0 and b == 0:
                                dump("vals", [vals], vals[:], [NE, NSLOT], F32)
                                dump("idxf", [idxf], idxf[:], [NE, NSLOT], F32)
                        yacc = S.sb("yacc", [128, NT, 1024], F32)
                        yall = [yacc.s(t_) for t_ in tiles]
                        MS("pool", yacc[:], 0.0, yall)
                        with S.scope():
                            ring = [S.sb("wring%d" % i, [128, 3072], BF16) for i in range(6)]
                            Ssel = S.sb("Ssel", [128, NT, NSLOT], BF16)
                            STl = S.sb("STl", [128, 2, SEQ], BF16)
                            STc = S.sb("STc", [CAPC, CTX], BF16)
                            iota = S.sb("iota", [128, SEQ], F32)
                            DMA("sp", iota[:], din["iota"], [], [iota])
                            xeT = S.sb("xeT", [128, 8, NSLOT], BF16)
                            hid = S.sb("hid", [128, NF, NSLOT], BF16)
                            sgt = S.sb("sgt", [128, NSLOT], F32)
                            yeg = S.sb("yeg", [128, 3, 1024], BF16)
                            idm = S.sb("idm", [NE, NSLOT], F32)
                            ps_ab = [S.ps("ps_ab%d" % i, [128, 512]) for i in range(2)]
                            ps_gt = S.ps("ps_gt", [128, 512])
                            ps_up = S.ps("ps_up", [128, 512])
                            ps_d = [S.ps("ps_d%d" % i, [128, 512]) for i in range(2)]
                            ps_sc = [S.ps("ps_sc%d" % i, [128, 512]) for i in range(2)]
                            fgroups = [(0, 3), (3, 3), (6, 3), (9, 2)]
                            nring = 0
                            nab = 0
                            nd = 0
                            nsc = 0
                            hall_ = [tokbuf.s(t_) for t_ in tiles]
                            pf = {"issued": 0, "consumed": 0}

                            def issue_piece(k):
                                e2, r2 = divmod(k, 12)
                                r_ = ring[k % 6]
                                if r2 < 8:
                                    gi = r2 // 2
                                    nm = "w_up" if (r2 % 2) else "w_gate"
                                    f0, nf = fgroups[gi]
                                    DMA("pool", r_[:, 0:8 * nf * 128].rearrange("p (c n) -> p c n", c=8),
                                        din[nm][l, e2, :, f0 * 128:(f0 + nf) * 128].rearrange("(c p) n -> p c n", p=128), [], [r_])
                                else:
                                    f0, nf = fgroups[r2 - 8]
                                    DMA("pool", r_[:, 0:nf * 1024].rearrange("p (c n) -> p c n", c=nf),
                                        din["w_down"][l, e2, f0 * 128:(f0 + nf) * 128, :].rearrange("(c p) n -> p c n", p=128), [], [r_])

                            def release(n):
                                pf["consumed"] += n
                                while pf["issued"] < pf["consumed"] + 6 and pf["issued"] < NE * 12:
                                    issue_piece(pf["issued"])
                                    pf["issued"] += 1

                            release(0)
                            for e_ in range(NE):
                                pieces = {}
                                for gi in range(4):
                                    for nm in ("w_gate", "w_up"):
                                        pieces[(nm, gi)] = ring[(e_ * 12 + gi * 2 + (nm == "w_up")) % 6]
                                TS("dve", idm[:, 0:nslot], idxf[:, 0:nslot], ident_f[0:NE, e_:e_ + 1], None, ALU.mult, None, [idxf, ident_f], [idm])
                                pi = ps_ab[nab % 2]
                                nab += 1
                                MM(pi[:, 0:nslot], ones_f[0:NE, :], idm[:, 0:nslot], [ones_f, idm], [pi])
                                for j in range(16):
                                    TS("dve", Ssel[:, 2 + j, 0:CAP], pi[:, 0:CAP], tpos[:, j:j + 1], None, ALU.is_equal, None, [pi, tpos], [Ssel])
                                if not last:
                                    for j in range(2):
                                        TS("dve", Ssel[:, j, CAP:NSLOT], pi[:, CAP:NSLOT], tpos[:, j:j + 1], None, ALU.is_equal, None,
                                           [pi, tpos], [Ssel])
                                for ck in range(2):
                                    TS("dve", STl[:, ck, :], iota[:], idxT[:, ck, e_:e_ + 1], None, ALU.is_equal, None, [iota, idxT], [STl])
                                if not last:
                                    TS("dve", STc[:], iota[0:CAPC, 0:CTX], idxT[0:CAPC, 2, e_:e_ + 1], None, ALU.is_equal, None, [iota, idxT], [STc])
                                for dc in range(8):
                                    pg = ps_ab[nab % 2]
                                    nab += 1
                                    for j in range(16):
                                        MM(pg[:, 0:CAP], tokbuf[:, 2 + j, dc * 128:(dc + 1) * 128], Ssel[:, 2 + j, 0:CAP],
                                           [tokbuf.s(2 + j), Ssel], [pg], start=(j == 0), stop=(j == 15))
                                    if not last:
                                        for j in range(2):
                                            MM(pg[:, CAP:NSLOT], tokbuf[:, j, dc * 128:(dc + 1) * 128], Ssel[:, j, CAP:NSLOT],
                                               [tokbuf.s(j), Ssel], [pg], start=(j == 0), stop=(j == 1))
                                    CP("act", xeT[:, dc, 0:nslot], pg[:, 0:nslot], [pg], [xeT])
                                for gi, (f0, nf) in enumerate(fgroups):
                                    wg_ = pieces[("w_gate", gi)]
                                    wu_ = pieces[("w_up", gi)]
                                    wg3 = wg_[:, 0:8 * nf * 128].rearrange("p (c n) -> p c n", c=8)
                                    wu3 = wu_[:, 0:8 * nf * 128].rearrange("p (c n) -> p c n", c=8)
                                    for fi in range(nf):
                                        f = f0 + fi
                                        for c_ in range(8):
                                            MM(ps_gt[:, 0:nslot], wg3[:, c_, fi * 128:(fi + 1) * 128], xeT[:, c_, 0:nslot], [wg_, xeT], [ps_gt],
                                               start=(c_ == 0), stop=(c_ == 7))
                                        for c_ in range(8):
                                            MM(ps_up[:, 0:nslot], wu3[:, c_, fi * 128:(fi + 1) * 128], xeT[:, c_, 0:nslot], [wu_, xeT], [ps_up],
                                               start=(c_ == 0), stop=(c_ == 7))
                                        ACT(sgt[:, 0:nslot], ps_gt[:, 0:nslot], AF.Silu, [ps_gt], [sgt])
                                        TT("dve", hid[:, f, 0:nslot], sgt[:, 0:nslot], ps_up[:, 0:nslot], ALU.mult, [sgt, ps_up], [hid])
                                    release(2)
                                dps = [ring[(e_ * 12 + 8 + gi) % 6] for gi in range(4)]
                                for ck, (c0, cn) in enumerate(cchunks):
                                    for half in range(2):
                                        pd = ps_d[nd % 2]
                                        nd += 1
                                        for gi, (f0, nf) in enumerate(fgroups):
                                            w3 = dps[gi][:, 0:nf * 1024].rearrange("p (c n) -> p c n", c=nf)
                                            for fi in range(nf):
                                                f = f0 + fi
                                                MM(pd[0:cn, :], hid[:, f, c0:c0 + cn], w3[:, fi, half * 512:(half + 1) * 512], [hid, dps[gi]], [pd],
                                                   start=(f == 0), stop=(f == NF - 1))
                                        ACT(yeg[0:cn, ck, half * 512:(half + 1) * 512], pd[0:cn, :], AF.Copy, [pd, valsT], [yeg],
                                            scale=valsT[0:cn, ck, e_:e_ + 1])
                                release(4)
                                for j in range(16):
                                    for half in range(2):
                                        pq = ps_sc[nsc % 2]
                                        nsc += 1
                                        for ck in range(2):
                                            MM(pq[:], STl[:, ck, j * 128:(j + 1) * 128], yeg[:, ck, half * 512:(half + 1) * 512], [STl, yeg], [pq],
                                               start=(ck == 0), stop=(ck == 1))
                                        ya = yacc[:, 2 + j, half * 512:(half + 1) * 512]
                                        TT("dve", ya, pq[:], ya, ALU.add, [pq, yacc.s(2 + j)], [yacc.s(2 + j)])
                                if not last:
                                    for j in range(2):
                                        for half in range(2):
                                            pq = ps_sc[nsc % 2]
                                            nsc += 1
                                            MM(pq[:], STc[:, j * 128:(j + 1) * 128], yeg[0:CAPC, 2, half * 512:(half + 1) * 512], [STc, yeg], [pq])
                                            ya = yacc[:, j, half * 512:(half + 1) * 512]
                                            TT("dve", ya, pq[:], ya, ALU.add, [pq, yacc.s(j)], [yacc.s(j)])
                            if l == 0 and b == 0:
                                dump("yacc", yall, yacc[:], [128, NT, 1024], F32)
                        with S.scope():
                            modG = S.sb("modG2", [128, 2, D], F32)
                            load_mod(modG, 5)
                            xin = [S.sb("xin3_%d" % i, [128, D], F32) for i in range(2)]
                            xo = [S.sb("xo%d" % i, [128, D], F32) for i in range(2)]
                            tg = S.sb("tg2", [128, D], F32)
                            for tt in [t_ for t_ in tiles if not (last and t_ < 2)]:
                                k2 = tt % 2
                                j = 1 if tt < 2 else 0
                                DMA("sp", xin[k2][:], xres[tt * 128:(tt + 1) * 128, :], [xres_b[tt]], [xin[k2]])
                                TT("dve", tg[:], yacc[:, tt, :], modG[:, j, :], ALU.mult, [yacc.s(tt), modG], [tg])
                                TT("pool", xo[k2][:], tg[:], xin[k2][:], ALU.add, [tg, xin[k2]], [xo[k2]])
                                if last:
                                    DMA("sp", out[b, (tt - 2) * 128:(tt - 1) * 128, :], xo[k2][:], [xo[k2]], [])
                                else:
                                    DMA("sp", xres[tt * 128:(tt + 1) * 128, :], xo[k2][:], [xo[k2]], [xres_b[tt]])
                            if l == 0 and b == 0:
                                dump("xfin", list(xres_b), xres, [T, D], F32)
                S.pop()
                S.new_epoch()
        S.barrier()
        S.emit()
    return nc, dumps


_CACHE = {}


def kernel(**inputs):
    nb = 2
    n_cores = 8
    if "nc" not in _CACHE:
        _CACHE["nc"] = build(nb=nb, depth=DEPTH)[0]
    nc = _CACHE["nc"]
    consts = host_consts()
    in_maps = []
    for i in range(n_cores):
        m = {"x": np.ascontiguousarray(inputs["x"][i * nb:(i + 1) * nb]),
             "ctx": np.ascontiguousarray(inputs["ctx"][i * nb:(i + 1) * nb]),
             "c": np.ascontiguousarray(inputs["c"][i * nb:(i + 1) * nb])}
        for k_ in PARAM_SHAPES:
            m[k_] = np.ascontiguousarray(inputs[k_], dtype=np.float32)
        for k_ in CONST_SHAPES:
            m["k_" + k_] = consts[k_]
        in_maps.append(m)
    res = run_bass_kernel_spmd(nc, in_maps, core_ids=list(range(n_cores)))
    return np.concatenate([r["out"] for r in res.results], axis=0)
```

```python
import math
from contextlib import ExitStack, contextmanager
import numpy as np
import concourse.bass as bass
import concourse.mybir as mybir
from concourse.bass_utils import run_bass_kernel_spmd

F32 = mybir.dt.float32
BF16 = mybir.dt.bfloat16
U32 = mybir.dt.uint32
AF = mybir.ActivationFunctionType
ALU = mybir.AluOpType
AX = mybir.AxisListType

NRING = 8
D = 1024
SEQ = 2048
CTX = 256
T = SEQ + CTX
NT = T // 128
DEPTH = 4
P_IN = 3088
NE = 16
DE = 1408
NF = 11
CAP = 256
CAPC = 32
NSLOT = CAP + CAPC
EPS = 1e-6


class Buf:
    __slots__ = ("name", "w", "r")

    def __init__(self, name):
        self.name = name
        self.w = None
        self.r = []


class Tn:
    __slots__ = ("t", "_buf", "_subs", "name")

    def __init__(self, t, name):
        self.t = t
        self.name = name
        self._buf = Buf(name)
        self._subs = {}

    def __getitem__(self, k):
        return self.t[k]

    def s(self, i):
        b = self._subs.get(i)
        if b is None:
            b = self._subs[i] = Buf("%s.%s" % (self.name, i))
        return b


class Sched:
    ENGS = ("pe", "act", "dve", "pool", "sp")

    def __init__(self, nc, stack):
        self.nc = nc
        self.stacks = [stack]
        self.ops = {e: [] for e in self.ENGS}
        self.cnt = {e: 0 for e in self.ENGS}
        self.sem = {e: stack.enter_context(nc.semaphore("s_" + e)) for e in self.ENGS}
        self.dring = {e: [stack.enter_context(nc.semaphore("d_%s%d" % (e, i))) for i in range(NRING)]
                      for e in ("sp", "pool", "act")}
        self.dcnt = {e: 0 for e in ("sp", "pool", "act")}
        self.waited = {e: {} for e in self.ENGS}
        self.semobj = {}
        self.uid = 0
        self.retired = set()
        self.nep = 0

    def new_epoch(self):
        self.nep += 1
        for e in self.ENGS:
            self.retired.add(id(self.sem[e]))
            self.sem[e] = self.stacks[0].enter_context(self.nc.semaphore("s_%s_%d" % (e, self.nep)))
            self.cnt[e] = 0

    def sb(self, name, shape, dt):
        self.uid += 1
        nm = "%s_%d" % (name, self.uid)
        t = self.stacks[-1].enter_context(self.nc.sbuf_tensor(nm, list(shape), dt))
        return Tn(t, nm)

    def ps(self, name, shape, dt=F32):
        self.uid += 1
        nm = "%s_%d" % (name, self.uid)
        t = self.stacks[-1].enter_context(self.nc.psum_tensor(nm, list(shape), dt))
        return Tn(t, nm)

    def push(self):
        self.stacks.append(ExitStack())

    def pop(self):
        self.barrier()
        self.stacks.pop().close()

    @contextmanager
    def scope(self):
        st = ExitStack()
        self.stacks.append(st)
        try:
            yield
            self.barrier()
        finally:
            self.stacks.pop()
            st.close()

    def _need(self, eng, tok, waits):
        sid, val, teng, is_dma = tok
        if sid in self.retired:
            return
        w = self.waited[eng]
        if w.get(sid, 0) >= val:
            return
        w[sid] = val
        waits.append((sid, val))

    def _deps(self, eng, reads, writes, waits):
        for b in reads:
            if b.w is not None:
                self._need(eng, b.w, waits)
        for b in writes:
            tok = b.w
            if tok is not None and not (tok[2] == eng and not tok[3]):
                self._need(eng, tok, waits)
            for tok in b.r:
                if not (tok[2] == eng and not tok[3]):
                    self._need(eng, tok, waits)

    @staticmethod
    def _bufs(xs):
        out = []
        for x in xs:
            if x is None:
                continue
            out.append(x if isinstance(x, Buf) else x._buf)
        return out

    def _commit(self, tok, reads, writes):
        for b in reads:
            b.r.append(tok)
        for b in writes:
            b.w = tok
            b.r = []

    def op(self, eng, fn, reads=(), writes=()):
        reads = self._bufs(reads)
        writes = self._bufs(writes)
        waits = []
        self._deps(eng, reads, writes, waits)
        self.cnt[eng] += 1
        s = self.sem[eng]
        self.semobj[id(s)] = s
        tok = (id(s), self.cnt[eng], eng, False)
        self._commit(tok, reads, writes)
        self.ops[eng].append((waits, fn, s, 1))
        return tok

    def dma(self, q, fn, reads=(), writes=()):
        reads = self._bufs(reads)
        writes = self._bufs(writes)
        waits = []
        self._deps(q, reads, writes, waits)
        k = self.dcnt[q]
        self.dcnt[q] += 1
        s = self.dring[q][k % NRING]
        self.semobj[id(s)] = s
        target = 16 * (k // NRING + 1)
        if k >= NRING:
            self._need(q, (id(s), target - 16, q, True), waits)
        tok = (id(s), target, q, True)
        self._commit(tok, reads, writes)
        self.ops[q].append((waits, fn, s, 16))
        return tok

    def all_tokens(self):
        toks = []
        for e in self.ENGS:
            if self.cnt[e] > 0:
                s = self.sem[e]
                self.semobj[id(s)] = s
                toks.append((id(s), self.cnt[e], e, False))
        for q in self.dcnt:
            k = self.dcnt[q]
            for i in range(min(k, NRING)):
                uses = (k - i + NRING - 1) // NRING
                toks.append((id(self.dring[q][i]), 16 * uses, q, True))
        return toks

    def barrier(self):
        toks = self.all_tokens()
        for e in self.ENGS:
            waits = []
            for t in toks:
                if t[2] == e and not t[3]:
                    continue
                self._need(e, t, waits)
            if waits:
                self.ops[e].append((waits, None, None, 0))

    def emit(self):
        nc = self.nc
        with nc.Block() as block:
            def run(e):
                def body(engobj):
                    for waits, fn, s, inc in self.ops[e]:
                        for sid, val in waits:
                            engobj.wait_ge(self.semobj[sid], val)
                        if fn is not None:
                            fn(engobj).then_inc(s, inc)
                return body
            block.tensor(run("pe"))
            block.scalar(run("act"))
            block.vector(run("dve"))
            block.gpsimd(run("pool"))
            block.sync(run("sp"))


def host_consts():
    k = np.arange(128)
    c = {}
    c["ident"] = np.eye(128, dtype=np.float32)
    c["ones"] = np.ones((128, 128), np.float32)
    tri_f = (k[:, None] <= k[None, :]).astype(np.float32)
    tri_b = (k[:, None] >= k[None, :]).astype(np.float32)
    c["tri"] = np.stack([tri_f, tri_b])
    mf = np.where(k[None, :] >= k[:, None], 0.0, -1e5).astype(np.float32)
    mb = np.where(k[None, :] <= k[:, None], 0.0, -1e5).astype(np.float32)
    c["mneg"] = np.stack([np.tile(mf, (1, 4)), np.tile(mb, (1, 4))])
    n = SEQ
    row = np.repeat(np.arange(n // 64), 64)
    col = np.tile(np.arange(64), n // 64)
    inv = (10000.0 ** (-np.arange(16, dtype=np.float32) / 16)).astype(np.float32)
    ang = np.stack([row, col], -1).astype(np.float32)[..., None] * inv
    cs, sn = np.cos(ang).astype(np.float32), np.sin(ang).astype(np.float32)
    C = np.stack([cs, cs], 2)
    Sg = np.stack([-sn, sn], 2)
    c["ropeC"] = C.reshape(n, 64).astype(np.float32)
    c["ropeS"] = Sg.reshape(n, 64).astype(np.float32)
    c["iota"] = np.tile(np.arange(SEQ, dtype=np.float32)[None, :], (128, 1))
    c["tpos"] = (k[:, None] + 128 * np.arange(16)[None, :]).astype(np.float32)
    return c


CONST_SHAPES = {"ident": [128, 128], "ones": [128, 128], "tri": [2, 128, 128], "mneg": [2, 128, 512],
                "ropeC": [SEQ, 64], "ropeS": [SEQ, 64], "iota": [128, SEQ], "tpos": [128, 16]}

def param_shapes(depth=DEPTH, moe=True):
    ps = {
        "c_ctx": [D], "w_mod": [depth, D, 6 * D], "b_mod": [depth, 6 * D], "norm1_w": [depth, D], "norm2_w": [depth, D],
        "w_in": [depth, D, P_IN], "q_norm_w": [depth, 64], "k_norm_w": [depth, 64],
        "lambda_q1": [depth, 64], "lambda_k1": [depth, 64], "lambda_q2": [depth, 64], "lambda_k2": [depth, 64],
        "subln_w": [depth, 128], "conv_w": [depth, 3, D], "conv_b": [depth, D],
        "dt_bias_f": [depth, 8], "dt_bias_b": [depth, 8], "a_log_f": [depth, 8], "a_log_b": [depth, 8],
        "d_skip": [depth, 8], "ssd_norm_w": [depth, 512], "w_out": [depth, D, D], "w_router": [depth, D, NE],
    }
    if moe:
        ps.update({"w_gate": [depth, NE, D, DE], "w_up": [depth, NE, D, DE], "w_down": [depth, NE, DE, D]})
    return ps


PARAM_SHAPES = param_shapes()


def build(nb=2, depth=DEPTH, dbg=None, moe=True, force_last=False):
    nc = bass.Bass("TRN2", target_bir_lowering=False)
    din = {}
    din["x"] = nc.dram_tensor("x", [nb, SEQ, D], F32, kind="ExternalInput").ap()
    din["ctx"] = nc.dram_tensor("ctx", [nb, CTX, D], F32, kind="ExternalInput").ap()
    din["c"] = nc.dram_tensor("c", [nb, D], F32, kind="ExternalInput").ap()
    for k_, shp in param_shapes(depth, moe).items():
        din[k_] = nc.dram_tensor(k_, shp, F32, kind="ExternalInput").ap()
    for k_, shp in CONST_SHAPES.items():
        din[k_] = nc.dram_tensor("k_" + k_, shp, F32, kind="ExternalInput").ap()
    out = nc.dram_tensor("out", [nb, SEQ, D], F32, kind="ExternalOutput").ap()
    xres = nc.dram_tensor("xres", [T, D], F32).ap()
    modrow = nc.dram_tensor("modrow", [depth, nb + 1, 6 * D], F32).ap()
    affd = nc.dram_tensor("affd", [NE, T], F32).ap()
    hTd = nc.dram_tensor("hTd", [128, 8, T], BF16).ap()
    dumps = {}

    with ExitStack() as st0:
        S = Sched(nc, st0)
        st0.enter_context(nc.allow_non_contiguous_dma(reason="small param layouts"))
        st0.enter_context(nc.allow_low_precision(reason="bf16 matmul operands by design"))

        def MM(oap, lap, rap, R, W, start=True, stop=True):
            S.op("pe", lambda e: e.matmul(oap, lhsT=lap, rhs=rap, start=start, stop=stop), R, W)

        def TR(oap, iap, idap, R, W):
            S.op("pe", lambda e: e.transpose(out=oap, in_=iap, identity=idap), R, W)

        def ACT(oap, iap, func, R, W, **kw):
            S.op("act", lambda e: e.activation(out=oap, in_=iap, func=func, **kw), R, W)

        def TT(eng, oap, a, b, op, R, W):
            S.op(eng, lambda e: e.tensor_tensor(out=oap, in0=a, in1=b, op=op), R, W)

        def TS(eng, oap, a, s1, s2, op0, op1, R, W):
            if op1 is None:
                S.op(eng, lambda e: e.tensor_scalar(out=oap, in0=a, scalar1=s1, scalar2=None, op0=op0), R, W)
            else:
                S.op(eng, lambda e: e.tensor_scalar(out=oap, in0=a, scalar1=s1, scalar2=s2, op0=op0, op1=op1), R, W)

        def STT(oap, a, s, b, op0, op1, R, W):
            S.op("dve", lambda e: e.scalar_tensor_tensor(out=oap, in0=a, scalar=s, in1=b, op0=op0, op1=op1), R, W)

        def CP(eng, oap, iap, R, W):
            if eng == "act":
                S.op("act", lambda e: e.copy(out=oap, in_=iap), R, W)
            else:
                S.op(eng, lambda e: e.tensor_copy(out=oap, in_=iap), R, W)

        def RED(oap, iap, R, W, op=ALU.add):
            S.op("dve", lambda e: e.tensor_reduce(out=oap, in_=iap, axis=AX.X, op=op), R, W)

        def RCP(oap, iap, R, W):
            S.op("dve", lambda e: e.reciprocal(out=oap, in_=iap), R, W)

        def MS(eng, ap, val, W):
            S.op(eng, lambda e: e.memset(ap, val), (), W)

        def DMA(q, oap, iap, R, W):
            return S.dma(q, lambda e: e.dma_start(out=oap, in_=iap), R, W)

        xres_b = [Buf("xres%d" % i) for i in range(NT)]
        modrow_b = Buf("modrow")
        affd_b = Buf("affd")
        hTd_b = Buf("hTd")
        out_toks = []

        def dump(name, tn_or_bufs, ap, shape, dt=F32):
            if dbg is None or name not in dbg or name in dumps:
                return
            d = nc.dram_tensor("dbg_" + name, list(shape), dt, kind="ExternalOutput").ap()
            dumps[name] = (list(shape), dt)
            R = tn_or_bufs if isinstance(tn_or_bufs, (list, tuple)) else [tn_or_bufs]
            out_toks.append(DMA("sp", d, ap, R, []))

        ident_f = S.sb("ident_f", [128, 128], F32)
        ident_b = S.sb("ident_b", [128, 128], BF16)
        ones_f = S.sb("ones_f", [128, 128], F32)
        tri = S.sb("tri", [128, 2, 128], F32)
        tpos = S.sb("tpos", [128, 16], F32)
        DMA("sp", ident_f[:], din["ident"], [], [ident_f])
        DMA("sp", ones_f[:], din["ones"], [], [ones_f])
        DMA("sp", tri[:], din["tri"].rearrange("a p l -> p a l"), [], [tri])
        DMA("sp", tpos[:], din["tpos"], [], [tpos])
        CP("dve", ident_b[:], ident_f[:], [ident_f], [ident_b])

        with S.scope():
            cT = S.sb("cT", [128, 8, nb + 1], F32)
            sT = S.sb("sT", [128, 8, nb + 1], BF16)
            sg = S.sb("sg", [128, 8, nb + 1], F32)
            for j in range(nb):
                DMA("sp", cT[:, :, j], din["c"][j].rearrange("(c p) -> p c", p=128), [], [cT])
            DMA("sp", cT[:, :, nb], din["c_ctx"].rearrange("(c p) -> p c", p=128), [], [cT])
            ACT(sg[:], cT[:], AF.Sigmoid, [cT], [sg])
            TT("dve", sT[:], cT[:], sg[:], ALU.mult, [cT, sg], [sT])
            wm = [S.sb("wm%d" % i, [128, 8, 1536], BF16) for i in range(2)]
            bm = S.sb("bm", [nb + 1, 6 * D], F32)
            mr = S.sb("mr", [nb + 1, 6 * D], F32)
            pm = [S.ps("pm%d" % i, [nb + 1, 512]) for i in range(2)]
            it = 0
            for l in range(depth):
                DMA("sp", bm[:], din["b_mod"][l:l + 1, :].to_broadcast([nb + 1, 6 * D]), [], [bm])
                for blk in range(4):
                    w_ = wm[it % 2]
                    it += 1
                    DMA("pool", w_[:], din["w_mod"][l, :, blk * 1536:(blk + 1) * 1536].rearrange("(c p) n -> p c n", p=128),
                        [], [w_])
                    for sub in range(3):
                        p_ = pm[sub % 2]
                        for c_ in range(8):
                            MM(p_[:], sT[:, c_, :], w_[:, c_, sub * 512:(sub + 1) * 512], [sT, w_], [p_],
                               start=(c_ == 0), stop=(c_ == 7))
                        col = blk * 1536 + sub * 512
                        TT("dve", mr[:, col:col + 512], p_[:], bm[:, col:col + 512], ALU.add, [p_, bm], [mr])
                DMA("sp", modrow[l], mr[:], [mr], [modrow_b])

        for b in range(nb):
            for l in range(depth):
                last = (l == DEPTH - 1) or force_last
                lam_init = 0.8 - 0.6 * math.exp(-0.3 * l)
                tiles = list(range(NT))

                def xsrc(tt):
                    if l == 0:
                        if tt < 2:
                            return din["ctx"][b, tt * 128:(tt + 1) * 128, :], []
                        return din["x"][b, (tt - 2) * 128:(tt - 1) * 128, :], []
                    return xres[tt * 128:(tt + 1) * 128, :], [xres_b[tt]]

                def load_mod(dst, which, eng="sp"):
                    DMA(eng, dst[:, 0, :], modrow[l, b:b + 1, which * D:(which + 1) * D].to_broadcast([128, D]), [modrow_b], [dst])
                    DMA(eng, dst[:, 1, :], modrow[l, nb:nb + 1, which * D:(which + 1) * D].to_broadcast([128, D]), [modrow_b], [dst])

                def do_norm1(hT):
                    with S.scope():
                        modA = S.sb("modA", [128, 2, D], F32)
                        modS = S.sb("modS", [128, 2, D], F32)
                        nw = S.sb("nw", [128, D], F32)
                        load_mod(modS, 0)
                        load_mod(modA, 1)
                        DMA("sp", nw[:], din["norm1_w"][l:l + 1, :].to_broadcast([128, D]), [], [nw])
                        for j in range(2):
                            STT(modA[:, j, :], modA[:, j, :], 1.0, nw[:], ALU.add, ALU.mult, [modA, nw], [modA])
                        xin = [S.sb("xin%d" % i, [128, D], F32) for i in range(2)]
                        junk = S.sb("junk", [128, D], BF16)
                        t1 = [S.sb("t1_%d" % i, [128, D], F32) for i in range(2)]
                        hb = [S.sb("hb%d" % i, [128, D], BF16) for i in range(2)]
                        st = [S.sb("st%d" % i, [128, 4], F32) for i in range(2)]
                        pT8 = [S.ps("pT8_%d" % i, [128, 8, 128], BF16) for i in range(2)]
                        for tt in tiles:
                            k2 = tt % 2
                            j = 1 if tt < 2 else 0
                            src, sb_ = xsrc(tt)
                            DMA("sp", xin[k2][:], src, sb_, [xin[k2]])
                            ACT(junk[:], xin[k2][:], AF.Square, [xin[k2]], [junk, st[k2]], accum_out=st[k2][:, 0:1])
                            ACT(st[k2][:, 1:2], st[k2][:, 0:1], AF.Sqrt, [st[k2]], [st[k2]], scale=1.0 / D, bias=EPS)
                            RCP(st[k2][:, 2:3], st[k2][:, 1:2], [st[k2]], [st[k2]])
                            STT(t1[k2][:], xin[k2][:], st[k2][:, 2:3], modA[:, j, :], ALU.mult, ALU.mult,
                                [xin[k2], st[k2], modA], [t1[k2]])
                            TT("pool", hb[k2][:], t1[k2][:], modS[:, j, :], ALU.add, [t1[k2], modS], [hb[k2]])
                            for c_ in range(8):
                                TR(pT8[k2][:, c_, :], hb[k2][:, c_ * 128:(c_ + 1) * 128], ident_b[:], [hb[k2], ident_b], [pT8[k2]])
                            CP("act", hT[:, :, tt * 128:(tt + 1) * 128], pT8[k2][:], [pT8[k2]], [hT.s(tt)])
                        if l == 0 and b == 0:
                            dump("hT", [hT.s(t_) for t_ in tiles], hT[:], [128, 8, T], BF16)


                S.push()
                tokbuf = S.sb("tokbuf", [128, NT, 1024], BF16)
                mixtok = tokbuf
                with S.scope():
                    hT = S.sb("hT", [128, 8, T], BF16)
                    do_norm1(hT)
                    DMA("sp", hTd, hT[:], [hT.s(t_) for t_ in tiles], [hTd_b])
                    with S.scope():
                        qkT = S.sb("qkT", [128, 8, T], BF16)
                        v_aug = S.sb("v_aug", [128, NT, 4, 132], BF16)
                        MS("pool", v_aug[:], 1.0, [v_aug])
                        nlam = S.sb("nlam", [128, 4], F32)
                        subw = S.sb("subw", [128, 128], F32)
                        with S.scope():
                            wqkv = S.sb("wqkv", [128, 8, 1536], BF16)
                            ropeC = S.sb("ropeC", [128, 16, 64], F32)
                            ropeS = S.sb("ropeS", [128, 16, 64], F32)
                            DMA("sp", ropeC[:], din["ropeC"].rearrange("(j p) d -> p j d", p=128), [], [ropeC])
                            DMA("sp", ropeS[:], din["ropeS"].rearrange("(j p) d -> p j d", p=128), [], [ropeS])
                            DMA("pool", wqkv[:], din["w_in"][l, :, 0:1536].rearrange("(c p) n -> p c n", p=128), [], [wqkv])
                            qkW = S.sb("qkW", [128, 16, 64], F32)
                            for g in range(8):
                                DMA("sp", qkW[:, g, :], din["q_norm_w"][l:l + 1, :].to_broadcast([128, 64]), [], [qkW])
                                DMA("sp", qkW[:, 8 + g, :], din["k_norm_w"][l:l + 1, :].to_broadcast([128, 64]), [], [qkW])
                            lp = S.sb("lp", [128, 4, 64], F32)
                            for i_, nm in enumerate(["lambda_q1", "lambda_k1", "lambda_q2", "lambda_k2"]):
                                DMA("sp", lp[:, i_, :], din[nm][l:l + 1, :].to_broadcast([128, 64]), [], [lp])
                            lpr = S.sb("lpr", [128, 2, 64], F32)
                            TT("dve", lpr[:, 0, :], lp[:, 0, :], lp[:, 1, :], ALU.mult, [lp], [lpr])
                            TT("dve", lpr[:, 1, :], lp[:, 2, :], lp[:, 3, :], ALU.mult, [lp], [lpr])
                            RED(nlam[:, 0:2], lpr[:], [lpr], [nlam])
                            ACT(nlam[:, 0:2], nlam[:, 0:2], AF.Exp, [nlam], [nlam])
                            TT("dve", nlam[:, 2:3], nlam[:, 1:2], nlam[:, 0:1], ALU.subtract, [nlam], [nlam])
                            TS("dve", nlam[:, 3:4], nlam[:, 2:3], -lam_init, None, ALU.add, None, [nlam], [nlam])
                            DMA("sp", subw[:], din["subln_w"][l:l + 1, :].to_broadcast([128, 128]), [], [subw])
                            TS("dve", subw[:], subw[:], 1.0 - lam_init, None, ALU.mult, None, [subw], [subw])

                            ps_qkv = S.ps("ps_qkv", [128, 1536])
                            pT8 = [S.ps("pT8q_%d" % i, [128, 8, 128], BF16) for i in range(2)]
                            sqb = S.sb("sqb", [128, 16, 64], F32)
                            s16 = S.sb("s16", [128, 3, 16], F32)
                            qn = S.sb("qn", [128, 16, 64], F32)
                            qn2 = S.sb("qn2", [128, 16, 64], F32)
                            ta = S.sb("ta", [128, 16, 64], F32)
                            tb = S.sb("tb", [128, 16, 64], F32)
                            qr = [S.sb("qr%d" % i, [128, 16 * 64], BF16) for i in range(2)]
                            for tt in tiles:
                                k2 = tt % 2
                                for j in range(3):
                                    for c_ in range(8):
                                        MM(ps_qkv[:, j * 512:(j + 1) * 512], hT[:, c_, tt * 128:(tt + 1) * 128],
                                           wqkv[:, c_, j * 512:(j + 1) * 512], [hT.s(tt), wqkv], [ps_qkv],
                                           start=(c_ == 0), stop=(c_ == 7))
                                CP("act", v_aug[:, tt, :, 0:128], ps_qkv[:, 1024:1536].rearrange("p (h e) -> p h e", h=4),
                                   [ps_qkv], [v_aug.s(tt)])
                                qk3 = ps_qkv[:, 0:1024].rearrange("p (g e) -> p g e", g=16)
                                ACT(sqb[:], qk3, AF.Square, [ps_qkv], [sqb])
                                RED(s16[:, 0, :], sqb[:], [sqb], [s16])
                                ACT(s16[:, 1, :], s16[:, 0, :], AF.Sqrt, [s16], [s16], scale=1.0 / 64, bias=EPS)
                                RCP(s16[:, 2, :], s16[:, 1, :], [s16], [s16])
                                TT("dve", qn[:], qk3, s16[:, 2, :].unsqueeze(2).to_broadcast([128, 16, 64]), ALU.mult,
                                   [ps_qkv, s16], [qn])
                                if tt < 2:
                                    TT("pool", qr[k2][:].rearrange("p (g e) -> p g e", g=16), qn[:], qkW[:], ALU.mult,
                                       [qn, qkW], [qr[k2]])
                                else:
                                    jj = tt - 2
                                    TT("pool", qn2[:], qn[:], qkW[:], ALU.mult, [qn, qkW], [qn2])
                                    TT("pool", ta[:], qn2[:], ropeC[:, jj, :].unsqueeze(1).to_broadcast([128, 16, 64]), ALU.mult,
                                       [qn2, ropeC], [ta])
                                    q5 = qn2[:].rearrange("p g (a h f) -> p g a h f", a=2, h=2)
                                    t5 = tb[:].rearrange("p g (a h f) -> p g a h f", a=2, h=2)
                                    s5 = ropeS[:, jj, :].rearrange("p (a h f) -> p a h f", a=2, h=2)
                                    for hh in range(2):
                                        TT("dve", t5[:, :, :, hh, :], q5[:, :, :, 1 - hh, :],
                                           s5[:, :, hh, :].unsqueeze(1).to_broadcast([128, 16, 2, 16]), ALU.mult,
                                           [qn2, ropeS], [tb])
                                    TT("dve", qr[k2][:].rearrange("p (g e) -> p g e", g=16), ta[:], tb[:], ALU.add,
                                       [ta, tb], [qr[k2]])
                                for m in range(8):
                                    TR(pT8[k2][:, m, :], qr[k2][:, m * 128:(m + 1) * 128], ident_b[:], [qr[k2], ident_b], [pT8[k2]])
                                CP("act", qkT[:, :, tt * 128:(tt + 1) * 128], pT8[k2][:], [pT8[k2]], [qkT.s(tt)])
                            if l == 0 and b == 0:
                                dump("qkT", [qkT.s(t_) for t_ in tiles], qkT[:], [128, 8, T], BF16)
                                dump("v_aug", [v_aug.s(t_) for t_ in tiles] + [v_aug], v_aug[:], [128, NT, 4, 132], BF16)

                        with S.scope():
                            pTb = [S.sb("pTb%d" % i, [128, NT, 512], BF16) for i in range(2)]
                            ps_s = [S.ps("ps_s%d" % i, [128, 512]) for i in range(3)]
                            ps_o = [[S.ps("ps_o%d_%d" % (i, h_), [128, 512]) for h_ in range(2)] for i in range(2)]
                            oraw = [[S.sb("oraw%d_%d" % (i, q_), [128, 132], F32) for q_ in range(4)] for i in range(2)]
                            ob = [S.sb("ob%d" % i, [128, 2, 128], F32) for i in range(2)]
                            sc_ = [S.sb("sc%d" % i, [128, 8], F32) for i in range(2)]
                            junk2 = S.sb("junk2", [128, 128], BF16)
                            vall = [v_aug] + [v_aug.s(t_) for t_ in tiles]
                            blocks = []
                            if not last:
                                blocks.append((0, 256, [0, 1]))
                            for bq in range(4):
                                blocks.append((256 + bq * 512, 512, list(range(NT))))
                            units = [(hd, blk, i) for hd in range(4) for blk in blocks for i in range(2)]
                            cnt_ = {"sc": 0, "comb": 0}

                            def s_ops(u):
                                hd, (q0, qn_, kcs), i = u
                                ops_ = []
                                for kc in kcs:
                                    def f(kc=kc):
                                        p_ = ps_s[cnt_["sc"] % 3]
                                        cnt_["sc"] += 1
                                        MM(p_[:, 0:qn_], qkT[i * 64:(i + 1) * 64, 4 + hd, kc * 128:(kc + 1) * 128],
                                           qkT[i * 64:(i + 1) * 64, hd, q0:q0 + qn_],
                                           [qkT.s(kc)] + [qkT.s(q0 // 128 + u_) for u_ in range(qn_ // 128)], [p_])
                                        ACT(pTb[i][:, kc, 0:qn_], p_[:, 0:qn_], AF.Exp, [p_], [pTb[i].s(kc)], scale=0.125)
                                    ops_.append(f)
                                return ops_

                            def v_ops(u):
                                hd, (q0, qn_, kcs), i = u
                                ops_ = []
                                for tq in range(qn_ // 128):
                                    po = ps_o[i][tq // 2]
                                    c0 = (tq % 2) * 256
                                    for n_, kc in enumerate(kcs):
                                        def f(tq=tq, po=po, c0=c0, n_=n_, kc=kc):
                                            MM(po[:, c0:c0 + 129], pTb[i][:, kc, tq * 128:(tq + 1) * 128], v_aug[:, kc, hd, 0:129],
                                               [pTb[i].s(kc)] + vall, [po], start=(n_ == 0), stop=(n_ == len(kcs) - 1))
                                        ops_.append(f)

                                    def g(tq=tq, po=po, c0=c0):
                                        CP("dve", oraw[i][tq][:, 0:129], po[:, c0:c0 + 129], [po], [oraw[i][tq]])
                                        if i == 1:
                                            tt = q0 // 128 + tq
                                            k2 = cnt_["comb"] % 2
                                            cnt_["comb"] += 1
                                            s_ = sc_[k2]
                                            o_ = ob[k2]
                                            o0, o1 = oraw[0][tq], oraw[1][tq]
                                            RCP(s_[:, 0:1], o0[:, 128:129], [o0], [s_])
                                            RCP(s_[:, 1:2], o1[:, 128:129], [o1], [s_])
                                            TT("dve", s_[:, 2:3], s_[:, 1:2], nlam[:, 3:4], ALU.mult, [s_, nlam], [s_])
                                            TS("dve", o_[:, 0, :], o0[:, 0:128], s_[:, 0:1], None, ALU.mult, None, [o0, s_], [o_])
                                            STT(o_[:, 1, :], o1[:, 0:128], s_[:, 2:3], o_[:, 0, :], ALU.mult, ALU.add, [o1, s_, o_], [o_])
                                            ACT(junk2[:], o_[:, 1, :], AF.Square, [o_], [junk2, s_], accum_out=s_[:, 3:4])
                                            ACT(s_[:, 4:5], s_[:, 3:4], AF.Sqrt, [s_], [s_], scale=1.0 / 128, bias=EPS)
                                            RCP(s_[:, 5:6], s_[:, 4:5], [s_], [s_])
                                            STT(mixtok[:, tt, hd * 128:(hd + 1) * 128], o_[:, 1, :], s_[:, 5:6], subw[:], ALU.mult, ALU.mult,
                                                [o_, s_, subw], [mixtok.s(tt)])
                                    ops_.append(g)
                                return ops_

                            prev = []
                            for u in units + [None]:
                                cur = s_ops(u) if u is not None else []
                                ratio = (len(prev) + max(len(cur), 1) - 1) // max(len(cur), 1)
                                pi_ = 0
                                for f in cur:
                                    f()
                                    for _ in range(ratio):
                                        if pi_ < len(prev):
                                            prev[pi_]()
                                            pi_ += 1
                                while pi_ < len(prev):
                                    prev[pi_]()
                                    pi_ += 1
                                prev = v_ops(u) if u is not None else []
                            if l == 0 and b == 0:
                                dump("mixattn", [mixtok.s(t_) for t_ in tiles], mixtok[:, :, 0:512], [128, NT, 512], BF16)

                with S.scope():
                    BCT = S.sb("BCT", [128, 4, T], BF16)
                    xsB = S.sb("xsB", [128, NT, 768], BF16)
                    sz = S.sb("sz", [128, NT, 512], BF16)
                    dtt = S.sb("dtt", [128, NT, 16], F32)
                    dA = S.sb("dA", [128, NT, 16], F32)
                    dsk = S.sb("dsk", [128, 8], F32)
                    ssdw = S.sb("ssdw", [128, 512], F32)
                    DMA("sp", dsk[:], din["d_skip"][l:l + 1, :].to_broadcast([128, 8]), [], [dsk])
                    DMA("sp", ssdw[:], din["ssd_norm_w"][l:l + 1, :].to_broadcast([128, 512]), [], [ssdw])
                    with S.scope():
                        hT = S.sb("hT2", [128, 8, T], BF16)
                        DMA("sp", hT[:], hTd, [hTd_b], [hT.s(t_) for t_ in tiles])
                        hall = [hT.s(t_) for t_ in tiles]
                        with S.scope():
                            wz = S.sb("wz", [128, 8, 528], BF16)
                            DMA("pool", wz[:, :, 0:512], din["w_in"][l, :, 1536:2048].rearrange("(c p) n -> p c n", p=128), [], [wz])
                            DMA("pool", wz[:, :, 512:528], din["w_in"][l, :, 3072:3088].rearrange("(c p) n -> p c n", p=128), [], [wz])
                            dtb = S.sb("dtb", [128, 16], F32)
                            abc = S.sb("abc", [128, 16], F32)
                            DMA("sp", dtb[:, 0:8], din["dt_bias_f"][l:l + 1, :].to_broadcast([128, 8]), [], [dtb])
                            DMA("sp", dtb[:, 8:16], din["dt_bias_b"][l:l + 1, :].to_broadcast([128, 8]), [], [dtb])
                            DMA("sp", abc[:, 0:8], din["a_log_f"][l:l + 1, :].to_broadcast([128, 8]), [], [abc])
                            DMA("sp", abc[:, 8:16], din["a_log_b"][l:l + 1, :].to_broadcast([128, 8]), [], [abc])
                            ACT(abc[:], abc[:], AF.Exp, [abc], [abc])
                            TS("dve", abc[:], abc[:], -1.0, None, ALU.mult, None, [abc], [abc])
                            ps_z = [S.ps("ps_z%d" % i, [128, 512]) for i in range(2)]
                            ps_dt = [S.ps("ps_dt%d" % i, [128, 16]) for i in range(2)]
                            for tt in tiles:
                                k2 = tt % 2
                                for c_ in range(8):
                                    MM(ps_z[k2][:], hT[:, c_, tt * 128:(tt + 1) * 128], wz[:, c_, 0:512], [hT.s(tt), wz], [ps_z[k2]],
                                       start=(c_ == 0), stop=(c_ == 7))
                                for c_ in range(8):
                                    MM(ps_dt[k2][:], hT[:, c_, tt * 128:(tt + 1) * 128], wz[:, c_, 512:528], [hT.s(tt), wz], [ps_dt[k2]],
                                       start=(c_ == 0), stop=(c_ == 7))
                                ACT(sz[:, tt, :], ps_z[k2][:], AF.Silu, [ps_z[k2]], [sz.s(tt)])
                                TT("dve", dtt[:, tt, :], ps_dt[k2][:], dtb[:], ALU.add, [ps_dt[k2], dtb], [dtt])
                            ACT(dtt[:], dtt[:], AF.Exp, [dtt], [dtt])
                            ACT(dtt[:], dtt[:], AF.Ln, [dtt], [dtt], bias=1.0)
                            TT("dve", dA[:], dtt[:], abc[:].unsqueeze(1).to_broadcast([128, NT, 16]), ALU.mult, [dtt, abc], [dA])
                        with S.scope():
                            wx = S.sb("wx", [128, 8, 1024], BF16)
                            DMA("pool", wx[:], din["w_in"][l, :, 2048:3072].rearrange("(c p) n -> p c n", p=128), [], [wx])
                            cw = S.sb("cw", [128, 8, 3], F32)
                            cbv = S.sb("cbv", [128, 8], F32)
                            for k_ in range(3):
                                DMA("sp", cw[:, :, k_], din["conv_w"][l, k_].rearrange("(c p) -> p c", p=128), [], [cw])
                            DMA("sp", cbv[:], din["conv_b"][l].rearrange("(c p) -> p c", p=128), [], [cbv])
                            raw = [S.sb("raw%d" % i, [128, T + 4], F32) for i in range(1)]
                            acc = S.sb("acc", [128, T], F32)
                            fmb = S.sb("fmb", [128, T], BF16)
                            ps_x = [S.ps("ps_x%d" % i, [128, 512]) for i in range(2)]
                            pT4 = [S.ps("pT4_%d" % i, [128, 4, 128], BF16) for i in range(2)]
                            MS("pool", raw[0][:], 0.0, [raw[0]])
                            npx = 0
                            ntr = 0
                            segs = [(0, 256, 1), (256, 512, 259), (768, 512, 259 + 512), (1280, 512, 259 + 1024), (1792, 512, 259 + 1536)]
                            for ch in range(8):
                                r_ = raw[0]
                                for (c0, cn, ro) in segs:
                                    p_ = ps_x[npx % 2]
                                    npx += 1
                                    for c_ in range(8):
                                        MM(p_[:, 0:cn], wx[:, c_, ch * 128:(ch + 1) * 128], hT[:, c_, c0:c0 + cn],
                                           [wx] + [hT.s(c0 // 128 + u) for u in range(cn // 128)], [p_], start=(c_ == 0), stop=(c_ == 7))
                                    CP("act", r_[:, ro:ro + cn], p_[:, 0:cn], [p_], [r_])
                                for (a0, an, ro) in [(0, 256, 1), (256, 2048, 259)]:
                                    TS("pool", acc[:, a0:a0 + an], r_[:, ro:ro + an], cw[:, ch, 1:2], cbv[:, ch:ch + 1], ALU.mult, ALU.add,
                                       [r_, cw, cbv], [acc])
                                    STT(acc[:, a0:a0 + an], r_[:, ro - 1:ro - 1 + an], cw[:, ch, 0:1], acc[:, a0:a0 + an], ALU.mult, ALU.add,
                                        [r_, cw, acc], [acc])
                                    STT(acc[:, a0:a0 + an], r_[:, ro + 1:ro + 1 + an], cw[:, ch, 2:3], acc[:, a0:a0 + an], ALU.mult, ALU.add,
                                        [r_, cw, acc], [acc])
                                if ch >= 4:
                                    ACT(BCT[:, ch - 4, :], acc[:], AF.Silu, [acc], [BCT.s(ch - 4)])
                                    src_t, src_b = BCT, BCT.s(ch - 4)
                                    src_ap = lambda c0_, cn_: BCT[:, ch - 4, c0_:c0_ + cn_]
                                else:
                                    ACT(fmb[:], acc[:], AF.Silu, [acc], [fmb])
                                    src_b = fmb._buf
                                    src_ap = lambda c0_, cn_: fmb[:, c0_:c0_ + cn_]
                                if ch < 6:
                                    for t4 in range(0, NT, 4):
                                        n4 = min(4, NT - t4)
                                        p4 = pT4[ntr % 2]
                                        ntr += 1
                                        for u in range(n4):
                                            TR(p4[:, u, :], src_ap((t4 + u) * 128, 128), ident_b[:], [src_b, ident_b], [p4])
                                        CP("dve", xsB[:, t4:t4 + n4, ch * 128:(ch + 1) * 128], p4[:, 0:n4, :], [p4], [xsB])
                            if l == 0 and b == 0:
                                dump("BCT", [BCT.s(i) for i in range(4)], BCT[:], [128, 4, T], BF16)
                                dump("xsB", [xsB], xsB[:], [128, NT, 768], BF16)
                                dump("dtt", [dtt], dtt[:], [128, NT, 16], F32)

                    with S.scope():
                        yss = S.sb("yss", [128, NT, 512], F32)
                        mneg = S.sb("mneg", [128, 2, 512], F32)
                        DMA("sp", mneg[:], din["mneg"].rearrange("a p l -> p a l"), [], [mneg])
                        H = [S.sb("H%d" % i, [128, 8, 64], F32) for i in range(2)]
                        Hb = [S.sb("Hb%d" % i, [128, 8, 64], BF16) for i in range(2)]
                        dAtri_ = [S.sb("dAtri%d" % i, [128, 8, 128], F32) for i in range(2)]
                        negdA_ = [S.sb("negdA%d" % i, [128, 8, 128], F32) for i in range(2)]
                        dec_ = [S.sb("dec%d" % i, [128, 8, 128], F32) for i in range(2)]
                        Mt_ = [S.sb("Mt%d" % i, [128, 8, 128], BF16) for i in range(2)]
                        sm_ = [S.sb("sm%d" % i, [128, 48], F32) for i in range(2)]
                        xdt_ = [S.sb("xdt%d" % i, [128, 8, 64], BF16) for i in range(2)]
                        xdte_ = [S.sb("xdte%d" % i, [128, 8, 64], BF16) for i in range(2)]
                        tmpy_ = [S.sb("tmpy%d" % i, [128, 8, 64], F32) for i in range(2)]
                        tmpH_ = [S.sb("tmpH%d" % i, [128, 8, 64], F32) for i in range(2)]
                        tmpy2 = S.sb("tmpy2", [128, 8, 64], F32)
                        ps_ca_ = [S.ps("ps_ca%d" % i, [128, 512]) for i in range(2)]
                        ps_seg_ = [S.ps("ps_seg%d" % i, [128, 512]) for i in range(2)]
                        ps_y = S.ps("ps_y", [128, 512])
                        ps_yo = S.ps("ps_yo", [128, 512])
                        ps_st = S.ps("ps_st", [128, 512])
                        orders = [list(range(NT)), [1, 0] + list(range(NT - 1, 1, -1))]
                        for d in range(2):
                            MS("dve", H[d][:], 0.0, [H[d]])
                            MS("dve", Hb[d][:], 0.0, [Hb[d]])
                        ywritten = set()
                        for step in range(NT):
                            for d in range(2):
                                tt = orders[d][step]
                                dAtri, negdA, dec, Mt, sm, xdt, xdte, tmpy, tmpH = (dAtri_[d], negdA_[d], dec_[d], Mt_[d], sm_[d], xdt_[d],
                                                                                    xdte_[d], tmpy_[d], tmpH_[d])
                                ps_ca, ps_seg = ps_ca_[d], ps_seg_[d]
                                cs = slice(tt * 128, (tt + 1) * 128)
                                dAd = dA[:, tt, d * 8:(d + 1) * 8]
                                dtd = dtt[:, tt, d * 8:(d + 1) * 8]
                                for g in range(2):
                                    MM(ps_ca[:, g * 128:(g + 1) * 128], BCT[:, g, cs], BCT[:, 2 + g, cs], [BCT.s(g), BCT.s(2 + g)], [ps_ca])
                                MM(ps_ca[:, 256:264], tri[:, d, :], dAd, [tri, dA], [ps_ca])
                                MM(ps_ca[:, 264:272], ones_f[:], dAd, [ones_f, dA], [ps_ca])
                                TT("pool", dAtri[:], tri[:, d, :].unsqueeze(1).to_broadcast([128, 8, 128]),
                                   dAd.unsqueeze(2).to_broadcast([128, 8, 128]), ALU.mult, [tri, dA], [dAtri])
                                ACT(negdA[:], dAd.unsqueeze(2).to_broadcast([128, 8, 128]), AF.Copy, [dA], [negdA], scale=-1.0)
                                for hh in range(2):
                                    MM(ps_seg[:], ones_f[:], dAtri[:, hh * 4:(hh + 1) * 4, :].rearrange("p a b -> p (a b)"), [ones_f, dAtri], [ps_seg],
                                       start=True, stop=False)
                                    MM(ps_seg[:], tri[:, d, :], negdA[:, hh * 4:(hh + 1) * 4, :].rearrange("p a b -> p (a b)"), [tri, negdA], [ps_seg],
                                       start=False, stop=False)
                                    MM(ps_seg[:], ident_f[:], mneg[:, d, :], [ident_f, mneg], [ps_seg], start=False, stop=True)
                                    ACT(dec[:, hh * 4:(hh + 1) * 4, :].rearrange("p a b -> p (a b)"), ps_seg[:], AF.Exp, [ps_seg], [dec])
                                TT("dve", Mt[:].rearrange("p (g r) l -> p g r l", g=2), dec[:].rearrange("p (g r) l -> p g r l", g=2),
                                   ps_ca[:, 0:256].rearrange("p (g l) -> p g l", g=2).unsqueeze(2).to_broadcast([128, 2, 4, 128]), ALU.mult,
                                   [dec, ps_ca], [Mt])
                                ACT(sm[:, 0:16], ps_ca[:, 256:272], AF.Exp, [ps_ca], [sm])
                                CP("act", sm[:, 32:48], ps_ca[:, 256:272], [ps_ca], [sm])
                                TT("dve", sm[:, 16:24], sm[:, 40:48], sm[:, 32:40], ALU.subtract, [sm], [sm])
                                ACT(sm[:, 16:24], sm[:, 16:24], AF.Exp, [sm], [sm])
                                TT("dve", sm[:, 24:32], sm[:, 16:24], dtd, ALU.mult, [sm, dtt], [sm])
                                xs3 = xsB[:, tt, 0:512].rearrange("p (h e) -> p h e", h=8)
                                TT("dve", xdt[:], xs3, dtd.unsqueeze(2).to_broadcast([128, 8, 64]), ALU.mult, [xsB, dtt], [xdt])
                                TT("dve", xdte[:], xs3, sm[:, 24:32].unsqueeze(2).to_broadcast([128, 8, 64]), ALU.mult, [xsB, sm], [xdte])
                                for h_ in range(8):
                                    MM(ps_y[:, h_ * 64:(h_ + 1) * 64], Mt[:, h_, :], xdt[:, h_, :], [Mt, xdt], [ps_y])
                                for g in range(2):
                                    MM(ps_yo[:, g * 256:(g + 1) * 256], BCT[:, 2 + g, cs], Hb[d][:, g * 4:(g + 1) * 4, :].rearrange("p a b -> p (a b)"),
                                       [BCT.s(2 + g), Hb[d]], [ps_yo])
                                for g in range(2):
                                    MM(ps_st[:, g * 256:(g + 1) * 256], xsB[:, tt, 512 + g * 128:512 + (g + 1) * 128],
                                       xdte[:, g * 4:(g + 1) * 4, :].rearrange("p a b -> p (a b)"), [xsB, xdte], [ps_st])
                                TT("dve", tmpy[:], ps_yo[:].rearrange("p (h e) -> p h e", h=8), sm[:, 0:8].unsqueeze(2).to_broadcast([128, 8, 64]),
                                   ALU.mult, [ps_yo, sm], [tmpy])
                                if tt not in ywritten:
                                    ywritten.add(tt)
                                    TT("dve", yss[:, tt, :], ps_y[:], tmpy[:].rearrange("p a b -> p (a b)"), ALU.add, [ps_y, tmpy], [yss.s(tt)])
                                else:
                                    TT("dve", tmpy[:].rearrange("p a b -> p (a b)"), ps_y[:], tmpy[:].rearrange("p a b -> p (a b)"), ALU.add,
                                       [ps_y, tmpy], [tmpy])
                                    TT("pool", yss[:, tt, :], yss[:, tt, :], tmpy[:].rearrange("p a b -> p (a b)"), ALU.add,
                                       [yss.s(tt), tmpy], [yss.s(tt)])
                                TT("dve", tmpH[:], H[d][:], sm[:, 8:16].unsqueeze(2).to_broadcast([128, 8, 64]), ALU.mult, [H[d], sm], [tmpH])
                                TT("dve", H[d][:].rearrange("p a b -> p (a b)"), tmpH[:].rearrange("p a b -> p (a b)"), ps_st[:], ALU.add,
                                   [tmpH, ps_st], [H[d]])
                                CP("act", Hb[d][:], H[d][:], [H[d]], [Hb[d]])
                        tmpy = tmpy_[0]
                        gg = S.sb("gg", [128, 512], F32)
                        junk3 = S.sb("junk3", [128, 256], BF16)
                        gs = S.sb("gs", [128, 8], F32)
                        for tt in tiles:
                            xs3 = xsB[:, tt, 0:512].rearrange("p (h e) -> p h e", h=8)
                            TT("pool", tmpy[:], xs3, dsk[:].unsqueeze(2).to_broadcast([128, 8, 64]), ALU.mult, [xsB, dsk], [tmpy])
                            TT("dve", tmpy2[:].rearrange("p a b -> p (a b)"), yss[:, tt, :], tmpy[:].rearrange("p a b -> p (a b)"), ALU.add,
                               [yss.s(tt), tmpy], [tmpy2])
                            TT("dve", gg[:], tmpy2[:].rearrange("p a b -> p (a b)"), sz[:, tt, :], ALU.mult, [tmpy2, sz.s(tt)], [gg])
                            for g in range(2):
                                ACT(junk3[:], gg[:, g * 256:(g + 1) * 256], AF.Square, [gg], [junk3, gs], accum_out=gs[:, g:g + 1])
                            ACT(gs[:, 2:4], gs[:, 0:2], AF.Sqrt, [gs], [gs], scale=1.0 / 256, bias=EPS)
                            RCP(gs[:, 4:6], gs[:, 2:4], [gs], [gs])
                            for g in range(2):
                                STT(mixtok[:, tt, 512 + g * 256:512 + (g + 1) * 256], gg[:, g * 256:(g + 1) * 256], gs[:, 4 + g:5 + g],
                                    ssdw[:, g * 256:(g + 1) * 256], ALU.mult, ALU.mult, [gg, gs, ssdw], [mixtok.s(tt)])
                        if l == 0 and b == 0:
                            dump("mixssd", [mixtok.s(t_) for t_ in tiles], mixtok[:, :, 512:1024], [128, NT, 512], BF16)
                            dump("yss", [yss.s(t_) for t_ in tiles], yss[:], [128, NT, 512], F32)

                with S.scope():
                    wo = S.sb("wo", [128, 8, D], BF16)
                    DMA("pool", wo[:], din["w_out"][l].rearrange("(c p) n -> p c n", p=128), [], [wo])
                    wr = S.sb("wr", [128, 8, NE], F32)
                    DMA("sp", wr[:], din["w_router"][l].rearrange("(c p) n -> p c n", p=128), [], [wr])
                    modG = S.sb("modG", [128, 2, D], F32)
                    modA = S.sb("modA2", [128, 2, D], F32)
                    modS = S.sb("modS2", [128, 2, D], F32)
                    nw = S.sb("nw2", [128, D], F32)
                    load_mod(modG, 2)
                    load_mod(modS, 3)
                    load_mod(modA, 4)
                    DMA("sp", nw[:], din["norm2_w"][l:l + 1, :].to_broadcast([128, D]), [], [nw])
                    for j in range(2):
                        STT(modA[:, j, :], modA[:, j, :], 1.0, nw[:], ALU.add, ALU.mult, [modA, nw], [modA])
                    mixT = [S.sb("mixT%d" % i, [128, 8, 128], BF16) for i in range(2)]
                    xin = [S.sb("xin2_%d" % i, [128, D], F32) for i in range(2)]
                    xnew = [S.sb("xnew%d" % i, [128, D], F32) for i in range(2)]
                    tg_ = [S.sb("tg%d" % i, [128, D], F32) for i in range(2)]
                    t1_ = [S.sb("t1b%d" % i, [128, D], F32) for i in range(2)]
                    h2f_ = [S.sb("h2f%d" % i, [128, D], F32) for i in range(2)]
                    h2fT_ = [S.sb("h2fT%d" % i, [128, 8, 128], F32) for i in range(2)]
                    junk_ = [S.sb("junk4_%d" % i, [128, D], BF16) for i in range(2)]
                    st = [S.sb("st2_%d" % i, [128, 4], F32) for i in range(2)]
                    rt_ = [S.sb("rt%d" % i, [128, 40], F32) for i in range(2)]
                    att = [S.sb("att%d" % i, [NE, 128], F32) for i in range(2)]
                    pT8_ = [S.ps("pT8o%d" % i, [128, 8, 128], BF16) for i in range(2)]
                    ps_out = S.ps("ps_out", [128, 1024])
                    ps_hT = S.ps("ps_hT", [128, 8, 128], F32)
                    ps_la_ = [S.ps("ps_la%d" % i, [128, 512]) for i in range(2)]
                    ptiles = [t_ for t_ in tiles if not (last and t_ < 2)]
                    for tt in ptiles:
                        k2 = tt % 2
                        j = 1 if tt < 2 else 0
                        tg, t1, h2f, h2fT, junk, rt, pT8 = tg_[k2], t1_[k2], h2f_[k2], h2fT_[k2], junk_[k2], rt_[k2], pT8_[k2]
                        ps_lg = ps_la_[k2]
                        for c_ in range(8):
                            TR(pT8[:, c_, :], mixtok[:, tt, c_ * 128:(c_ + 1) * 128], ident_b[:], [mixtok.s(tt), ident_b], [pT8])
                        CP("act", mixT[k2][:], pT8[:], [pT8], [mixT[k2]])
                        for half in range(2):
                            for c_ in range(8):
                                MM(ps_out[:, half * 512:(half + 1) * 512], mixT[k2][:, c_, :], wo[:, c_, half * 512:(half + 1) * 512],
                                   [mixT[k2], wo], [ps_out], start=(c_ == 0), stop=(c_ == 7))
                        src, sb_ = xsrc(tt)
                        DMA("sp", xin[k2][:], src, sb_, [xin[k2]])
                        TT("dve", tg[:], ps_out[:], modG[:, j, :], ALU.mult, [ps_out, modG], [tg])
                        TT("pool", xnew[k2][:], tg[:], xin[k2][:], ALU.add, [tg, xin[k2]], [xnew[k2]])
                        DMA("sp", xres[tt * 128:(tt + 1) * 128, :], xnew[k2][:], [xnew[k2]], [xres_b[tt]])
                        ACT(junk[:], xnew[k2][:], AF.Square, [xnew[k2]], [junk, st[k2]], accum_out=st[k2][:, 0:1])
                        ACT(st[k2][:, 1:2], st[k2][:, 0:1], AF.Sqrt, [st[k2]], [st[k2]], scale=1.0 / D, bias=EPS)
                        RCP(st[k2][:, 2:3], st[k2][:, 1:2], [st[k2]], [st[k2]])
                        STT(t1[:], xnew[k2][:], st[k2][:, 2:3], modA[:, j, :], ALU.mult, ALU.mult, [xnew[k2], st[k2], modA], [t1])
                        TT("pool", h2f[:], t1[:], modS[:, j, :], ALU.add, [t1, modS], [h2f])
                        CP("act", tokbuf[:, tt, :], h2f[:], [h2f], [tokbuf.s(tt)])
                        for c_ in range(8):
                            TR(ps_hT[:, c_, :], h2f[:, c_ * 128:(c_ + 1) * 128], ident_f[:], [h2f, ident_f], [ps_hT])
                        CP("dve", h2fT[:], ps_hT[:], [ps_hT], [h2fT])
                        for c_ in range(8):
                            MM(ps_lg[:, 0:16], h2fT[:, c_, :], wr[:, c_, :], [h2fT, wr], [ps_lg], start=(c_ == 0), stop=(c_ == 7))
                        RED(rt[:, 0:1], ps_lg[:, 0:16], [ps_lg], [rt], op=ALU.max)
                        TS("dve", rt[:, 1:2], rt[:, 0:1], -1.0, None, ALU.mult, None, [rt], [rt])
                        ACT(rt[:, 8:24], ps_lg[:, 0:16], AF.Exp, [ps_lg, rt], [rt], bias=rt[:, 1:2], accum_out=rt[:, 2:3])
                        RCP(rt[:, 3:4], rt[:, 2:3], [rt], [rt])
                        TS("dve", rt[:, 24:40], rt[:, 8:24], rt[:, 3:4], None, ALU.mult, None, [rt], [rt])
                        TR(ps_lg[0:NE, 128:256], rt[:, 24:40], ident_f[:], [rt, ident_f], [ps_lg])
                        CP("act", att[k2][:], ps_lg[0:NE, 128:256], [ps_lg], [att[k2]])
                        DMA("sp", affd[:, tt * 128:(tt + 1) * 128], att[k2][:], [att[k2]], [affd_b])
                    if l == 0 and b == 0:
                        dump("h2tok", [tokbuf.s(t_) for t_ in ptiles], tokbuf[:], [128, NT, 1024], BF16)
                        dump("affT", [affd_b], affd, [NE, T], F32)
                        dump("xres", [xres_b[t_] for t_ in ptiles], xres, [T, D], F32)
                if moe:
                    nslot = CAP if last else NSLOT
                    cchunks = [(0, 128), (128, 128)] + ([] if last else [(256, CAPC)])
                    ltiles = list(range(2, NT))
                    with S.scope():
                        vals = S.sb("vals", [NE, NSLOT], F32)
                        idxu = S.sb("idxu", [NE, NSLOT], U32)
                        idxf = S.sb("idxf", [NE, NSLOT], F32)
                        valsT = S.sb("valsT", [128, 3, NE], F32)
                        idxT = S.sb("idxT", [128, 3, NE], F32)
                        with S.scope():
                            affw = S.sb("affw", [NE, T], F32)
                            DMA("sp", affw[:], affd, [affd_b], [affw])
                            segs_k = [(256, SEQ, 0, CAP // 8)] + ([] if last else [(0, CTX, CAP, CAPC // 8)])
                            for (a0, an, s0, nr) in segs_k:
                                src = affw[:, a0:a0 + an]
                                for r in range(nr):
                                    vs = vals[:, s0 + r * 8:s0 + (r + 1) * 8]
                                    S.op("dve", lambda e, vs=vs, src=src: e.max(out=vs, in_=src), [affw], [vals])
                                    ix = idxu[:, s0 + r * 8:s0 + (r + 1) * 8]
                                    S.op("dve", lambda e, ix=ix, vs=vs, src=src: e.max_index(out=ix, in_max=vs, in_values=src),
                                         [affw, vals], [idxu])
                                    if r < nr - 1:
                                        S.op("dve", lambda e, vs=vs, src=src: e.match_replace(out=src, in_to_replace=vs, in_values=src,
                                                                                             imm_value=-1.0), [affw, vals], [affw])
                            CP("dve", idxf[:, 0:nslot], idxu[:, 0:nslot], [idxu], [idxf])
                            ps_t = S.ps("ps_t", [128, 2, 3, NE])
                            for ck, (c0, cn) in enumerate(cchunks):
                                TR(ps_t[0:cn, 0, ck, :], vals[:, c0:c0 + cn], ident_f[0:NE, 0:NE], [vals, ident_f], [ps_t])
                                TR(ps_t[0:cn, 1, ck, :], idxf[:, c0:c0 + cn], ident_f[0:NE, 0:NE], [idxf, ident_f], [ps_t])
                                CP("dve", valsT[0:cn, ck, :], ps_t[0:cn, 0, ck, :], [ps_t], [valsT])
                                CP("dve", idxT[0:cn, ck, :], ps_t[0:cn, 1, ck, :], [ps_t], [idxT])
                            if l == 0 and b == 0:
                                dump("vals", [vals], vals[:], [NE, NSLOT], F32)
                                dump("idxf", [idxf], idxf[:], [NE, NSLOT], F32)
                        yacc = S.sb("yacc", [128, NT, 1024], F32)
                        yall = [yacc.s(t_) for t_ in tiles]
                        MS("pool", yacc[:], 0.0, yall)
                        with S.scope():
                            ring = [S.sb("wring%d" % i, [128, 3072], BF16) for i in range(6)]
                            Ssel = S.sb("Ssel", [128, NT, NSLOT], BF16)
                            STl = S.sb("STl", [128, 2, SEQ], BF16)
                            STc = S.sb("STc", [CAPC, CTX], BF16)
                            iota = S.sb("iota", [128, SEQ], F32)
                            DMA("sp", iota[:], din["iota"], [], [iota])
                            xeT = S.sb("xeT", [128, 8, NSLOT], BF16)
                            hid = S.sb("hid", [128, NF, NSLOT], BF16)
                            sgt = S.sb("sgt", [128, NSLOT], F32)
                            yeg = S.sb("yeg", [128, 3, 1024], BF16)
                            idm = S.sb("idm", [NE, NSLOT], F32)
                            ps_ab = [S.ps("ps_ab%d" % i, [128, 512]) for i in range(2)]
                            ps_gt = S.ps("ps_gt", [128, 512])
                            ps_up = S.ps("ps_up", [128, 512])
                            ps_d = [S.ps("ps_d%d" % i, [128, 512]) for i in range(2)]
                            ps_sc = [S.ps("ps_sc%d" % i, [128, 512]) for i in range(2)]
                            fgroups = [(0, 3), (3, 3), (6, 3), (9, 2)]
                            nring = 0
                            nab = 0
                            nd = 0
                            nsc = 0
                            hall_ = [tokbuf.s(t_) for t_ in tiles]
                            pf = {"issued": 0, "consumed": 0}

                            def issue_piece(k):
                                e2, r2 = divmod(k, 12)
                                r_ = ring[k % 6]
                                if r2 < 8:
                                    gi = r2 // 2
                                    nm = "w_up" if (r2 % 2) else "w_gate"
                                    f0, nf = fgroups[gi]
                                    DMA("pool", r_[:, 0:8 * nf * 128].rearrange("p (c n) -> p c n", c=8),
                                        din[nm][l, e2, :, f0 * 128:(f0 + nf) * 128].rearrange("(c p) n -> p c n", p=128), [], [r_])
                                else:
                                    f0, nf = fgroups[r2 - 8]
                                    DMA("pool", r_[:, 0:nf * 1024].rearrange("p (c n) -> p c n", c=nf),
                                        din["w_down"][l, e2, f0 * 128:(f0 + nf) * 128, :].rearrange("(c p) n -> p c n", p=128), [], [r_])

                            def release(n):
                                pf["consumed"] += n
                                while pf["issued"] < pf["consumed"] + 6 and pf["issued"] < NE * 12:
                                    issue_piece(pf["issued"])
                                    pf["issued"] += 1

                            release(0)
                            for e_ in range(NE):
                                pieces = {}
                                for gi in range(4):
                                    for nm in ("w_gate", "w_up"):
                                        pieces[(nm, gi)] = ring[(e_ * 12 + gi * 2 + (nm == "w_up")) % 6]
                                TS("dve", idm[:, 0:nslot], idxf[:, 0:nslot], ident_f[0:NE, e_:e_ + 1], None, ALU.mult, None, [idxf, ident_f], [idm])
                                pi = ps_ab[nab % 2]
                                nab += 1
                                MM(pi[:, 0:nslot], ones_f[0:NE, :], idm[:, 0:nslot], [ones_f, idm], [pi])
                                for j in range(16):
                                    TS("dve", Ssel[:, 2 + j, 0:CAP], pi[:, 0:CAP], tpos[:, j:j + 1], None, ALU.is_equal, None, [pi, tpos], [Ssel])
                                if not last:
                                    for j in range(2):
                                        TS("dve", Ssel[:, j, CAP:NSLOT], pi[:, CAP:NSLOT], tpos[:, j:j + 1], None, ALU.is_equal, None,
                                           [pi, tpos], [Ssel])
                                for ck in range(2):
                                    TS("dve", STl[:, ck, :], iota[:], idxT[:, ck, e_:e_ + 1], None, ALU.is_equal, None, [iota, idxT], [STl])
                                if not last:
                                    TS("dve", STc[:], iota[0:CAPC, 0:CTX], idxT[0:CAPC, 2, e_:e_ + 1], None, ALU.is_equal, None, [iota, idxT], [STc])
                                for dc in range(8):
                                    pg = ps_ab[nab % 2]
                                    nab += 1
                                    for j in range(16):
                                        MM(pg[:, 0:CAP], tokbuf[:, 2 + j, dc * 128:(dc + 1) * 128], Ssel[:, 2 + j, 0:CAP],
                                           [tokbuf.s(2 + j), Ssel], [pg], start=(j == 0), stop=(j == 15))
                                    if not last:
                                        for j in range(2):
                                            MM(pg[:, CAP:NSLOT], tokbuf[:, j, dc * 128:(dc + 1) * 128], Ssel[:, j, CAP:NSLOT],
                                               [tokbuf.s(j), Ssel], [pg], start=(j == 0), stop=(j == 1))
                                    CP("act", xeT[:, dc, 0:nslot], pg[:, 0:nslot], [pg], [xeT])
                                for gi, (f0, nf) in enumerate(fgroups):
                                    wg_ = pieces[("w_gate", gi)]
                                    wu_ = pieces[("w_up", gi)]
                                    wg3 = wg_[:, 0:8 * nf * 128].rearrange("p (c n) -> p c n", c=8)
                                    wu3 = wu_[:, 0:8 * nf * 128].rearrange("p (c n) -> p c n", c=8)
                                    for fi in range(nf):
                                        f = f0 + fi
                                        for c_ in range(8):
                                            MM(ps_gt[:, 0:nslot], wg3[:, c_, fi * 128:(fi + 1) * 128], xeT[:, c_, 0:nslot], [wg_, xeT], [ps_gt],
                                               start=(c_ == 0), stop=(c_ == 7))
                                        for c_ in range(8):
                                            MM(ps_up[:, 0:nslot], wu3[:, c_, fi * 128:(fi + 1) * 128], xeT[:, c_, 0:nslot], [wu_, xeT], [ps_up],
                                               start=(c_ == 0), stop=(c_ == 7))
                                        ACT(sgt[:, 0:nslot], ps_gt[:, 0:nslot], AF.Silu, [ps_gt], [sgt])
                                        TT("dve", hid[:, f, 0:nslot], sgt[:, 0:nslot], ps_up[:, 0:nslot], ALU.mult, [sgt, ps_up], [hid])
                                    release(2)
                                dps = [ring[(e_ * 12 + 8 + gi) % 6] for gi in range(4)]
                                for ck, (c0, cn) in enumerate(cchunks):
                                    for half in range(2):
                                        pd = ps_d[nd % 2]
                                        nd += 1
                                        for gi, (f0, nf) in enumerate(fgroups):
                                            w3 = dps[gi][:, 0:nf * 1024].rearrange("p (c n) -> p c n", c=nf)
                                            for fi in range(nf):
                                                f = f0 + fi
                                                MM(pd[0:cn, :], hid[:, f, c0:c0 + cn], w3[:, fi, half * 512:(half + 1) * 512], [hid, dps[gi]], [pd],
                                                   start=(f == 0), stop=(f == NF - 1))
                                        ACT(yeg[0:cn, ck, half * 512:(half + 1) * 512], pd[0:cn, :], AF.Copy, [pd, valsT], [yeg],
                                            scale=valsT[0:cn, ck, e_:e_ + 1])
                                release(4)
                                for j in range(16):
                                    for half in range(2):
                                        pq = ps_sc[nsc % 2]
                                        nsc += 1
                                        for ck in range(2):
                                            MM(pq[:], STl[:, ck, j * 128:(j + 1) * 128], yeg[:, ck, half * 512:(half + 1) * 512], [STl, yeg], [pq],
                                               start=(ck == 0), stop=(ck == 1))
                                        ya = yacc[:, 2 + j, half * 512:(half + 1) * 512]
                                        TT("dve", ya, pq[:], ya, ALU.add, [pq, yacc.s(2 + j)], [yacc.s(2 + j)])
                                if not last:
                                    for j in range(2):
                                        for half in range(2):
                                            pq = ps_sc[nsc % 2]
                                            nsc += 1
                                            MM(pq[:], STc[:, j * 128:(j + 1) * 128], yeg[0:CAPC, 2, half * 512:(half + 1) * 512], [STc, yeg], [pq])
                                            ya = yacc[:, j, half * 512:(half + 1) * 512]
                                            TT("dve", ya, pq[:], ya, ALU.add, [pq, yacc.s(j)], [yacc.s(j)])
                            if l == 0 and b == 0:
                                dump("yacc", yall, yacc[:], [128, NT, 1024], F32)
                        with S.scope():
                            modG = S.sb("modG2", [128, 2, D], F32)
                            load_mod(modG, 5)
                            xin = [S.sb("xin3_%d" % i, [128, D], F32) for i in range(2)]
                            xo = [S.sb("xo%d" % i, [128, D], F32) for i in range(2)]
                            tg = S.sb("tg2", [128, D], F32)
                            for tt in [t_ for t_ in tiles if not (last and t_ < 2)]:
                                k2 = tt % 2
                                j = 1 if tt < 2 else 0
                                DMA("sp", xin[k2][:], xres[tt * 128:(tt + 1) * 128, :], [xres_b[tt]], [xin[k2]])
                                TT("dve", tg[:], yacc[:, tt, :], modG[:, j, :], ALU.mult, [yacc.s(tt), modG], [tg])
                                TT("pool", xo[k2][:], tg[:], xin[k2][:], ALU.add, [tg, xin[k2]], [xo[k2]])
                                if last:
                                    DMA("sp", out[b, (tt - 2) * 128:(tt - 1) * 128, :], xo[k2][:], [xo[k2]], [])
                                else:
                                    DMA("sp", xres[tt * 128:(tt + 1) * 128, :], xo[k2][:], [xo[k2]], [xres_b[tt]])
                            if l == 0 and b == 0:
                                dump("xfin", list(xres_b), xres, [T, D], F32)
                S.pop()
                S.new_epoch()
        S.barrier()
        S.emit()
    return nc, dumps


_CACHE = {}


def kernel(**inputs):
    nb = 2
    n_cores = 8
    if "nc" not in _CACHE:
        _CACHE["nc"] = build(nb=nb, depth=DEPTH)[0]
    nc = _CACHE["nc"]
    consts = host_consts()
    in_maps = []
    for i in range(n_cores):
        m = {"x": np.ascontiguousarray(inputs["x"][i * nb:(i + 1) * nb]),
             "ctx": np.ascontiguousarray(inputs["ctx"][i * nb:(i + 1) * nb]),
             "c": np.ascontiguousarray(inputs["c"][i * nb:(i + 1) * nb])}
        for k_ in PARAM_SHAPES:
            m[k_] = np.ascontiguousarray(inputs[k_], dtype=np.float32)
        for k_ in CONST_SHAPES:
            m["k_" + k_] = consts[k_]
        in_maps.append(m)
    res = run_bass_kernel_spmd(nc, in_maps, core_ids=list(range(n_cores)))
    return np.concatenate([r["out"] for r in res.results], axis=0)
```

```python
import math
from contextlib import ExitStack, contextmanager
import numpy as np
import concourse.bass as bass
import concourse.mybir as mybir
from concourse.bass_utils import run_bass_kernel_spmd

F32 = mybir.dt.float32
BF16 = mybir.dt.bfloat16
U32 = mybir.dt.uint32
AF = mybir.ActivationFunctionType
ALU = mybir.AluOpType
AX = mybir.AxisListType

NRING = 8
D = 1024
SEQ = 2048
CTX = 256
T = SEQ + CTX
NT = T // 128
DEPTH = 4
P_IN = 3088
NE = 16
DE = 1408
NF = 11
CAP = 256
CAPC = 32
NSLOT = CAP + CAPC
EPS = 1e-6


class Buf:
    __slots__ = ("name", "w", "r")

    def __init__(self, name):
        self.name = name
        self.w = None
        self.r = []


class Tn:
    __slots__ = ("t", "_buf", "_subs", "name")

    def __init__(self, t, name):
        self.t = t
        self.name = name
        self._buf = Buf(name)
        self._subs = {}

    def __getitem__(self, k):
        return self.t[k]

    def s(self, i):
        b = self._subs.get(i)
        if b is None:
            b = self._subs[i] = Buf("%s.%s" % (self.name, i))
        return b


class Sched:
    ENGS = ("pe", "act", "dve", "pool", "sp")

    def __init__(self, nc, stack):
        self.nc = nc
        self.stacks = [stack]
        self.ops = {e: [] for e in self.ENGS}
        self.cnt = {e: 0 for e in self.ENGS}
        self.sem = {e: stack.enter_context(nc.semaphore("s_" + e)) for e in self.ENGS}
        self.dring = {e: [stack.enter_context(nc.semaphore("d_%s%d" % (e, i))) for i in range(NRING)]
                      for e in ("sp", "pool", "act")}
        self.dcnt = {e: 0 for e in ("sp", "pool", "act")}
        self.waited = {e: {} for e in self.ENGS}
        self.semobj = {}
        self.uid = 0
        self.retired = set()
        self.nep = 0

    def new_epoch(self):
        self.nep += 1
        for e in self.ENGS:
            self.retired.add(id(self.sem[e]))
            self.sem[e] = self.stacks[0].enter_context(self.nc.semaphore("s_%s_%d" % (e, self.nep)))
            self.cnt[e] = 0

    def sb(self, name, shape, dt):
        self.uid += 1
        nm = "%s_%d" % (name, self.uid)
        t = self.stacks[-1].enter_context(self.nc.sbuf_tensor(nm, list(shape), dt))
        return Tn(t, nm)

    def ps(self, name, shape, dt=F32):
        self.uid += 1
        nm = "%s_%d" % (name, self.uid)
        t = self.stacks[-1].enter_context(self.nc.psum_tensor(nm, list(shape), dt))
        return Tn(t, nm)

    def push(self):
        self.stacks.append(ExitStack())

    def pop(self):
        self.barrier()
        self.stacks.pop().close()

    @contextmanager
    def scope(self):
        st = ExitStack()
        self.stacks.append(st)
        try:
            yield
            self.barrier()
        finally:
            self.stacks.pop()
            st.close()

    def _need(self, eng, tok, waits):
        sid, val, teng, is_dma = tok
        if sid in self.retired:
            return
        w = self.waited[eng]
        if w.get(sid, 0) >= val:
            return
        w[sid] = val
        waits.append((sid, val))

    def _deps(self, eng, reads, writes, waits):
        for b in reads:
            if b.w is not None:
                self._need(eng, b.w, waits)
        for b in writes:
            tok = b.w
            if tok is not None and not (tok[2] == eng and not tok[3]):
                self._need(eng, tok, waits)
            for tok in b.r:
                if not (tok[2] == eng and not tok[3]):
                    self._need(eng, tok, waits)

    @staticmethod
    def _bufs(xs):
        out = []
        for x in xs:
            if x is None:
                continue
            out.append(x if isinstance(x, Buf) else x._buf)
        return out

    def _commit(self, tok, reads, writes):
        for b in reads:
            b.r.append(tok)
        for b in writes:
            b.w = tok
            b.r = []

    def op(self, eng, fn, reads=(), writes=()):
        reads = self._bufs(reads)
        writes = self._bufs(writes)
        waits = []
        self._deps(eng, reads, writes, waits)
        self.cnt[eng] += 1
        s = self.sem[eng]
        self.semobj[id(s)] = s
        tok = (id(s), self.cnt[eng], eng, False)
        self._commit(tok, reads, writes)
        self.ops[eng].append((waits, fn, s, 1))
        return tok

    def dma(self, q, fn, reads=(), writes=()):
        reads = self._bufs(reads)
        writes = self._bufs(writes)
        waits = []
        self._deps(q, reads, writes, waits)
        k = self.dcnt[q]
        self.dcnt[q] += 1
        s = self.dring[q][k % NRING]
        self.semobj[id(s)] = s
        target = 16 * (k // NRING + 1)
        if k >= NRING:
            self._need(q, (id(s), target - 16, q, True), waits)
        tok = (id(s), target, q, True)
        self._commit(tok, reads, writes)
        self.ops[q].append((waits, fn, s, 16))
        return tok

    def all_tokens(self):
        toks = []
        for e in self.ENGS:
            if self.cnt[e] > 0:
                s = self.sem[e]
                self.semobj[id(s)] = s
                toks.append((id(s), self.cnt[e], e, False))
        for q in self.dcnt:
            k = self.dcnt[q]
            for i in range(min(k, NRING)):
                uses = (k - i + NRING - 1) // NRING
                toks.append((id(self.dring[q][i]), 16 * uses, q, True))
        return toks

    def barrier(self):
        toks = self.all_tokens()
        for e in self.ENGS:
            waits = []
            for t in toks:
                if t[2] == e and not t[3]:
                    continue
                self._need(e, t, waits)
            if waits:
                self.ops[e].append((waits, None, None, 0))

    def emit(self):
        nc = self.nc
        with nc.Block() as block:
            def run(e):
                def body(engobj):
                    for waits, fn, s, inc in self.ops[e]:
                        for sid, val in waits:
                            engobj.wait_ge(self.semobj[sid], val)
                        if fn is not None:
                            fn(engobj).then_inc(s, inc)
                return body
            block.tensor(run("pe"))
            block.scalar(run("act"))
            block.vector(run("dve"))
            block.gpsimd(run("pool"))
            block.sync(run("sp"))


def host_consts():
    k = np.arange(128)
    c = {}
    c["ident"] = np.eye(128, dtype=np.float32)
    c["ones"] = np.ones((128, 128), np.float32)
    tri_f = (k[:, None] <= k[None, :]).astype(np.float32)
    tri_b = (k[:, None] >= k[None, :]).astype(np.float32)
    c["tri"] = np.stack([tri_f, tri_b])
    mf = np.where(k[None, :] >= k[:, None], 0.0, -1e5).astype(np.float32)
    mb = np.where(k[None, :] <= k[:, None], 0.0, -1e5).astype(np.float32)
    c["mneg"] = np.stack([np.tile(mf, (1, 4)), np.tile(mb, (1, 4))])
    n = SEQ
    row = np.repeat(np.arange(n // 64), 64)
    col = np.tile(np.arange(64), n // 64)
    inv = (10000.0 ** (-np.arange(16, dtype=np.float32) / 16)).astype(np.float32)
    ang = np.stack([row, col], -1).astype(np.float32)[..., None] * inv
    cs, sn = np.cos(ang).astype(np.float32), np.sin(ang).astype(np.float32)
    C = np.stack([cs, cs], 2)
    Sg = np.stack([-sn, sn], 2)
    c["ropeC"] = C.reshape(n, 64).astype(np.float32)
    c["ropeS"] = Sg.reshape(n, 64).astype(np.float32)
    c["iota"] = np.tile(np.arange(SEQ, dtype=np.float32)[None, :], (128, 1))
    c["tpos"] = (k[:, None] + 128 * np.arange(16)[None, :]).astype(np.float32)
    return c


CONST_SHAPES = {"ident": [128, 128], "ones": [128, 128], "tri": [2, 128, 128], "mneg": [2, 128, 512],
                "ropeC": [SEQ, 64], "ropeS": [SEQ, 64], "iota": [128, SEQ], "tpos": [128, 16]}

def param_shapes(depth=DEPTH, moe=True):
    ps = {
        "c_ctx": [D], "w_mod": [depth, D, 6 * D], "b_mod": [depth, 6 * D], "norm1_w": [depth, D], "norm2_w": [depth, D],
        "w_in": [depth, D, P_IN], "q_norm_w": [depth, 64], "k_norm_w": [depth, 64],
        "lambda_q1": [depth, 64], "lambda_k1": [depth, 64], "lambda_q2": [depth, 64], "lambda_k2": [depth, 64],
        "subln_w": [depth, 128], "conv_w": [depth, 3, D], "conv_b": [depth, D],
        "dt_bias_f": [depth, 8], "dt_bias_b": [depth, 8], "a_log_f": [depth, 8], "a_log_b": [depth, 8],
        "d_skip": [depth, 8], "ssd_norm_w": [depth, 512], "w_out": [depth, D, D], "w_router": [depth, D, NE],
    }
    if moe:
        ps.update({"w_gate": [depth, NE, D, DE], "w_up": [depth, NE, D, DE], "w_down": [depth, NE, DE, D]})
    return ps


PARAM_SHAPES = param_shapes()


def build(nb=2, depth=DEPTH, dbg=None, moe=True, force_last=False):
    nc = bass.Bass("TRN2", target_bir_lowering=False)
    din = {}
    din["x"] = nc.dram_tensor("x", [nb, SEQ, D], F32, kind="ExternalInput").ap()
    din["ctx"] = nc.dram_tensor("ctx", [nb, CTX, D], F32, kind="ExternalInput").ap()
    din["c"] = nc.dram_tensor("c", [nb, D], F32, kind="ExternalInput").ap()
    for k_, shp in param_shapes(depth, moe).items():
        din[k_] = nc.dram_tensor(k_, shp, F32, kind="ExternalInput").ap()
    for k_, shp in CONST_SHAPES.items():
        din[k_] = nc.dram_tensor("k_" + k_, shp, F32, kind="ExternalInput").ap()
    out = nc.dram_tensor("out", [nb, SEQ, D], F32, kind="ExternalOutput").ap()
    xres = nc.dram_tensor("xres", [T, D], F32).ap()
    modrow = nc.dram_tensor("modrow", [depth, nb + 1, 6 * D], F32).ap()
    affd = nc.dram_tensor("affd", [NE, T], F32).ap()
    hTd = nc.dram_tensor("hTd", [128, 8, T], BF16).ap()
    dumps = {}

    with ExitStack() as st0:
        S = Sched(nc, st0)
        st0.enter_context(nc.allow_non_contiguous_dma(reason="small param layouts"))
        st0.enter_context(nc.allow_low_precision(reason="bf16 matmul operands by design"))

        def MM(oap, lap, rap, R, W, start=True, stop=True):
            S.op("pe", lambda e: e.matmul(oap, lhsT=lap, rhs=rap, start=start, stop=stop), R, W)

        def TR(oap, iap, idap, R, W):
            S.op("pe", lambda e: e.transpose(out=oap, in_=iap, identity=idap), R, W)

        def ACT(oap, iap, func, R, W, **kw):
            S.op("act", lambda e: e.activation(out=oap, in_=iap, func=func, **kw), R, W)

        def TT(eng, oap, a, b, op, R, W):
            S.op(eng, lambda e: e.tensor_tensor(out=oap, in0=a, in1=b, op=op), R, W)

        def TS(eng, oap, a, s1, s2, op0, op1, R, W):
            if op1 is None:
                S.op(eng, lambda e: e.tensor_scalar(out=oap, in0=a, scalar1=s1, scalar2=None, op0=op0), R, W)
            else:
                S.op(eng, lambda e: e.tensor_scalar(out=oap, in0=a, scalar1=s1, scalar2=s2, op0=op0, op1=op1), R, W)

        def STT(oap, a, s, b, op0, op1, R, W):
            S.op("dve", lambda e: e.scalar_tensor_tensor(out=oap, in0=a, scalar=s, in1=b, op0=op0, op1=op1), R, W)

        def CP(eng, oap, iap, R, W):
            if eng == "act":
                S.op("act", lambda e: e.copy(out=oap, in_=iap), R, W)
            else:
                S.op(eng, lambda e: e.tensor_copy(out=oap, in_=iap), R, W)

        def RED(oap, iap, R, W, op=ALU.add):
            S.op("dve", lambda e: e.tensor_reduce(out=oap, in_=iap, axis=AX.X, op=op), R, W)

        def RCP(oap, iap, R, W):
            S.op("dve", lambda e: e.reciprocal(out=oap, in_=iap), R, W)

        def MS(eng, ap, val, W):
            S.op(eng, lambda e: e.memset(ap, val), (), W)

        def DMA(q, oap, iap, R, W):
            return S.dma(q, lambda e: e.dma_start(out=oap, in_=iap), R, W)

        xres_b = [Buf("xres%d" % i) for i in range(NT)]
        modrow_b = Buf("modrow")
        affd_b = Buf("affd")
        hTd_b = Buf("hTd")
        out_toks = []

        def dump(name, tn_or_bufs, ap, shape, dt=F32):
            if dbg is None or name not in dbg or name in dumps:
                return
            d = nc.dram_tensor("dbg_" + name, list(shape), dt, kind="ExternalOutput").ap()
            dumps[name] = (list(shape), dt)
            R = tn_or_bufs if isinstance(tn_or_bufs, (list, tuple)) else [tn_or_bufs]
            out_toks.append(DMA("sp", d, ap, R, []))

        ident_f = S.sb("ident_f", [128, 128], F32)
        ident_b = S.sb("ident_b", [128, 128], BF16)
        ones_f = S.sb("ones_f", [128, 128], F32)
        tri = S.sb("tri", [128, 2, 128], F32)
        tpos = S.sb("tpos", [128, 16], F32)
        DMA("sp", ident_f[:], din["ident"], [], [ident_f])
        DMA("sp", ones_f[:], din["ones"], [], [ones_f])
        DMA("sp", tri[:], din["tri"].rearrange("a p l -> p a l"), [], [tri])
        DMA("sp", tpos[:], din["tpos"], [], [tpos])
        CP("dve", ident_b[:], ident_f[:], [ident_f], [ident_b])

        with S.scope():
            cT = S.sb("cT", [128, 8, nb + 1], F32)
            sT = S.sb("sT", [128, 8, nb + 1], BF16)
            sg = S.sb("sg", [128, 8, nb + 1], F32)
            for j in range(nb):
                DMA("sp", cT[:, :, j], din["c"][j].rearrange("(c p) -> p c", p=128), [], [cT])
            DMA("sp", cT[:, :, nb], din["c_ctx"].rearrange("(c p) -> p c", p=128), [], [cT])
            ACT(sg[:], cT[:], AF.Sigmoid, [cT], [sg])
            TT("dve", sT[:], cT[:], sg[:], ALU.mult, [cT, sg], [sT])
            wm = [S.sb("wm%d" % i, [128, 8, 1536], BF16) for i in range(2)]
            bm = S.sb("bm", [nb + 1, 6 * D], F32)
            mr = S.sb("mr", [nb + 1, 6 * D], F32)
            pm = [S.ps("pm%d" % i, [nb + 1, 512]) for i in range(2)]
            it = 0
            for l in range(depth):
                DMA("sp", bm[:], din["b_mod"][l:l + 1, :].to_broadcast([nb + 1, 6 * D]), [], [bm])
                for blk in range(4):
                    w_ = wm[it % 2]
                    it += 1
                    DMA("pool", w_[:], din["w_mod"][l, :, blk * 1536:(blk + 1) * 1536].rearrange("(c p) n -> p c n", p=128),
                        [], [w_])
                    for sub in range(3):
                        p_ = pm[sub % 2]
                        for c_ in range(8):
                            MM(p_[:], sT[:, c_, :], w_[:, c_, sub * 512:(sub + 1) * 512], [sT, w_], [p_],
                               start=(c_ == 0), stop=(c_ == 7))
                        col = blk * 1536 + sub * 512
                        TT("dve", mr[:, col:col + 512], p_[:], bm[:, col:col + 512], ALU.add, [p_, bm], [mr])
                DMA("sp", modrow[l], mr[:], [mr], [modrow_b])

        for b in range(nb):
            for l in range(depth):
                last = (l == DEPTH - 1) or force_last
                lam_init = 0.8 - 0.6 * math.exp(-0.3 * l)
                tiles = list(range(NT))

                def xsrc(tt):
                    if l == 0:
                        if tt < 2:
                            return din["ctx"][b, tt * 128:(tt + 1) * 128, :], []
                        return din["x"][b, (tt - 2) * 128:(tt - 1) * 128, :], []
                    return xres[tt * 128:(tt + 1) * 128, :], [xres_b[tt]]

                def load_mod(dst, which, eng="sp"):
                    DMA(eng, dst[:, 0, :], modrow[l, b:b + 1, which * D:(which + 1) * D].to_broadcast([128, D]), [modrow_b], [dst])
                    DMA(eng, dst[:, 1, :], modrow[l, nb:nb + 1, which * D:(which + 1) * D].to_broadcast([128, D]), [modrow_b], [dst])

                def do_norm1(hT):
                    with S.scope():
                        modA = S.sb("modA", [128, 2, D], F32)
                        modS = S.sb("modS", [128, 2, D], F32)
                        nw = S.sb("nw", [128, D], F32)
                        load_mod(modS, 0)
                        load_mod(modA, 1)
                        DMA("sp", nw[:], din["norm1_w"][l:l + 1, :].to_broadcast([128, D]), [], [nw])
                        for j in range(2):
                            STT(modA[:, j, :], modA[:, j, :], 1.0, nw[:], ALU.add, ALU.mult, [modA, nw], [modA])
                        xin = [S.sb("xin%d" % i, [128, D], F32) for i in range(2)]
                        junk = S.sb("junk", [128, D], BF16)
                        t1 = [S.sb("t1_%d" % i, [128, D], F32) for i in range(2)]
                        hb = [S.sb("hb%d" % i, [128, D], BF16) for i in range(2)]
                        st = [S.sb("st%d" % i, [128, 4], F32) for i in range(2)]
                        pT8 = [S.ps("pT8_%d" % i, [128, 8, 128], BF16) for i in range(2)]
                        for tt in tiles:
                            k2 = tt % 2
                            j = 1 if tt < 2 else 0
                            src, sb_ = xsrc(tt)
                            DMA("sp", xin[k2][:], src, sb_, [xin[k2]])
                            ACT(junk[:], xin[k2][:], AF.Square, [xin[k2]], [junk, st[k2]], accum_out=st[k2][:, 0:1])
                            ACT(st[k2][:, 1:2], st[k2][:, 0:1], AF.Sqrt, [st[k2]], [st[k2]], scale=1.0 / D, bias=EPS)
                            RCP(st[k2][:, 2:3], st[k2][:, 1:2], [st[k2]], [st[k2]])
                            STT(t1[k2][:], xin[k2][:], st[k2][:, 2:3], modA[:, j, :], ALU.mult, ALU.mult,
                                [xin[k2], st[k2], modA], [t1[k2]])
                            TT("dve", hb[k2][:], t1[k2][:], modS[:, j, :], ALU.add, [t1[k2], modS], [hb[k2]])
                            for c_ in range(8):
                                TR(pT8[k2][:, c_, :], hb[k2][:, c_ * 128:(c_ + 1) * 128], ident_b[:], [hb[k2], ident_b], [pT8[k2]])
                            CP("act", hT[:, :, tt * 128:(tt + 1) * 128], pT8[k2][:], [pT8[k2]], [hT.s(tt)])
                        if l == 0 and b == 0:
                            dump("hT", [hT.s(t_) for t_ in tiles], hT[:], [128, 8, T], BF16)


                S.push()
                tokbuf = S.sb("tokbuf", [128, NT, 1024], BF16)
                mixtok = tokbuf
                with S.scope():
                    hT = S.sb("hT", [128, 8, T], BF16)
                    do_norm1(hT)
                    DMA("sp", hTd, hT[:], [hT.s(t_) for t_ in tiles], [hTd_b])
                    with S.scope():
                        qkT = S.sb("qkT", [128, 8, T], BF16)
                        v_aug = S.sb("v_aug", [128, NT, 4, 132], BF16)
                        MS("pool", v_aug[:], 1.0, [v_aug])
                        nlam = S.sb("nlam", [128, 4], F32)
                        subw = S.sb("subw", [128, 128], F32)
                        with S.scope():
                            wqkv = S.sb("wqkv", [128, 8, 1536], BF16)
                            ropeC = S.sb("ropeC", [128, 16, 64], F32)
                            ropeS = S.sb("ropeS", [128, 16, 64], F32)
                            DMA("sp", ropeC[:], din["ropeC"].rearrange("(j p) d -> p j d", p=128), [], [ropeC])
                            DMA("sp", ropeS[:], din["ropeS"].rearrange("(j p) d -> p j d", p=128), [], [ropeS])
                            DMA("pool", wqkv[:], din["w_in"][l, :, 0:1536].rearrange("(c p) n -> p c n", p=128), [], [wqkv])
                            qkW = S.sb("qkW", [128, 16, 64], F32)
                            for g in range(8):
                                DMA("sp", qkW[:, g, :], din["q_norm_w"][l:l + 1, :].to_broadcast([128, 64]), [], [qkW])
                                DMA("sp", qkW[:, 8 + g, :], din["k_norm_w"][l:l + 1, :].to_broadcast([128, 64]), [], [qkW])
                            lp = S.sb("lp", [128, 4, 64], F32)
                            for i_, nm in enumerate(["lambda_q1", "lambda_k1", "lambda_q2", "lambda_k2"]):
                                DMA("sp", lp[:, i_, :], din[nm][l:l + 1, :].to_broadcast([128, 64]), [], [lp])
                            lpr = S.sb("lpr", [128, 2, 64], F32)
                            TT("dve", lpr[:, 0, :], lp[:, 0, :], lp[:, 1, :], ALU.mult, [lp], [lpr])
                            TT("dve", lpr[:, 1, :], lp[:, 2, :], lp[:, 3, :], ALU.mult, [lp], [lpr])
                            RED(nlam[:, 0:2], lpr[:], [lpr], [nlam])
                            ACT(nlam[:, 0:2], nlam[:, 0:2], AF.Exp, [nlam], [nlam])
                            TT("dve", nlam[:, 2:3], nlam[:, 1:2], nlam[:, 0:1], ALU.subtract, [nlam], [nlam])
                            TS("dve", nlam[:, 3:4], nlam[:, 2:3], -lam_init, None, ALU.add, None, [nlam], [nlam])
                            DMA("sp", subw[:], din["subln_w"][l:l + 1, :].to_broadcast([128, 128]), [], [subw])
                            TS("dve", subw[:], subw[:], 1.0 - lam_init, None, ALU.mult, None, [subw], [subw])

                            ps_qkv_ = [S.ps("ps_qkv%d" % i, [128, 1536]) for i in range(2)]
                            pT8 = [S.ps("pT8q_%d" % i, [128, 8, 128], BF16) for i in range(2)]
                            sqb = S.sb("sqb", [128, 16, 64], F32)
                            s16 = S.sb("s16", [128, 3, 16], F32)
                            qn = S.sb("qn", [128, 16, 64], F32)
                            qn2 = S.sb("qn2", [128, 16, 64], F32)
                            ta = S.sb("ta", [128, 16, 64], F32)
                            tb = S.sb("tb", [128, 16, 64], F32)
                            qr = [S.sb("qr%d" % i, [128, 16 * 64], BF16) for i in range(2)]
                            for tt in tiles:
                                k2 = tt % 2
                                ps_qkv = ps_qkv_[k2]
                                for j in range(3):
                                    for c_ in range(8):
                                        MM(ps_qkv[:, j * 512:(j + 1) * 512], hT[:, c_, tt * 128:(tt + 1) * 128],
                                           wqkv[:, c_, j * 512:(j + 1) * 512], [hT.s(tt), wqkv], [ps_qkv],
                                           start=(c_ == 0), stop=(c_ == 7))
                                CP("act", v_aug[:, tt, :, 0:128], ps_qkv[:, 1024:1536].rearrange("p (h e) -> p h e", h=4),
                                   [ps_qkv], [v_aug.s(tt)])
                                qk3 = ps_qkv[:, 0:1024].rearrange("p (g e) -> p g e", g=16)
                                ACT(sqb[:], qk3, AF.Square, [ps_qkv], [sqb])
                                RED(s16[:, 0, :], sqb[:], [sqb], [s16])
                                ACT(s16[:, 1, :], s16[:, 0, :], AF.Sqrt, [s16], [s16], scale=1.0 / 64, bias=EPS)
                                RCP(s16[:, 2, :], s16[:, 1, :], [s16], [s16])
                                TT("dve", qn[:], qk3, s16[:, 2, :].unsqueeze(2).to_broadcast([128, 16, 64]), ALU.mult,
                                   [ps_qkv, s16], [qn])
                                if tt < 2:
                                    TT("pool", qr[k2][:].rearrange("p (g e) -> p g e", g=16), qn[:], qkW[:], ALU.mult,
                                       [qn, qkW], [qr[k2]])
                                else:
                                    jj = tt - 2
                                    TT("dve", qn2[:], qn[:], qkW[:], ALU.mult, [qn, qkW], [qn2])
                                    TT("pool", ta[:], qn2[:], ropeC[:, jj, :].unsqueeze(1).to_broadcast([128, 16, 64]), ALU.mult,
                                       [qn2, ropeC], [ta])
                                    q5 = qn2[:].rearrange("p g (a h f) -> p g a h f", a=2, h=2)
                                    t5 = tb[:].rearrange("p g (a h f) -> p g a h f", a=2, h=2)
                                    s5 = ropeS[:, jj, :].rearrange("p (a h f) -> p a h f", a=2, h=2)
                                    for hh in range(2):
                                        TT("dve", t5[:, :, :, hh, :], q5[:, :, :, 1 - hh, :],
                                           s5[:, :, hh, :].unsqueeze(1).to_broadcast([128, 16, 2, 16]), ALU.mult,
                                           [qn2, ropeS], [tb])
                                    TT("dve", qr[k2][:].rearrange("p (g e) -> p g e", g=16), ta[:], tb[:], ALU.add,
                                       [ta, tb], [qr[k2]])
                                for m in range(8):
                                    TR(pT8[k2][:, m, :], qr[k2][:, m * 128:(m + 1) * 128], ident_b[:], [qr[k2], ident_b], [pT8[k2]])
                                CP("act", qkT[:, :, tt * 128:(tt + 1) * 128], pT8[k2][:], [pT8[k2]], [qkT.s(tt)])
                            if l == 0 and b == 0:
                                dump("qkT", [qkT.s(t_) for t_ in tiles], qkT[:], [128, 8, T], BF16)
                                dump("v_aug", [v_aug.s(t_) for t_ in tiles] + [v_aug], v_aug[:], [128, NT, 4, 132], BF16)

                        with S.scope():
                            pTb = [S.sb("pTb%d" % i, [128, NT, 512], BF16) for i in range(2)]
                            ps_s = [S.ps("ps_s%d" % i, [128, 512]) for i in range(3)]
                            ps_o = [[S.ps("ps_o%d_%d" % (i, h_), [128, 512]) for h_ in range(2)] for i in range(2)]
                            oraw = [[S.sb("oraw%d_%d" % (i, q_), [128, 132], F32) for q_ in range(4)] for i in range(2)]
                            ob = [S.sb("ob%d" % i, [128, 2, 128], F32) for i in range(2)]
                            sc_ = [S.sb("sc%d" % i, [128, 8], F32) for i in range(2)]
                            junk2 = S.sb("junk2", [128, 128], BF16)
                            vall = [v_aug] + [v_aug.s(t_) for t_ in tiles]
                            blocks = []
                            if not last:
                                blocks.append((0, 256, [0, 1]))
                            for bq in range(4):
                                blocks.append((256 + bq * 512, 512, list(range(NT))))
                            units = [(hd, blk, i) for hd in range(4) for blk in blocks for i in range(2)]
                            cnt_ = {"sc": 0, "comb": 0}

                            def s_ops(u):
                                hd, (q0, qn_, kcs), i = u
                                ops_ = []
                                for kc in kcs:
                                    def f(kc=kc):
                                        p_ = ps_s[cnt_["sc"] % 3]
                                        cnt_["sc"] += 1
                                        MM(p_[:, 0:qn_], qkT[i * 64:(i + 1) * 64, 4 + hd, kc * 128:(kc + 1) * 128],
                                           qkT[i * 64:(i + 1) * 64, hd, q0:q0 + qn_],
                                           [qkT.s(kc)] + [qkT.s(q0 // 128 + u_) for u_ in range(qn_ // 128)], [p_])
                                        ACT(pTb[i][:, kc, 0:qn_], p_[:, 0:qn_], AF.Exp, [p_], [pTb[i].s(kc)], scale=0.125)
                                    ops_.append(f)
                                return ops_

                            def v_ops(u):
                                hd, (q0, qn_, kcs), i = u
                                ops_ = []
                                for tq in range(qn_ // 128):
                                    po = ps_o[i][tq // 2]
                                    c0 = (tq % 2) * 256
                                    for n_, kc in enumerate(kcs):
                                        def f(tq=tq, po=po, c0=c0, n_=n_, kc=kc):
                                            MM(po[:, c0:c0 + 129], pTb[i][:, kc, tq * 128:(tq + 1) * 128], v_aug[:, kc, hd, 0:129],
                                               [pTb[i].s(kc)] + vall, [po], start=(n_ == 0), stop=(n_ == len(kcs) - 1))
                                        ops_.append(f)

                                    def g(tq=tq, po=po, c0=c0):
                                        CP("dve", oraw[i][tq][:, 0:129], po[:, c0:c0 + 129], [po], [oraw[i][tq]])
                                        if i == 1:
                                            tt = q0 // 128 + tq
                                            k2 = cnt_["comb"] % 2
                                            cnt_["comb"] += 1
                                            s_ = sc_[k2]
                                            o_ = ob[k2]
                                            o0, o1 = oraw[0][tq], oraw[1][tq]
                                            RCP(s_[:, 0:1], o0[:, 128:129], [o0], [s_])
                                            RCP(s_[:, 1:2], o1[:, 128:129], [o1], [s_])
                                            TT("dve", s_[:, 2:3], s_[:, 1:2], nlam[:, 3:4], ALU.mult, [s_, nlam], [s_])
                                            TS("dve", o_[:, 0, :], o0[:, 0:128], s_[:, 0:1], None, ALU.mult, None, [o0, s_], [o_])
                                            STT(o_[:, 1, :], o1[:, 0:128], s_[:, 2:3], o_[:, 0, :], ALU.mult, ALU.add, [o1, s_, o_], [o_])
                                            ACT(junk2[:], o_[:, 1, :], AF.Square, [o_], [junk2, s_], accum_out=s_[:, 3:4])
                                            ACT(s_[:, 4:5], s_[:, 3:4], AF.Sqrt, [s_], [s_], scale=1.0 / 128, bias=EPS)
                                            RCP(s_[:, 5:6], s_[:, 4:5], [s_], [s_])
                                            STT(mixtok[:, tt, hd * 128:(hd + 1) * 128], o_[:, 1, :], s_[:, 5:6], subw[:], ALU.mult, ALU.mult,
                                                [o_, s_, subw], [mixtok.s(tt)])
                                    ops_.append(g)
                                return ops_

                            prev = []
                            for u in units + [None]:
                                cur = s_ops(u) if u is not None else []
                                ratio = (len(prev) + max(len(cur), 1) - 1) // max(len(cur), 1)
                                pi_ = 0
                                for f in cur:
                                    f()
                                    for _ in range(ratio):
                                        if pi_ < len(prev):
                                            prev[pi_]()
                                            pi_ += 1
                                while pi_ < len(prev):
                                    prev[pi_]()
                                    pi_ += 1
                                prev = v_ops(u) if u is not None else []
                            if l == 0 and b == 0:
                                dump("mixattn", [mixtok.s(t_) for t_ in tiles], mixtok[:, :, 0:512], [128, NT, 512], BF16)

                with S.scope():
                    BCT = S.sb("BCT", [128, 4, T], BF16)
                    xsB = S.sb("xsB", [128, NT, 768], BF16)
                    sz = S.sb("sz", [128, NT, 512], BF16)
                    dtt = S.sb("dtt", [128, NT, 16], F32)
                    dA = S.sb("dA", [128, NT, 16], F32)
                    dsk = S.sb("dsk", [128, 8], F32)
                    ssdw = S.sb("ssdw", [128, 512], F32)
                    DMA("sp", dsk[:], din["d_skip"][l:l + 1, :].to_broadcast([128, 8]), [], [dsk])
                    DMA("sp", ssdw[:], din["ssd_norm_w"][l:l + 1, :].to_broadcast([128, 512]), [], [ssdw])
                    with S.scope():
                        hT = S.sb("hT2", [128, 8, T], BF16)
                        DMA("sp", hT[:], hTd, [hTd_b], [hT.s(t_) for t_ in tiles])
                        hall = [hT.s(t_) for t_ in tiles]
                        with S.scope():
                            wz = S.sb("wz", [128, 8, 528], BF16)
                            DMA("pool", wz[:, :, 0:512], din["w_in"][l, :, 1536:2048].rearrange("(c p) n -> p c n", p=128), [], [wz])
                            DMA("pool", wz[:, :, 512:528], din["w_in"][l, :, 3072:3088].rearrange("(c p) n -> p c n", p=128), [], [wz])
                            dtb = S.sb("dtb", [128, 16], F32)
                            abc = S.sb("abc", [128, 16], F32)
                            DMA("sp", dtb[:, 0:8], din["dt_bias_f"][l:l + 1, :].to_broadcast([128, 8]), [], [dtb])
                            DMA("sp", dtb[:, 8:16], din["dt_bias_b"][l:l + 1, :].to_broadcast([128, 8]), [], [dtb])
                            DMA("sp", abc[:, 0:8], din["a_log_f"][l:l + 1, :].to_broadcast([128, 8]), [], [abc])
                            DMA("sp", abc[:, 8:16], din["a_log_b"][l:l + 1, :].to_broadcast([128, 8]), [], [abc])
                            ACT(abc[:], abc[:], AF.Exp, [abc], [abc])
                            TS("dve", abc[:], abc[:], -1.0, None, ALU.mult, None, [abc], [abc])
                            ps_z = [S.ps("ps_z%d" % i, [128, 512]) for i in range(2)]
                            ps_dt = [S.ps("ps_dt%d" % i, [128, 16]) for i in range(2)]
                            for tt in tiles:
                                k2 = tt % 2
                                for c_ in range(8):
                                    MM(ps_z[k2][:], hT[:, c_, tt * 128:(tt + 1) * 128], wz[:, c_, 0:512], [hT.s(tt), wz], [ps_z[k2]],
                                       start=(c_ == 0), stop=(c_ == 7))
                                for c_ in range(8):
                                    MM(ps_dt[k2][:], hT[:, c_, tt * 128:(tt + 1) * 128], wz[:, c_, 512:528], [hT.s(tt), wz], [ps_dt[k2]],
                                       start=(c_ == 0), stop=(c_ == 7))
                                ACT(sz[:, tt, :], ps_z[k2][:], AF.Silu, [ps_z[k2]], [sz.s(tt)])
                                TT("dve", dtt[:, tt, :], ps_dt[k2][:], dtb[:], ALU.add, [ps_dt[k2], dtb], [dtt])
                            ACT(dtt[:], dtt[:], AF.Exp, [dtt], [dtt])
                            ACT(dtt[:], dtt[:], AF.Ln, [dtt], [dtt], bias=1.0)
                            TT("dve", dA[:], dtt[:], abc[:].unsqueeze(1).to_broadcast([128, NT, 16]), ALU.mult, [dtt, abc], [dA])
                        with S.scope():
                            wx = S.sb("wx", [128, 8, 1024], BF16)
                            DMA("pool", wx[:], din["w_in"][l, :, 2048:3072].rearrange("(c p) n -> p c n", p=128), [], [wx])
                            cw = S.sb("cw", [128, 8, 3], F32)
                            cbv = S.sb("cbv", [128, 8], F32)
                            for k_ in range(3):
                                DMA("sp", cw[:, :, k_], din["conv_w"][l, k_].rearrange("(c p) -> p c", p=128), [], [cw])
                            DMA("sp", cbv[:], din["conv_b"][l].rearrange("(c p) -> p c", p=128), [], [cbv])
                            raw = [S.sb("raw%d" % i, [128, T + 4], F32) for i in range(1)]
                            acc = S.sb("acc", [128, T], F32)
                            fmb = S.sb("fmb", [128, T], BF16)
                            ps_x = [S.ps("ps_x%d" % i, [128, 512]) for i in range(2)]
                            pT4 = [S.ps("pT4_%d" % i, [128, 4, 128], BF16) for i in range(2)]
                            MS("pool", raw[0][:], 0.0, [raw[0]])
                            npx = 0
                            ntr = 0
                            segs = [(0, 256, 1), (256, 512, 259), (768, 512, 259 + 512), (1280, 512, 259 + 1024), (1792, 512, 259 + 1536)]
                            for ch in range(8):
                                r_ = raw[0]
                                for (c0, cn, ro) in segs:
                                    p_ = ps_x[npx % 2]
                                    npx += 1
                                    for c_ in range(8):
                                        MM(p_[:, 0:cn], wx[:, c_, ch * 128:(ch + 1) * 128], hT[:, c_, c0:c0 + cn],
                                           [wx] + [hT.s(c0 // 128 + u) for u in range(cn // 128)], [p_], start=(c_ == 0), stop=(c_ == 7))
                                    CP("act", r_[:, ro:ro + cn], p_[:, 0:cn], [p_], [r_])
                                for (a0, an, ro) in [(0, 256, 1), (256, 2048, 259)]:
                                    TS("pool", acc[:, a0:a0 + an], r_[:, ro:ro + an], cw[:, ch, 1:2], cbv[:, ch:ch + 1], ALU.mult, ALU.add,
                                       [r_, cw, cbv], [acc])
                                    STT(acc[:, a0:a0 + an], r_[:, ro - 1:ro - 1 + an], cw[:, ch, 0:1], acc[:, a0:a0 + an], ALU.mult, ALU.add,
                                        [r_, cw, acc], [acc])
                                    STT(acc[:, a0:a0 + an], r_[:, ro + 1:ro + 1 + an], cw[:, ch, 2:3], acc[:, a0:a0 + an], ALU.mult, ALU.add,
                                        [r_, cw, acc], [acc])
                                if ch >= 4:
                                    ACT(BCT[:, ch - 4, :], acc[:], AF.Silu, [acc], [BCT.s(ch - 4)])
                                    src_t, src_b = BCT, BCT.s(ch - 4)
                                    src_ap = lambda c0_, cn_: BCT[:, ch - 4, c0_:c0_ + cn_]
                                else:
                                    ACT(fmb[:], acc[:], AF.Silu, [acc], [fmb])
                                    src_b = fmb._buf
                                    src_ap = lambda c0_, cn_: fmb[:, c0_:c0_ + cn_]
                                if ch < 6:
                                    for t4 in range(0, NT, 4):
                                        n4 = min(4, NT - t4)
                                        p4 = pT4[ntr % 2]
                                        ntr += 1
                                        for u in range(n4):
                                            TR(p4[:, u, :], src_ap((t4 + u) * 128, 128), ident_b[:], [src_b, ident_b], [p4])
                                        CP("dve", xsB[:, t4:t4 + n4, ch * 128:(ch + 1) * 128], p4[:, 0:n4, :], [p4], [xsB])
                            if l == 0 and b == 0:
                                dump("BCT", [BCT.s(i) for i in range(4)], BCT[:], [128, 4, T], BF16)
                                dump("xsB", [xsB], xsB[:], [128, NT, 768], BF16)
                                dump("dtt", [dtt], dtt[:], [128, NT, 16], F32)

                    with S.scope():
                        yss = S.sb("yss", [128, NT, 512], F32)
                        mneg = S.sb("mneg", [128, 2, 512], F32)
                        DMA("sp", mneg[:], din["mneg"].rearrange("a p l -> p a l"), [], [mneg])
                        H = [S.sb("H%d" % i, [128, 8, 64], F32) for i in range(2)]
                        Hb = [S.sb("Hb%d" % i, [128, 8, 64], BF16) for i in range(2)]
                        dAtri_ = [S.sb("dAtri%d" % i, [128, 8, 128], F32) for i in range(2)]
                        negdA_ = [S.sb("negdA%d" % i, [128, 8, 128], F32) for i in range(2)]
                        dec_ = [S.sb("dec%d" % i, [128, 8, 128], F32) for i in range(2)]
                        Mt_ = [S.sb("Mt%d" % i, [128, 8, 128], BF16) for i in range(2)]
                        sm_ = [S.sb("sm%d" % i, [128, 48], F32) for i in range(2)]
                        xdt_ = [S.sb("xdt%d" % i, [128, 8, 64], BF16) for i in range(2)]
                        xdte_ = [S.sb("xdte%d" % i, [128, 8, 64], BF16) for i in range(2)]
                        tmpy_ = [S.sb("tmpy%d" % i, [128, 8, 64], F32) for i in range(2)]
                        tmpH_ = [S.sb("tmpH%d" % i, [128, 8, 64], F32) for i in range(2)]
                        tmpy2 = S.sb("tmpy2", [128, 8, 64], F32)
                        ps_ca_ = [S.ps("ps_ca%d" % i, [128, 512]) for i in range(2)]
                        ps_seg_ = [S.ps("ps_seg%d" % i, [128, 512]) for i in range(2)]
                        ps_y = S.ps("ps_y", [128, 512])
                        ps_yo = S.ps("ps_yo", [128, 512])
                        ps_st = S.ps("ps_st", [128, 512])
                        orders = [list(range(NT)), [1, 0] + list(range(NT - 1, 1, -1))]
                        for d in range(2):
                            MS("dve", H[d][:], 0.0, [H[d]])
                            MS("dve", Hb[d][:], 0.0, [Hb[d]])
                        ywritten = set()
                        for step in range(NT):
                            for d in range(2):
                                tt = orders[d][step]
                                dAtri, negdA, dec, Mt, sm, xdt, xdte, tmpy, tmpH = (dAtri_[d], negdA_[d], dec_[d], Mt_[d], sm_[d], xdt_[d],
                                                                                    xdte_[d], tmpy_[d], tmpH_[d])
                                ps_ca, ps_seg = ps_ca_[d], ps_seg_[d]
                                cs = slice(tt * 128, (tt + 1) * 128)
                                dAd = dA[:, tt, d * 8:(d + 1) * 8]
                                dtd = dtt[:, tt, d * 8:(d + 1) * 8]
                                for g in range(2):
                                    MM(ps_ca[:, g * 128:(g + 1) * 128], BCT[:, g, cs], BCT[:, 2 + g, cs], [BCT.s(g), BCT.s(2 + g)], [ps_ca])
                                MM(ps_ca[:, 256:264], tri[:, d, :], dAd, [tri, dA], [ps_ca])
                                MM(ps_ca[:, 264:272], ones_f[:], dAd, [ones_f, dA], [ps_ca])
                                TT("pool", dAtri[:], tri[:, d, :].unsqueeze(1).to_broadcast([128, 8, 128]),
                                   dAd.unsqueeze(2).to_broadcast([128, 8, 128]), ALU.mult, [tri, dA], [dAtri])
                                ACT(negdA[:], dAd.unsqueeze(2).to_broadcast([128, 8, 128]), AF.Copy, [dA], [negdA], scale=-1.0)
                                for hh in range(2):
                                    MM(ps_seg[:], ones_f[:], dAtri[:, hh * 4:(hh + 1) * 4, :].rearrange("p a b -> p (a b)"), [ones_f, dAtri], [ps_seg],
                                       start=True, stop=False)
                                    MM(ps_seg[:], tri[:, d, :], negdA[:, hh * 4:(hh + 1) * 4, :].rearrange("p a b -> p (a b)"), [tri, negdA], [ps_seg],
                                       start=False, stop=False)
                                    MM(ps_seg[:], ident_f[:], mneg[:, d, :], [ident_f, mneg], [ps_seg], start=False, stop=True)
                                    ACT(dec[:, hh * 4:(hh + 1) * 4, :].rearrange("p a b -> p (a b)"), ps_seg[:], AF.Exp, [ps_seg], [dec])
                                TT("dve", Mt[:].rearrange("p (g r) l -> p g r l", g=2), dec[:].rearrange("p (g r) l -> p g r l", g=2),
                                   ps_ca[:, 0:256].rearrange("p (g l) -> p g l", g=2).unsqueeze(2).to_broadcast([128, 2, 4, 128]), ALU.mult,
                                   [dec, ps_ca], [Mt])
                                ACT(sm[:, 0:16], ps_ca[:, 256:272], AF.Exp, [ps_ca], [sm])
                                CP("act", sm[:, 32:48], ps_ca[:, 256:272], [ps_ca], [sm])
                                TT("dve", sm[:, 16:24], sm[:, 40:48], sm[:, 32:40], ALU.subtract, [sm], [sm])
                                ACT(sm[:, 16:24], sm[:, 16:24], AF.Exp, [sm], [sm])
                                TT("dve", sm[:, 24:32], sm[:, 16:24], dtd, ALU.mult, [sm, dtt], [sm])
                                xs3 = xsB[:, tt, 0:512].rearrange("p (h e) -> p h e", h=8)
                                TT("dve", xdt[:], xs3, dtd.unsqueeze(2).to_broadcast([128, 8, 64]), ALU.mult, [xsB, dtt], [xdt])
                                TT("dve", xdte[:], xs3, sm[:, 24:32].unsqueeze(2).to_broadcast([128, 8, 64]), ALU.mult, [xsB, sm], [xdte])
                                for h_ in range(8):
                                    MM(ps_y[:, h_ * 64:(h_ + 1) * 64], Mt[:, h_, :], xdt[:, h_, :], [Mt, xdt], [ps_y])
                                for g in range(2):
                                    MM(ps_yo[:, g * 256:(g + 1) * 256], BCT[:, 2 + g, cs], Hb[d][:, g * 4:(g + 1) * 4, :].rearrange("p a b -> p (a b)"),
                                       [BCT.s(2 + g), Hb[d]], [ps_yo])
                                for g in range(2):
                                    MM(ps_st[:, g * 256:(g + 1) * 256], xsB[:, tt, 512 + g * 128:512 + (g + 1) * 128],
                                       xdte[:, g * 4:(g + 1) * 4, :].rearrange("p a b -> p (a b)"), [xsB, xdte], [ps_st])
                                TT("dve", tmpy[:], ps_yo[:].rearrange("p (h e) -> p h e", h=8), sm[:, 0:8].unsqueeze(2).to_broadcast([128, 8, 64]),
                                   ALU.mult, [ps_yo, sm], [tmpy])
                                if tt not in ywritten:
                                    ywritten.add(tt)
                                    TT("dve", yss[:, tt, :], ps_y[:], tmpy[:].rearrange("p a b -> p (a b)"), ALU.add, [ps_y, tmpy], [yss.s(tt)])
                                else:
                                    TT("dve", tmpy[:].rearrange("p a b -> p (a b)"), ps_y[:], tmpy[:].rearrange("p a b -> p (a b)"), ALU.add,
                                       [ps_y, tmpy], [tmpy])
                                    TT("pool", yss[:, tt, :], yss[:, tt, :], tmpy[:].rearrange("p a b -> p (a b)"), ALU.add,
                                       [yss.s(tt), tmpy], [yss.s(tt)])
                                TT("dve", tmpH[:], H[d][:], sm[:, 8:16].unsqueeze(2).to_broadcast([128, 8, 64]), ALU.mult, [H[d], sm], [tmpH])
                                TT("dve", H[d][:].rearrange("p a b -> p (a b)"), tmpH[:].rearrange("p a b -> p (a b)"), ps_st[:], ALU.add,
                                   [tmpH, ps_st], [H[d]])
                                CP("act", Hb[d][:], H[d][:], [H[d]], [Hb[d]])
                        tmpy = tmpy_[0]
                        gg = S.sb("gg", [128, 512], F32)
                        junk3 = S.sb("junk3", [128, 256], BF16)
                        gs = S.sb("gs", [128, 8], F32)
                        for tt in tiles:
                            xs3 = xsB[:, tt, 0:512].rearrange("p (h e) -> p h e", h=8)
                            TT("pool", tmpy[:], xs3, dsk[:].unsqueeze(2).to_broadcast([128, 8, 64]), ALU.mult, [xsB, dsk], [tmpy])
                            TT("dve", tmpy2[:].rearrange("p a b -> p (a b)"), yss[:, tt, :], tmpy[:].rearrange("p a b -> p (a b)"), ALU.add,
                               [yss.s(tt), tmpy], [tmpy2])
                            TT("dve", gg[:], tmpy2[:].rearrange("p a b -> p (a b)"), sz[:, tt, :], ALU.mult, [tmpy2, sz.s(tt)], [gg])
                            for g in range(2):
                                ACT(junk3[:], gg[:, g * 256:(g + 1) * 256], AF.Square, [gg], [junk3, gs], accum_out=gs[:, g:g + 1])
                            ACT(gs[:, 2:4], gs[:, 0:2], AF.Sqrt, [gs], [gs], scale=1.0 / 256, bias=EPS)
                            RCP(gs[:, 4:6], gs[:, 2:4], [gs], [gs])
                            for g in range(2):
                                STT(mixtok[:, tt, 512 + g * 256:512 + (g + 1) * 256], gg[:, g * 256:(g + 1) * 256], gs[:, 4 + g:5 + g],
                                    ssdw[:, g * 256:(g + 1) * 256], ALU.mult, ALU.mult, [gg, gs, ssdw], [mixtok.s(tt)])
                        if l == 0 and b == 0:
                            dump("mixssd", [mixtok.s(t_) for t_ in tiles], mixtok[:, :, 512:1024], [128, NT, 512], BF16)
                            dump("yss", [yss.s(t_) for t_ in tiles], yss[:], [128, NT, 512], F32)

                with S.scope():
                    wo = S.sb("wo", [128, 8, D], BF16)
                    DMA("pool", wo[:], din["w_out"][l].rearrange("(c p) n -> p c n", p=128), [], [wo])
                    wr = S.sb("wr", [128, 8, NE], F32)
                    DMA("sp", wr[:], din["w_router"][l].rearrange("(c p) n -> p c n", p=128), [], [wr])
                    modG = S.sb("modG", [128, 2, D], F32)
                    modA = S.sb("modA2", [128, 2, D], F32)
                    modS = S.sb("modS2", [128, 2, D], F32)
                    nw = S.sb("nw2", [128, D], F32)
                    load_mod(modG, 2)
                    load_mod(modS, 3)
                    load_mod(modA, 4)
                    DMA("sp", nw[:], din["norm2_w"][l:l + 1, :].to_broadcast([128, D]), [], [nw])
                    for j in range(2):
                        STT(modA[:, j, :], modA[:, j, :], 1.0, nw[:], ALU.add, ALU.mult, [modA, nw], [modA])
                    mixT = [S.sb("mixT%d" % i, [128, 8, 128], BF16) for i in range(2)]
                    xin = [S.sb("xin2_%d" % i, [128, D], F32) for i in range(2)]
                    xnew = [S.sb("xnew%d" % i, [128, D], F32) for i in range(2)]
                    tg_ = [S.sb("tg%d" % i, [128, D], F32) for i in range(2)]
                    t1_ = [S.sb("t1b%d" % i, [128, D], F32) for i in range(2)]
                    h2f_ = [S.sb("h2f%d" % i, [128, D], F32) for i in range(2)]
                    h2fT_ = [S.sb("h2fT%d" % i, [128, 8, 128], F32) for i in range(2)]
                    junk_ = [S.sb("junk4_%d" % i, [128, D], BF16) for i in range(2)]
                    st = [S.sb("st2_%d" % i, [128, 4], F32) for i in range(2)]
                    rt_ = [S.sb("rt%d" % i, [128, 40], F32) for i in range(2)]
                    att = [S.sb("att%d" % i, [NE, 128], F32) for i in range(2)]
                    pT8_ = [S.ps("pT8o%d" % i, [128, 8, 128], BF16) for i in range(2)]
                    ps_out = S.ps("ps_out", [128, 1024])
                    ps_hT = S.ps("ps_hT", [128, 8, 128], F32)
                    ps_la_ = [S.ps("ps_la%d" % i, [128, 512]) for i in range(2)]
                    ptiles = [t_ for t_ in tiles if not (last and t_ < 2)]
                    for tt in ptiles:
                        k2 = tt % 2
                        j = 1 if tt < 2 else 0
                        tg, t1, h2f, h2fT, junk, rt, pT8 = tg_[k2], t1_[k2], h2f_[k2], h2fT_[k2], junk_[k2], rt_[k2], pT8_[k2]
                        ps_lg = ps_la_[k2]
                        for c_ in range(8):
                            TR(pT8[:, c_, :], mixtok[:, tt, c_ * 128:(c_ + 1) * 128], ident_b[:], [mixtok.s(tt), ident_b], [pT8])
                        CP("act", mixT[k2][:], pT8[:], [pT8], [mixT[k2]])
                        for half in range(2):
                            for c_ in range(8):
                                MM(ps_out[:, half * 512:(half + 1) * 512], mixT[k2][:, c_, :], wo[:, c_, half * 512:(half + 1) * 512],
                                   [mixT[k2], wo], [ps_out], start=(c_ == 0), stop=(c_ == 7))
                        src, sb_ = xsrc(tt)
                        DMA("sp", xin[k2][:], src, sb_, [xin[k2]])
                        TT("dve", tg[:], ps_out[:], modG[:, j, :], ALU.mult, [ps_out, modG], [tg])
                        TT("dve", xnew[k2][:], tg[:], xin[k2][:], ALU.add, [tg, xin[k2]], [xnew[k2]])
                        DMA("sp", xres[tt * 128:(tt + 1) * 128, :], xnew[k2][:], [xnew[k2]], [xres_b[tt]])
                        ACT(junk[:], xnew[k2][:], AF.Square, [xnew[k2]], [junk, st[k2]], accum_out=st[k2][:, 0:1])
                        ACT(st[k2][:, 1:2], st[k2][:, 0:1], AF.Sqrt, [st[k2]], [st[k2]], scale=1.0 / D, bias=EPS)
                        RCP(st[k2][:, 2:3], st[k2][:, 1:2], [st[k2]], [st[k2]])
                        STT(t1[:], xnew[k2][:], st[k2][:, 2:3], modA[:, j, :], ALU.mult, ALU.mult, [xnew[k2], st[k2], modA], [t1])
                        TT("dve", h2f[:], t1[:], modS[:, j, :], ALU.add, [t1, modS], [h2f])
                        CP("act", tokbuf[:, tt, :], h2f[:], [h2f], [tokbuf.s(tt)])
                        for c_ in range(8):
                            TR(ps_hT[:, c_, :], h2f[:, c_ * 128:(c_ + 1) * 128], ident_f[:], [h2f, ident_f], [ps_hT])
                        CP("dve", h2fT[:], ps_hT[:], [ps_hT], [h2fT])
                        for c_ in range(8):
                            MM(ps_lg[:, 0:16], h2fT[:, c_, :], wr[:, c_, :], [h2fT, wr], [ps_lg], start=(c_ == 0), stop=(c_ == 7))
                        RED(rt[:, 0:1], ps_lg[:, 0:16], [ps_lg], [rt], op=ALU.max)
                        TS("dve", rt[:, 1:2], rt[:, 0:1], -1.0, None, ALU.mult, None, [rt], [rt])
                        ACT(rt[:, 8:24], ps_lg[:, 0:16], AF.Exp, [ps_lg, rt], [rt], bias=rt[:, 1:2], accum_out=rt[:, 2:3])
                        RCP(rt[:, 3:4], rt[:, 2:3], [rt], [rt])
                        TS("dve", rt[:, 24:40], rt[:, 8:24], rt[:, 3:4], None, ALU.mult, None, [rt], [rt])
                        TR(ps_lg[0:NE, 128:256], rt[:, 24:40], ident_f[:], [rt, ident_f], [ps_lg])
                        CP("act", att[k2][:], ps_lg[0:NE, 128:256], [ps_lg], [att[k2]])
                        DMA("sp", affd[:, tt * 128:(tt + 1) * 128], att[k2][:], [att[k2]], [affd_b])
                    if l == 0 and b == 0:
                        dump("h2tok", [tokbuf.s(t_) for t_ in ptiles], tokbuf[:], [128, NT, 1024], BF16)
                        dump("affT", [affd_b], affd, [NE, T], F32)
                        dump("xres", [xres_b[t_] for t_ in ptiles], xres, [T, D], F32)
                if moe:
                    nslot = CAP if last else NSLOT
                    cchunks = [(0, 128), (128, 128)] + ([] if last else [(256, CAPC)])
                    ltiles = list(range(2, NT))
                    with S.scope():
                        vals = S.sb("vals", [NE, NSLOT], F32)
                        idxu = S.sb("idxu", [NE, NSLOT], U32)
                        idxf = S.sb("idxf", [NE, NSLOT], F32)
                        valsT = S.sb("valsT", [128, 3, NE], F32)
                        idxT = S.sb("idxT", [128, 3, NE], F32)
                        with S.scope():
                            affw = S.sb("affw", [NE, T], F32)
                            DMA("sp", affw[:], affd, [affd_b], [affw])
                            segs_k = [(256, SEQ, 0, CAP // 8)] + ([] if last else [(0, CTX, CAP, CAPC // 8)])
                            for (a0, an, s0, nr) in segs_k:
                                src = affw[:, a0:a0 + an]
                                for r in range(nr):
                                    vs = vals[:, s0 + r * 8:s0 + (r + 1) * 8]
                                    S.op("dve", lambda e, vs=vs, src=src: e.max(out=vs, in_=src), [affw], [vals])
                                    ix = idxu[:, s0 + r * 8:s0 + (r + 1) * 8]
                                    S.op("dve", lambda e, ix=ix, vs=vs, src=src: e.max_index(out=ix, in_max=vs, in_values=src),
                                         [affw, vals], [idxu])
                                    if r < nr - 1:
                                        S.op("dve", lambda e, vs=vs, src=src: e.match_replace(out=src, in_to_replace=vs, in_values=src,
                                                                                             imm_value=-1.0), [affw, vals], [affw])
                            CP("dve", idxf[:, 0:nslot], idxu[:, 0:nslot], [idxu], [idxf])
                            ps_t = S.ps("ps_t", [128, 2, 3, NE])
                            for ck, (c0, cn) in enumerate(cchunks):
                                TR(ps_t[0:cn, 0, ck, :], vals[:, c0:c0 + cn], ident_f[0:NE, 0:NE], [vals, ident_f], [ps_t])
                                TR(ps_t[0:cn, 1, ck, :], idxf[:, c0:c0 + cn], ident_f[0:NE, 0:NE], [idxf, ident_f], [ps_t])
                                CP("dve", valsT[0:cn, ck, :], ps_t[0:cn, 0, ck, :], [ps_t], [valsT])
                                CP("dve", idxT[0:cn, ck, :], ps_t[0:cn, 1, ck, :], [ps_t], [idxT])
                            if l == 0 and b == 0:
                                dump("vals", [vals], vals[:], [NE, NSLOT], F32)
                                dump("idxf", [idxf], idxf[:], [NE, NSLOT], F32)
                        yacc = S.sb("yacc", [128, NT, 1024], F32)
                        yall = [yacc.s(t_) for t_ in tiles]
                        MS("pool", yacc[:], 0.0, yall)
                        with S.scope():
                            ring = [S.sb("wring%d" % i, [128, 3072], BF16) for i in range(5)]
                            Ssl_ = [S.sb("Ssl%d" % i, [128, 16, CAP], BF16) for i in range(2)]
                            Ssc_ = [S.sb("Ssc%d" % i, [128, 2, CAPC], BF16) for i in range(2)]
                            idm_ = [S.sb("idm%d" % i, [NE, NSLOT], F32) for i in range(2)]
                            STl = S.sb("STl", [128, 2, SEQ], BF16)
                            STc = S.sb("STc", [CAPC, CTX], BF16)
                            iota = S.sb("iota", [128, SEQ], F32)
                            DMA("sp", iota[:], din["iota"], [], [iota])
                            xeT = S.sb("xeT", [128, 8, NSLOT], BF16)
                            hid = S.sb("hid", [128, NF, NSLOT], BF16)
                            sgt = S.sb("sgt", [128, NSLOT], F32)
                            yeg = S.sb("yeg", [128, 3, 1024], BF16)
                            ps_ab = [S.ps("ps_ab%d" % i, [128, 512]) for i in range(2)]
                            ps_gt = S.ps("ps_gt", [128, 512])
                            ps_up = S.ps("ps_up", [128, 512])
                            ps_d = [S.ps("ps_d%d" % i, [128, 512]) for i in range(2)]
                            ps_sc = [S.ps("ps_sc%d" % i, [128, 512]) for i in range(2)]
                            fgroups = [(0, 3), (3, 3), (6, 3), (9, 2)]
                            nring = 0
                            nab = 0
                            nd = 0
                            nsc = 0
                            hall_ = [tokbuf.s(t_) for t_ in tiles]
                            pf = {"issued": 0, "consumed": 0}

                            def issue_piece(k):
                                e2, r2 = divmod(k, 12)
                                r_ = ring[k % 5]
                                if r2 < 8:
                                    gi = r2 // 2
                                    nm = "w_up" if (r2 % 2) else "w_gate"
                                    f0, nf = fgroups[gi]
                                    DMA("pool", r_[:, 0:8 * nf * 128].rearrange("p (c n) -> p c n", c=8),
                                        din[nm][l, e2, :, f0 * 128:(f0 + nf) * 128].rearrange("(c p) n -> p c n", p=128), [], [r_])
                                else:
                                    f0, nf = fgroups[r2 - 8]
                                    DMA("pool", r_[:, 0:nf * 1024].rearrange("p (c n) -> p c n", c=nf),
                                        din["w_down"][l, e2, f0 * 128:(f0 + nf) * 128, :].rearrange("(c p) n -> p c n", p=128), [], [r_])

                            def release(n):
                                pf["consumed"] += n
                                while pf["issued"] < pf["consumed"] + 5 and pf["issued"] < NE * 12:
                                    issue_piece(pf["issued"])
                                    pf["issued"] += 1

                            release(0)
                            nabc = {"n": 0}

                            def build_sel(e2):
                                idm = idm_[e2 % 2]
                                Ssl2, Ssc2 = Ssl_[e2 % 2], Ssc_[e2 % 2]
                                TS("dve", idm[:, 0:nslot], idxf[:, 0:nslot], ident_f[0:NE, e2:e2 + 1], None, ALU.mult, None, [idxf, ident_f], [idm])
                                pi = ps_ab[nabc["n"] % 2]
                                nabc["n"] += 1
                                MM(pi[:, 0:nslot], ones_f[0:NE, :], idm[:, 0:nslot], [ones_f, idm], [pi])
                                for j in range(16):
                                    TS("dve", Ssl2[:, j, :], pi[:, 0:CAP], tpos[:, j:j + 1], None, ALU.is_equal, None, [pi, tpos], [Ssl2])
                                if not last:
                                    for j in range(2):
                                        TS("dve", Ssc2[:, j, :], pi[:, CAP:NSLOT], tpos[:, j:j + 1], None, ALU.is_equal, None, [pi, tpos], [Ssc2])

                            def build_ST(e2):
                                for ck in range(2):
                                    TS("dve", STl[:, ck, :], iota[:], idxT[:, ck, e2:e2 + 1], None, ALU.is_equal, None, [iota, idxT], [STl])
                                if not last:
                                    TS("dve", STc[:], iota[0:CAPC, 0:CTX], idxT[0:CAPC, 2, e2:e2 + 1], None, ALU.is_equal, None, [iota, idxT], [STc])

                            for e_ in range(NE):
                                pieces = {}
                                for gi in range(4):
                                    for nm in ("w_gate", "w_up"):
                                        pieces[(nm, gi)] = ring[(e_ * 12 + gi * 2 + (nm == "w_up")) % 5]
                                if e_ == 0:
                                    build_sel(0)
                                Ssl, Ssc = Ssl_[e_ % 2], Ssc_[e_ % 2]
                                for dc in range(8):
                                    pg = ps_ab[nabc["n"] % 2]
                                    nabc["n"] += 1
                                    for j in range(16):
                                        MM(pg[:, 0:CAP], tokbuf[:, 2 + j, dc * 128:(dc + 1) * 128], Ssl[:, j, :],
                                           [tokbuf.s(2 + j), Ssl], [pg], start=(j == 0), stop=(j == 15))
                                    if not last:
                                        for j in range(2):
                                            MM(pg[:, CAP:NSLOT], tokbuf[:, j, dc * 128:(dc + 1) * 128], Ssc[:, j, :],
                                               [tokbuf.s(j), Ssc], [pg], start=(j == 0), stop=(j == 1))
                                    CP("act", xeT[:, dc, 0:nslot], pg[:, 0:nslot], [pg], [xeT])
                                for gi, (f0, nf) in enumerate(fgroups):
                                    wg_ = pieces[("w_gate", gi)]
                                    wu_ = pieces[("w_up", gi)]
                                    wg3 = wg_[:, 0:8 * nf * 128].rearrange("p (c n) -> p c n", c=8)
                                    wu3 = wu_[:, 0:8 * nf * 128].rearrange("p (c n) -> p c n", c=8)
                                    for fi in range(nf):
                                        f = f0 + fi
                                        for c_ in range(8):
                                            MM(ps_gt[:, 0:nslot], wg3[:, c_, fi * 128:(fi + 1) * 128], xeT[:, c_, 0:nslot], [wg_, xeT], [ps_gt],
                                               start=(c_ == 0), stop=(c_ == 7))
                                        for c_ in range(8):
                                            MM(ps_up[:, 0:nslot], wu3[:, c_, fi * 128:(fi + 1) * 128], xeT[:, c_, 0:nslot], [wu_, xeT], [ps_up],
                                               start=(c_ == 0), stop=(c_ == 7))
                                        ACT(sgt[:, 0:nslot], ps_gt[:, 0:nslot], AF.Silu, [ps_gt], [sgt])
                                        TT("dve", hid[:, f, 0:nslot], sgt[:, 0:nslot], ps_up[:, 0:nslot], ALU.mult, [sgt, ps_up], [hid])
                                    release(2)
                                dps = [ring[(e_ * 12 + 8 + gi) % 5] for gi in range(4)]
                                for ck, (c0, cn) in enumerate(cchunks):
                                    for half in range(2):
                                        pd = ps_d[nd % 2]
                                        nd += 1
                                        for gi, (f0, nf) in enumerate(fgroups):
                                            w3 = dps[gi][:, 0:nf * 1024].rearrange("p (c n) -> p c n", c=nf)
                                            for fi in range(nf):
                                                f = f0 + fi
                                                MM(pd[0:cn, :], hid[:, f, c0:c0 + cn], w3[:, fi, half * 512:(half + 1) * 512], [hid, dps[gi]], [pd],
                                                   start=(f == 0), stop=(f == NF - 1))
                                        ACT(yeg[0:cn, ck, half * 512:(half + 1) * 512], pd[0:cn, :], AF.Copy, [pd, valsT], [yeg],
                                            scale=valsT[0:cn, ck, e_:e_ + 1])
                                release(4)
                                build_ST(e_)
                                if e_ + 1 < NE:
                                    build_sel(e_ + 1)
                                for j in range(16):
                                    for half in range(2):
                                        pq = ps_sc[nsc % 2]
                                        nsc += 1
                                        for ck in range(2):
                                            MM(pq[:], STl[:, ck, j * 128:(j + 1) * 128], yeg[:, ck, half * 512:(half + 1) * 512], [STl, yeg], [pq],
                                               start=(ck == 0), stop=(ck == 1))
                                        ya = yacc[:, 2 + j, half * 512:(half + 1) * 512]
                                        TT("dve", ya, pq[:], ya, ALU.add, [pq, yacc.s(2 + j)], [yacc.s(2 + j)])
                                if not last:
                                    for j in range(2):
                                        for half in range(2):
                                            pq = ps_sc[nsc % 2]
                                            nsc += 1
                                            MM(pq[:], STc[:, j * 128:(j + 1) * 128], yeg[0:CAPC, 2, half * 512:(half + 1) * 512], [STc, yeg], [pq])
                                            ya = yacc[:, j, half * 512:(half + 1) * 512]
                                            TT("dve", ya, pq[:], ya, ALU.add, [pq, yacc.s(j)], [yacc.s(j)])
                            if l == 0 and b == 0:
                                dump("yacc", yall, yacc[:], [128, NT, 1024], F32)
                        with S.scope():
                            modG = S.sb("modG2", [128, 2, D], F32)
                            load_mod(modG, 5)
                            xin = [S.sb("xin3_%d" % i, [128, D], F32) for i in range(2)]
                            xo = [S.sb("xo%d" % i, [128, D], F32) for i in range(2)]
                            tg = S.sb("tg2", [128, D], F32)
                            for tt in [t_ for t_ in tiles if not (last and t_ < 2)]:
                                k2 = tt % 2
                                j = 1 if tt < 2 else 0
                                DMA("sp", xin[k2][:], xres[tt * 128:(tt + 1) * 128, :], [xres_b[tt]], [xin[k2]])
                                TT("dve", tg[:], yacc[:, tt, :], modG[:, j, :], ALU.mult, [yacc.s(tt), modG], [tg])
                                TT("pool", xo[k2][:], tg[:], xin[k2][:], ALU.add, [tg, xin[k2]], [xo[k2]])
                                if last:
                                    DMA("sp", out[b, (tt - 2) * 128:(tt - 1) * 128, :], xo[k2][:], [xo[k2]], [])
                                else:
                                    DMA("sp", xres[tt * 128:(tt + 1) * 128, :], xo[k2][:], [xo[k2]], [xres_b[tt]])
                            if l == 0 and b == 0:
                                dump("xfin", list(xres_b), xres, [T, D], F32)
                S.pop()
                S.new_epoch()
        S.barrier()
        S.emit()
    return nc, dumps


_CACHE = {}


def kernel(**inputs):
    nb = 2
    n_cores = 8
    if "nc" not in _CACHE:
        _CACHE["nc"] = build(nb=nb, depth=DEPTH)[0]
    nc = _CACHE["nc"]
    consts = host_consts()
    in_maps = []
    for i in range(n_cores):
        m = {"x": np.ascontiguousarray(inputs["x"][i * nb:(i + 1) * nb]),
             "ctx": np.ascontiguousarray(inputs["ctx"][i * nb:(i + 1) * nb]),
             "c": np.ascontiguousarray(inputs["c"][i * nb:(i + 1) * nb])}
        for k_ in PARAM_SHAPES:
            m[k_] = np.ascontiguousarray(inputs[k_], dtype=np.float32)
        for k_ in CONST_SHAPES:
            m["k_" + k_] = consts[k_]
        in_maps.append(m)
    res = run_bass_kernel_spmd(nc, in_maps, core_ids=list(range(n_cores)))
    return np.concatenate([r["out"] for r in res.results], axis=0)
```
